# Optimizing a Trainium2 kernel written in Bass

```python
import math
import jax, jax.numpy as jnp
from jax import lax
import numpy as np

D_MODEL = 1024
BATCH = 8
SEQ = 2048
DEPTH = 4

N_BRANCHES = 3
SSM_WIDTH = D_MODEL // 2
SSM_GROUP = 16
SSM_GROUPS = SSM_WIDTH // SSM_GROUP
SSM_STATE = 64
SSM_DT_MIN = 1e-3
SSM_DT_MAX = 1e-1
MLSTM_WIDTH = D_MODEL // 2
MLSTM_HEADS = 4
MLSTM_HEAD_DIM = MLSTM_WIDTH // MLSTM_HEADS
MLSTM_CHUNK = 128
CONV_WIDTH = 4
MOBA_WIDTH = D_MODEL // 2
MOBA_HEADS = 8
MOBA_HEAD_DIM = MOBA_WIDTH // MOBA_HEADS
MOBA_BLOCK = 256
MOBA_TOPK = 3
MOBA_QUERY_BLOCK = 32
REL_BUCKETS = 32
REL_MAX_DIST = 128
D_FF = 4 * D_MODEL
RMS_EPS = 1e-6
NEG_INF = -1e30

IN_SIZES = (SSM_WIDTH,
            MLSTM_WIDTH, MLSTM_WIDTH, MLSTM_WIDTH, MLSTM_WIDTH, MLSTM_HEADS, MLSTM_HEADS,
            MOBA_WIDTH, MOBA_WIDTH, MOBA_WIDTH,
            N_BRANCHES * D_MODEL)
IN_COLS = sum(IN_SIZES)

kernel_name = "hybrid_s5_mlstm_moba_gated_block"


def rms_norm(x, g):
    xf = x.astype(jnp.float32)
    y = xf * lax.rsqrt(jnp.mean(xf * xf, axis=-1, keepdims=True) + RMS_EPS)
    return (y * g.astype(jnp.float32)).astype(x.dtype)


def split_cols(t, sizes):
    outs, start = [], 0
    for s in sizes:
        outs.append(t[..., start:start + s])
        start += s
    return outs


def causal_conv(x, w):
    k, c = w.shape
    return lax.conv_general_dilated(x, w[:, None, :], window_strides=(1,), padding=[(k - 1, 0)],
                                    dimension_numbers=('NWC', 'WIO', 'NWC'), feature_group_count=c)


def _ssm_combine(left, right):
    a1r, a1i, b1r, b1i = left
    a2r, a2i, b2r, b2i = right
    return (a2r * a1r - a2i * a1i, a2r * a1i + a2i * a1r,
            a2r * b1r - a2i * b1i + b2r, a2r * b1i + a2i * b1r + b2i)


def ssm_branch(u, a_re, a_im, log_dt, b_re, b_im, c_re, c_im, d_skip, w_glu):
    f32 = jnp.float32
    bsz, s, w = u.shape
    uf = u.astype(f32)
    ug = uf.reshape(bsz, s, SSM_GROUPS, SSM_GROUP)
    ar, ai = a_re.astype(f32), a_im.astype(f32)
    dt = jnp.exp(log_dt.astype(f32))[:, None]
    decay = jnp.exp(dt * ar)
    abar_r, abar_i = decay * jnp.cos(dt * ai), decay * jnp.sin(dt * ai)
    den = ar * ar + ai * ai
    nr, ni = abar_r - 1.0, abar_i
    fr, fi = (nr * ar + ni * ai) / den, (ni * ar - nr * ai) / den
    br, bi = b_re.astype(f32), b_im.astype(f32)
    bbar_r = fr[..., None] * br - fi[..., None] * bi
    bbar_i = fr[..., None] * bi + fi[..., None] * br
    bu_r = jnp.einsum('bsgh,gph->bsgp', ug, bbar_r)
    bu_i = jnp.einsum('bsgh,gph->bsgp', ug, bbar_i)
    a_seq_r = jnp.broadcast_to(abar_r, (1, s, SSM_GROUPS, SSM_STATE))
    a_seq_i = jnp.broadcast_to(abar_i, (1, s, SSM_GROUPS, SSM_STATE))
    _, _, st_r, st_i = lax.associative_scan(_ssm_combine, (a_seq_r, a_seq_i, bu_r, bu_i), axis=1)
    y = (jnp.einsum('bsgp,ghp->bsgh', st_r, c_re.astype(f32))
         - jnp.einsum('bsgp,ghp->bsgh', st_i, c_im.astype(f32)))
    y = y.reshape(bsz, s, w) + d_skip.astype(f32) * uf
    yg = jax.nn.gelu(y)
    out = yg * jax.nn.sigmoid(yg @ w_glu.astype(f32))
    return out.astype(u.dtype)


def mlstm_branch(q, k, v, o_pre, i_pre, f_pre, i_bias, f_bias, head_gain):
    f32 = jnp.float32
    bsz, s, w = q.shape
    nh, dh, lc = MLSTM_HEADS, MLSTM_HEAD_DIM, MLSTM_CHUNK
    nc = s // lc

    def heads(t):
        return t.astype(f32).reshape(bsz, nc, lc, nh, dh).transpose(0, 3, 1, 2, 4)

    def gates(t, bias):
        return (t.astype(f32) + bias.astype(f32)).reshape(bsz, nc, lc, nh).transpose(0, 3, 1, 2)

    qh, kh, vh = heads(q), heads(k) * (dh ** -0.5), heads(v)
    ig = gates(i_pre, i_bias)
    lf = jax.nn.log_sigmoid(gates(f_pre, f_bias))
    bcum = jnp.cumsum(lf, axis=-1)
    gtot = bcum[..., -1]

    causal = jnp.tril(jnp.ones((lc, lc), dtype=bool))
    log_d = jnp.where(causal, bcum[..., :, None] - bcum[..., None, :] + ig[..., None, :], -jnp.inf)
    m_intra = jnp.max(log_d, axis=-1)

    log_w = gtot[..., None] - bcum + ig
    m_loc = jnp.max(log_w, axis=-1)
    wgt = jnp.exp(log_w - m_loc[..., None])
    d_c = jnp.einsum('bhcsv,bhcsk->bhcvk', wgt[..., None] * vh, kh)
    d_n = jnp.einsum('bhcs,bhcsk->bhck', wgt, kh)

    def step(carry, xs):
        c_st, n_st, m_st = carry
        dc, dn, g, ml = xs
        m_new = jnp.maximum(g + m_st, ml)
        a = jnp.exp(g + m_st - m_new)
        bb = jnp.exp(ml - m_new)
        c_new = a[..., None, None] * c_st + bb[..., None, None] * dc
        n_new = a[..., None] * n_st + bb[..., None] * dn
        return (c_new, n_new, m_new), (c_st, n_st, m_st)

    init = (jnp.zeros((bsz, nh, dh, dh), f32), jnp.zeros((bsz, nh, dh), f32), jnp.zeros((bsz, nh), f32))
    xs = (jnp.moveaxis(d_c, 2, 0), jnp.moveaxis(d_n, 2, 0), jnp.moveaxis(gtot, 2, 0), jnp.moveaxis(m_loc, 2, 0))
    _, (c_prev, n_prev, m_prev) = lax.scan(step, init, xs)
    c_prev = jnp.moveaxis(c_prev, 0, 2)
    n_prev = jnp.moveaxis(n_prev, 0, 2)
    m_prev = jnp.moveaxis(m_prev, 0, 2)

    log_inter = bcum + m_prev[..., None]
    m_q = jnp.maximum(log_inter, m_intra)
    s_mat = jnp.einsum('bhcjd,bhcsd->bhcjs', qh, kh) * jnp.exp(log_d - m_q[..., None])
    inter = jnp.exp(log_inter - m_q)
    num = (jnp.einsum('bhcjs,bhcsv->bhcjv', s_mat, vh)
           + inter[..., None] * jnp.einsum('bhcvk,bhcjk->bhcjv', c_prev, qh))
    den = jnp.sum(s_mat, axis=-1) + inter * jnp.einsum('bhck,bhcjk->bhcj', n_prev, qh)
    h = num / jnp.maximum(jnp.abs(den), jnp.exp(-m_q))[..., None]
    h = h * lax.rsqrt(jnp.mean(h * h, axis=-1, keepdims=True) + RMS_EPS)
    h = h.transpose(0, 2, 3, 1, 4).reshape(bsz, s, w) * head_gain.astype(f32)
    out = jax.nn.sigmoid(o_pre.astype(f32)) * h
    return out.astype(q.dtype)


def t5_bucket(dist):
    max_exact = REL_BUCKETS // 2
    is_small = dist < max_exact
    large = max_exact + (jnp.log(jnp.maximum(dist, 1).astype(jnp.float32) / max_exact)
                         / math.log(REL_MAX_DIST / max_exact) * (REL_BUCKETS - max_exact)).astype(jnp.int32)
    large = jnp.minimum(large, REL_BUCKETS - 1)
    return jnp.where(is_small, dist, large)


def moba_branch(q, k, v, rel_bias):
    f32 = jnp.float32
    bsz, s, w = q.shape
    nh, dh, bs, qb = MOBA_HEADS, MOBA_HEAD_DIM, MOBA_BLOCK, MOBA_QUERY_BLOCK
    nb = -(-s // bs)
    s_pad = nb * bs
    topk = min(MOBA_TOPK, nb)
    nq = s // qb
    scale = dh ** -0.5

    def heads(t):
        return t.reshape(bsz, s, nh, dh).transpose(0, 2, 1, 3)

    pad = ((0, 0), (0, 0), (0, s_pad - s), (0, 0))
    kb = jnp.pad(heads(k), pad).reshape(bsz, nh, nb, bs, dh)
    vb = jnp.pad(heads(v), pad).reshape(bsz, nh, nb, bs, dh)
    k_mean = jnp.mean(kb.astype(f32), axis=3)
    q_blocks = heads(q).reshape(bsz, nh, nq, qb, dh).transpose(2, 0, 1, 3, 4)
    bias_tab = rel_bias.astype(f32)
    b_idx = jnp.arange(bsz)[:, None, None, None]
    h_idx = jnp.arange(nh)[None, :, None, None]
    blk_pos = jnp.arange(bs)

    def attend(args):
        qi, q_blk = args
        q_pos = qi * qb + jnp.arange(qb)
        cur = (qi * qb) // bs
        gate = jnp.einsum('bhqd,bhnd->bhqn', q_blk.astype(f32), k_mean)
        gate = jnp.where(jnp.arange(nb) < cur, gate, NEG_INF)
        _, sel = lax.top_k(gate, topk)
        sel_valid = (jnp.arange(topk) < cur)[:, None]
        k_sel = kb[b_idx, h_idx, sel]
        v_sel = vb[b_idx, h_idx, sel]
        lg_sel = jnp.einsum('bhqd,bhqjkd->bhqjk', q_blk, k_sel).astype(f32) * scale
        dist_sel = q_pos[None, None, :, None, None] - (sel[..., None] * bs + blk_pos)
        bias_sel = bias_tab[h_idx[..., None], t5_bucket(jnp.maximum(dist_sel, 0))]
        lg_sel = jnp.where(sel_valid, lg_sel + bias_sel, NEG_INF)
        k_own = lax.dynamic_slice_in_dim(kb, cur, 1, axis=2)[:, :, 0]
        v_own = lax.dynamic_slice_in_dim(vb, cur, 1, axis=2)[:, :, 0]
        lg_own = jnp.einsum('bhqd,bhkd->bhqk', q_blk, k_own).astype(f32) * scale
        dist_own = q_pos[:, None] - (cur * bs + blk_pos)[None, :]
        bias_own = bias_tab[:, t5_bucket(jnp.maximum(dist_own, 0))]
        lg_own = jnp.where(dist_own >= 0, lg_own + bias_own[None], NEG_INF)
        logits = jnp.concatenate([lg_sel.reshape(bsz, nh, qb, topk * bs), lg_own], axis=-1)
        p = jax.nn.softmax(logits, axis=-1).astype(v.dtype)
        p_sel = p[..., :topk * bs].reshape(bsz, nh, qb, topk, bs)
        p_own = p[..., topk * bs:]
        return (jnp.einsum('bhqjk,bhqjkd->bhqd', p_sel, v_sel)
                + jnp.einsum('bhqk,bhkd->bhqd', p_own, v_own))

    outs = lax.map(attend, (jnp.arange(nq), q_blocks))
    return outs.transpose(1, 0, 3, 2, 4).reshape(bsz, s, w).astype(q.dtype)


def setup_inputs(seed: int = 0) -> dict:
    key = jax.random.key(seed)
    ks = jax.random.split(key, 32)
    f32 = jnp.float32
    nrm = lambda k, shape, sc: jax.random.normal(k, shape, f32) * sc
    gain = lambda k: 1.0 + 0.02 * jax.random.normal(k, (DEPTH, D_MODEL), f32)
    n_idx = jnp.arange(SSM_STATE, dtype=f32)
    return {
        "x": jax.random.normal(ks[0], (BATCH, SEQ, D_MODEL), f32),
        "w_in": nrm(ks[1], (DEPTH, D_MODEL, IN_COLS), D_MODEL ** -0.5),
        "conv_w": nrm(ks[2], (DEPTH, CONV_WIDTH, 2 * MLSTM_WIDTH), CONV_WIDTH ** -0.5),
        "ssm_a_re": -0.5 + 0.01 * jax.random.normal(ks[3], (DEPTH, SSM_GROUPS, SSM_STATE), f32),
        "ssm_a_im": math.pi * n_idx + 0.01 * jax.random.normal(ks[4], (DEPTH, SSM_GROUPS, SSM_STATE), f32),
        "ssm_log_dt": jax.random.uniform(ks[5], (DEPTH, SSM_GROUPS), f32, math.log(SSM_DT_MIN), math.log(SSM_DT_MAX)),
        "ssm_b_re": nrm(ks[6], (DEPTH, SSM_GROUPS, SSM_STATE, SSM_GROUP), (2 * SSM_GROUP) ** -0.5),
        "ssm_b_im": nrm(ks[7], (DEPTH, SSM_GROUPS, SSM_STATE, SSM_GROUP), (2 * SSM_GROUP) ** -0.5),
        "ssm_c_re": nrm(ks[8], (DEPTH, SSM_GROUPS, SSM_GROUP, SSM_STATE), (2 * SSM_STATE) ** -0.5),
        "ssm_c_im": nrm(ks[9], (DEPTH, SSM_GROUPS, SSM_GROUP, SSM_STATE), (2 * SSM_STATE) ** -0.5),
        "ssm_d": nrm(ks[10], (DEPTH, SSM_WIDTH), 1.0),
        "ssm_w_glu": nrm(ks[11], (DEPTH, SSM_WIDTH, SSM_WIDTH), SSM_WIDTH ** -0.5),
        "mlstm_i_bias": nrm(ks[12], (DEPTH, MLSTM_HEADS), 0.1),
        "mlstm_f_bias": jnp.linspace(3.0, 6.0, MLSTM_HEADS, dtype=f32)[None, :] + nrm(ks[13], (DEPTH, MLSTM_HEADS), 0.1),
        "mlstm_head_gain": 1.0 + 0.02 * jax.random.normal(ks[14], (DEPTH, MLSTM_WIDTH), f32),
        "rel_bias": nrm(ks[15], (MOBA_HEADS, REL_BUCKETS), 0.5),
        "w_ssm_proj": nrm(ks[16], (DEPTH, SSM_WIDTH, D_MODEL), SSM_WIDTH ** -0.5),
        "w_mlstm_proj": nrm(ks[17], (DEPTH, MLSTM_WIDTH, D_MODEL), MLSTM_WIDTH ** -0.5),
        "w_moba_proj": nrm(ks[18], (DEPTH, MOBA_WIDTH, D_MODEL), MOBA_WIDTH ** -0.5),
        "w_out": nrm(ks[19], (DEPTH, D_MODEL, D_MODEL), D_MODEL ** -0.5),
        "w_ff1": nrm(ks[20], (DEPTH, D_MODEL, D_FF), D_MODEL ** -0.5),
        "w_ff2": nrm(ks[21], (DEPTH, D_FF, D_MODEL), D_FF ** -0.5),
        "norm_mix_pre": gain(ks[22]),
        "norm_mix_post": gain(ks[23]),
        "norm_ffn_pre": gain(ks[24]),
        "norm_ffn_post": gain(ks[25]),
    }


def reference(x, w_in, conv_w, ssm_a_re, ssm_a_im, ssm_log_dt, ssm_b_re, ssm_b_im, ssm_c_re, ssm_c_im,
              ssm_d, ssm_w_glu, mlstm_i_bias, mlstm_f_bias, mlstm_head_gain, rel_bias,
              w_ssm_proj, w_mlstm_proj, w_moba_proj, w_out, w_ff1, w_ff2,
              norm_mix_pre, norm_mix_post, norm_ffn_pre, norm_ffn_post):
    bsz, s, _ = x.shape
    for l in range(DEPTH):
        h = rms_norm(x, norm_mix_pre[l])
        proj = jnp.einsum('bsd,dc->bsc', h, w_in[l])
        (u_ssm, m_q, m_k, m_v, m_o, m_i, m_f, a_q, a_k, a_v, gate_pre) = split_cols(proj, IN_SIZES)
        qk = jax.nn.silu(causal_conv(jnp.concatenate([m_q, m_k], axis=-1), conv_w[l]))
        m_q, m_k = qk[..., :MLSTM_WIDTH], qk[..., MLSTM_WIDTH:]
        y_ssm = ssm_branch(u_ssm, ssm_a_re[l], ssm_a_im[l], ssm_log_dt[l], ssm_b_re[l], ssm_b_im[l],
                           ssm_c_re[l], ssm_c_im[l], ssm_d[l], ssm_w_glu[l])
        y_mlstm = mlstm_branch(m_q, m_k, m_v, m_o, m_i, m_f, mlstm_i_bias[l], mlstm_f_bias[l], mlstm_head_gain[l])
        y_moba = moba_branch(a_q, a_k, a_v, rel_bias)
        g = jax.nn.sigmoid(gate_pre.astype(jnp.float32)).astype(x.dtype).reshape(bsz, s, N_BRANCHES, D_MODEL)
        merged = (g[:, :, 0] * (y_ssm @ w_ssm_proj[l])
                  + g[:, :, 1] * (y_mlstm @ w_mlstm_proj[l])
                  + g[:, :, 2] * (y_moba @ w_moba_proj[l]))
        x = x + rms_norm(merged @ w_out[l], norm_mix_post[l])
        h = rms_norm(x, norm_ffn_pre[l])
        f = jnp.square(jax.nn.relu(h @ w_ff1[l])) @ w_ff2[l]
        x = x + rms_norm(f, norm_ffn_post[l])
    return x
```

```python
import math
from contextlib import ExitStack
import numpy as np
import concourse.bass as bass
import concourse.mybir as mybir
from concourse.bass_utils import run_bass_kernel_spmd

F32 = mybir.dt.float32
BF16 = mybir.dt.bfloat16
AF = mybir.ActivationFunctionType
ALU = mybir.AluOpType
AX = mybir.AxisListType

D_MODEL = 1024
SEQ = 2048
DEPTH = 4
NT = SEQ // 128
D_FF = 4096
IN_COLS = 7176
RMS_EPS = 1e-6
SSM_L = 16
NCH = SEQ // SSM_L

C_U = 0
C_MQ, C_MK, C_MV, C_MO = 512, 1024, 1536, 2048
C_MI, C_MF = 2560, 2564
C_AQ, C_AK, C_AV = 2568, 3080, 3592
C_G = 4104


class Ev:
    __slots__ = ("key", "val", "snap")

    def __init__(self, key, val, snap):
        self.key, self.val, self.snap = key, val, snap


class Tok:
    __slots__ = ("w", "r", "name")

    def __init__(self, name=""):
        self.w = None
        self.r = {}
        self.name = name


class Eng:
    def __init__(self, ctx, key, eng, is_pe=False):
        self.ctx, self.key, self.eng, self.is_pe = ctx, key, eng, is_pe
        self.sem = ctx.nc.semaphore("sem_" + key).__enter__()
        self.count = 0
        self.seen = {}
        self.pending = False

    def need(self, ev):
        if ev is None:
            return
        if ev.key == self.key:
            if self.is_pe:
                return
            if ev.val < self.count - 1:
                return
            if self.seen.get(self.key, 0) >= ev.val:
                return
            self.eng.wait_ge(self.sem, ev.val)
            self.seen[self.key] = ev.val
            return
        if self.seen.get(ev.key, 0) >= ev.val:
            return
        self.eng.wait_ge(self.ctx.sems[ev.key], ev.val)
        s = self.seen
        for k, v in ev.snap.items():
            if s.get(k, 0) < v:
                s[k] = v
        s[ev.key] = max(s.get(ev.key, 0), ev.val)


class Ctx:
    def __init__(self, nc, n_dma_sems=24):
        self.nc = nc
        self.sems = {}
        self.E = {}
        for key, eng, is_pe in (("pe", nc.tensor, True), ("dve", nc.vector, False), ("act", nc.scalar, False),
                                ("pool", nc.gpsimd, False), ("sp", nc.sync, False)):
            e = Eng(self, key, eng, is_pe)
            self.E[key] = e
            self.sems[key] = e.sem
        self.dpool = {}
        for q in ("sp", "pool", "act"):
            lst = []
            for i in range(n_dma_sems if q != "act" else 8):
                k = "d_%s_%d" % (q, i)
                s = nc.semaphore(k).__enter__()
                self.sems[k] = s
                lst.append([k, s, 0])
            self.dpool[q] = [lst, 0]

    def op(self, engname, fn, reads=(), writes=(), signal=True):
        E = self.E[engname]
        for t in reads:
            E.need(t.w)
        for t in writes:
            E.need(t.w)
            for ev in t.r.values():
                E.need(ev)
        ins = fn(E.eng)
        if signal:
            E.count += 1
            ins.then_inc(E.sem, 1)
            val = E.count
        else:
            val = E.count + 1
        snap = dict(E.seen)
        ev = Ev(E.key, val, snap)
        for t in reads:
            t.r[ev.key] = ev
        for t in writes:
            t.w = ev
            t.r = {}
        return ev

    def dma(self, q, out, in_, reads=(), writes=()):
        E = self.E[q]
        for t in reads:
            E.need(t.w)
        for t in writes:
            E.need(t.w)
            for ev in t.r.values():
                E.need(ev)
        lst, idx = self.dpool[q]
        ent = lst[idx % len(lst)]
        self.dpool[q][1] = idx + 1
        if ent[2] > 0:
            E.need(Ev(ent[0], ent[2], {}))
        ent[2] += 16
        E.eng.dma_start(out=out, in_=in_).then_inc(ent[1], 16)
        ev = Ev(ent[0], ent[2], dict(E.seen))
        for t in reads:
            t.r[ev.key] = ev
        for t in writes:
            t.w = ev
            t.r = {}
        return ev

    def barrier(self):
        evs = []
        for k, e in self.E.items():
            if e.count > 0:
                evs.append(Ev(k, e.count, {}))
        for q, (lst, _) in self.dpool.items():
            for ent in lst:
                if ent[2] > 0:
                    evs.append(Ev(ent[0], ent[2], {}))
        for k, e in self.E.items():
            for ev in evs:
                if ev.key != k:
                    e.need(ev)
                else:
                    if not e.is_pe and e.seen.get(k, 0) < ev.val:
                        e.eng.wait_ge(e.sem, ev.val)
                        e.seen[k] = ev.val


class Builder:
    def __init__(self, depth=DEPTH, debug=None, phases=None):
        self.depth = depth
        self.debug = debug or {}
        self.phases = phases
        nc = bass.Bass("TRN2", target_bir_lowering=False)
        self.nc = nc
        self.c = Ctx(nc)
        self.din = {}
        self.dbg_out = {}

    def inp(self, name, shape, dt=F32):
        t = self.nc.dram_tensor(name, list(shape), dt, kind="ExternalInput").ap()
        self.din[name] = t
        return t

    def sb(self, name, shape, dt):
        self.uid = getattr(self, "uid", 0) + 1
        return self.nc.sbuf_tensor("%s_u%d" % (name, self.uid), list(shape), dt)

    def ps(self, name, shape, dt=F32):
        self.uid = getattr(self, "uid", 0) + 1
        return self.nc.psum_tensor("%s_u%d" % (name, self.uid), list(shape), dt)

    def dbg_dump(self, name, src_ap, shape, reads, dt=F32):
        t = self.nc.dram_tensor("dbg_" + name, list(shape), dt, kind="ExternalOutput").ap()
        self.dbg_out[name] = t
        ev = self.c.dma("sp", t, src_ap, reads=reads)
        self.c.E["sp"].need(ev)
        return t

    def rstd_from_ss(self, ss2, rstd, tok_ss, tok_rstd, ncols=2):
        c = self.c
        if ncols == 2:
            c.op("dve", lambda e: e.tensor_tensor(out=rstd, in0=ss2[:, 0:1], in1=ss2[:, 1:2], op=ALU.add),
                 reads=[tok_ss], writes=[tok_rstd])
            src = rstd
        else:
            src = ss2[:, 0:1]
        c.op("act", lambda e: e.activation(out=rstd, in_=src, func=AF.Sqrt, scale=1.0 / D_MODEL, bias=self.eps_ap),
             reads=[tok_rstd, tok_ss], writes=[tok_rstd])
        c.op("dve", lambda e: e.reciprocal(out=rstd, in_=rstd), reads=[tok_rstd], writes=[tok_rstd])

    def declare_inputs(self):
        L = DEPTH
        inp = self.inp
        self.x_in = inp("x", [SEQ, D_MODEL])
        self.w_in = inp("w_in", [L, D_MODEL, IN_COLS])
        self.conv_w = inp("conv_w", [L, 4, 1024])
        self.w_glu = inp("ssm_w_glu", [L, 512, 512])
        self.w_sp = inp("w_ssm_proj", [L, 512, D_MODEL])
        self.w_mp = inp("w_mlstm_proj", [L, 512, D_MODEL])
        self.w_ap = inp("w_moba_proj", [L, 512, D_MODEL])
        self.w_out = inp("w_out", [L, D_MODEL, D_MODEL])
        self.w_ff1 = inp("w_ff1", [L, D_MODEL, D_FF])
        self.w_ff2 = inp("w_ff2", [L, D_FF, D_MODEL])
        self.n_mix_pre = inp("norm_mix_pre", [L, D_MODEL])
        self.n_mix_post = inp("norm_mix_post", [L, D_MODEL])
        self.n_ffn_pre = inp("norm_ffn_pre", [L, D_MODEL])
        self.n_ffn_post = inp("norm_ffn_post", [L, D_MODEL])
        self.ident_in = inp("ident", [128, 128])
        self.out = self.nc.dram_tensor("out", [SEQ, D_MODEL], F32, kind="ExternalOutput").ap()
        self.xres = self.nc.dram_tensor("xres", [SEQ, D_MODEL], F32).ap()
        self.xres2 = self.nc.dram_tensor("xres2", [SEQ, D_MODEL], F32).ap()

    def setup_consts(self):
        c = self.c
        nc = self.nc
        self.ident_f = self.sb("ident_f", [128, 128], F32).__enter__()
        self.ident_b = self.sb("ident_b", [128, 128], BF16).__enter__()
        self.eps_t = self.sb("eps_t", [128, 1], F32).__enter__()
        self.eps_ap = self.eps_t[:, 0:1]
        self.t_const = Tok("const")
        c.dma("sp", self.ident_f[:], self.ident_in[:, :], writes=[self.t_const])
        c.op("dve", lambda e: e.tensor_copy(out=self.ident_b[:], in_=self.ident_f[:]), reads=[self.t_const],
             writes=[self.t_const])
        c.op("dve", lambda e: e.memset(self.eps_t[:], RMS_EPS), writes=[self.t_const])
        self.hT = self.sb("hT", [128, 8, SEQ], BF16).__enter__()
        self.t_hT = [Tok("hT%d" % i) for i in range(NT)]

    def load_gain(self, dst, src_row, tok):
        self.c.dma("sp", dst, src_row.partition_broadcast(128), writes=[tok])

    def norm_to_hT(self, xt, t_x, gain, t_gain, tt, work):
        self.norm_part1(xt, t_x, gain, t_gain, tt, work)
        self.norm_part2(tt, work)

    def norm_part1(self, xt, t_x, gain, t_gain, tt, work):
        c = self.c
        junk, t_junk, ss, t_ss, rstd, t_rstd, hb, t_hb, pst, t_pst = work
        c.op("act", lambda e: e.activation(out=junk, in_=xt, func=AF.Square, accum_out=ss[:, 0:1]),
             reads=[t_x], writes=[t_junk, t_ss])
        self.rstd_from_ss(ss, rstd, t_ss, t_rstd, ncols=1)
        c.op("dve", lambda e: e.scalar_tensor_tensor(out=hb, in0=xt, scalar=rstd, in1=gain, op0=ALU.mult,
                                                     op1=ALU.mult),
             reads=[t_x, t_rstd, t_gain], writes=[t_hb])

    def norm_part2(self, tt, work):
        c = self.c
        junk, t_junk, ss, t_ss, rstd, t_rstd, hb, t_hb, pst, t_pst = work
        for k in range(8):
            c.op("pe", lambda e, k=k: e.transpose(pst[:, k * 128:(k + 1) * 128], hb[:, k * 128:(k + 1) * 128],
                                                  self.ident_b[:]),
                 reads=[t_hb, self.t_const], writes=[t_pst], signal=(k == 7))
        c.op("act", lambda e: e.copy(out=self.hT[:, :, tt * 128:(tt + 1) * 128],
                                     in_=pst.rearrange("p (k t) -> p k t", k=8)),
             reads=[], writes=[self.t_hT[tt], t_pst])

    def phase_A(self, l, src):
        c = self.c
        with ExitStack() as es:
            S = lambda n, sh, dt: es.enter_context(self.sb(n, sh, dt))
            P = lambda n, sh, dt=F32: es.enter_context(self.ps(n, sh, dt))
            xb = S("A_x", [128, 2, 1024], F32)
            g = S("A_g", [128, 1024], F32)
            junk = S("A_junk", [128, 1024], BF16)
            ss = S("A_ss", [128, 2, 2], F32)
            rstd = S("A_rstd", [128, 2, 1], F32)
            hb = S("A_hb", [128, 2, 1024], BF16)
            pst0 = P("A_pst0", [128, 1024], BF16)
            pst1 = P("A_pst1", [128, 1024], BF16)
            t_g = Tok()
            self.load_gain(g[:], self.n_mix_pre[l], t_g)
            t_x = [Tok(), Tok()]
            t_junk = Tok()
            t_ss = [Tok(), Tok()]
            t_rstd = [Tok(), Tok()]
            t_hb = [Tok(), Tok()]
            t_pst = [Tok(), Tok()]
            psts = [pst0, pst1]
            prev = None
            for tt in range(NT):
                b = tt % 2
                c.dma("sp", xb[:, b, :], src[tt * 128:(tt + 1) * 128, :], writes=[t_x[b]])
                work = (junk[:], t_junk, ss[:, b, :], t_ss[b], rstd[:, b, :], t_rstd[b], hb[:, b, :], t_hb[b],
                        psts[b][:], t_pst[b])
                self.norm_part1(xb[:, b, :], t_x[b], g[:], t_g, tt, work)
                if prev is not None:
                    self.norm_part2(*prev)
                prev = (tt, work)
            self.norm_part2(*prev)
        c.barrier()

    def phase_E(self, l, src, dst, next_gain=None):
        c = self.c
        TB = 256
        with ExitStack() as es:
            S = lambda n, sh, dt: es.enter_context(self.sb(n, sh, dt))
            P = lambda n, sh, dt=F32: es.enter_context(self.ps(n, sh, dt))
            f1 = S("E_f1", [128, 32, TB], BF16)
            rbuf = S("E_r", [128, 4, TB], F32)
            xb = S("E_x", [128, 2, 1024], F32)
            yb = S("E_y", [128, 1024], F32)
            g = S("E_g", [128, 1024], F32)
            junk = S("E_junk", [128, 1024], BF16)
            g2 = S("E_g2", [128, 1024], F32)
            hb = S("E_hb", [128, 1024], BF16)
            ss2 = S("E_ss2", [128, 2, 2], F32)
            rstd2 = S("E_rstd2", [128, 2, 1], F32)
            ss = S("E_ss", [128, 2, 2], F32)
            rstd = S("E_rstd", [128, 2, 1], F32)
            p0, p1, p2 = [P("E_p%d" % i, [128, 512]) for i in range(3)]
            pstE = P("E_pst", [128, 1024], BF16)
            t_pstE = Tok()
            t_g2, t_hbE = Tok(), Tok()
            t_ss2 = [Tok(), Tok()]
            t_rstd2 = [Tok(), Tok()]
            prevE = None
            if next_gain is not None:
                self.load_gain(g2[:], next_gain, t_g2)
            q0, q1, q2, q3 = [P("E_q%d" % i, [128, 512]) for i in range(4)]
            t_g = Tok()
            self.load_gain(g[:], self.n_ffn_post[l], t_g)
            if getattr(self, "w1", None) is None:
                self.ffn_prefetch(l, first=True)
            self.ffn_prefetch(l, first=False)
            w1, t_w1, t_w2 = self.w1, self.t_w1, self.t_w2
            w2a, w2b = self.w2a, self.w2b
            pb = [p0, p1, p2]
            t_pb = [Tok() for _ in range(3)]
            qb = [[q0, q1], [q2, q3]]
            t_qb = [[Tok(), Tok()], [Tok(), Tok()]]
            t_f1 = [Tok() for _ in range(32)]
            t_r = [Tok() for _ in range(3)]
            t_x = [Tok(), Tok()]
            t_y = Tok()
            t_junk = Tok()
            t_ss = [Tok(), Tok()]
            t_rstd = [Tok(), Tok()]
            for tb in range(SEQ // TB):
                tsl = slice(tb * TB, (tb + 1) * TB)
                for j in range(32):
                    pi = j % 3
                    for k in range(8):
                        c.op("pe", lambda e, k=k, j=j, pi=pi: e.matmul(pb[pi][:, 0:TB], w1[:, k, j * 128:(j + 1) * 128],
                                                                       self.hT[:, k, tsl], start=(k == 0), stop=(k == 7)),
                             reads=[t_w1[j // 4]] + self.t_hT[tb * 2:tb * 2 + 2], writes=[t_pb[pi]], signal=(k == 7))
                    c.op("act", lambda e, pi=pi: e.activation(out=rbuf[:, pi, :], in_=pb[pi][:, 0:TB], func=AF.Relu),
                         reads=[t_pb[pi]], writes=[t_r[pi]])
                    c.op("dve", lambda e, pi=pi, j=j: e.tensor_tensor(out=f1[:, j, :], in0=rbuf[:, pi, :],
                                                                      in1=rbuf[:, pi, :], op=ALU.mult),
                         reads=[t_r[pi]], writes=[t_f1[j]])
                for ti in range(TB // 128):
                    tt = tb * (TB // 128) + ti
                    b = tt % 2
                    c.dma("sp", xb[:, b, :], src[tt * 128:(tt + 1) * 128, :], writes=[t_x[b]])
                    for hf in range(2):
                        for j in range(32):
                            c.op("pe", lambda e, j=j, hf=hf, b=b, ti=ti: e.matmul(
                                qb[b][hf][:, :], f1[:, j, ti * 128:(ti + 1) * 128],
                                (w2a if j < 16 else w2b)[:, j % 16, hf * 512:(hf + 1) * 512],
                                start=(j == 0), stop=(j == 31)),
                                 reads=[t_f1[j], t_w2[j // 4]], writes=[t_qb[b][hf]], signal=(j == 31))
                        c.op("act", lambda e, hf=hf, b=b: e.activation(out=junk[:, 0:512], in_=qb[b][hf][:, :], func=AF.Square,
                                                                       accum_out=ss[:, b, hf:hf + 1]),
                             reads=[t_qb[b][hf]], writes=[t_junk, t_ss[b]])
                    if prevE is not None:
                        self.norm_part2(*prevE)
                        prevE = None
                    self.rstd_from_ss(ss[:, b, :], rstd[:, b, :], t_ss[b], t_rstd[b], ncols=2)
                    for hf in range(2):
                        c.op("dve", lambda e, hf=hf, b=b: e.scalar_tensor_tensor(
                            out=yb[:, hf * 512:(hf + 1) * 512], in0=qb[b][hf][:, :], scalar=rstd[:, b, :],
                            in1=g[:, hf * 512:(hf + 1) * 512], op0=ALU.mult, op1=ALU.mult),
                             reads=[t_qb[b][hf], t_rstd[b], t_g], writes=[t_y])
                    c.op("dve", lambda e, b=b: e.tensor_tensor(out=xb[:, b, :], in0=xb[:, b, :], in1=yb[:],
                                                               op=ALU.add),
                         reads=[t_y, t_x[b]], writes=[t_x[b]])
                    c.dma("sp", dst[tt * 128:(tt + 1) * 128, :], xb[:, b, :], reads=[t_x[b]])
                    if next_gain is not None:
                        work = (junk[:], t_junk, ss2[:, b, :], t_ss2[b], rstd2[:, b, :], t_rstd2[b], hb[:], t_hbE,
                                pstE[:], t_pstE)
                        self.norm_part1(xb[:, b, :], t_x[b], g2[:], t_g2, tt, work)
                        prevE = (tt, work)
            if prevE is not None:
                self.norm_part2(*prevE)
        c.barrier()
        self.w2b_cm.__exit__(None, None, None)
        self.w2a_cm.__exit__(None, None, None)
        self.w1_cm.__exit__(None, None, None)
        self.w1 = None

    def finish(self):
        c = self.c
        c.barrier()

    def build_test_trunk(self):
        self.declare_inputs()
        self.setup_consts()
        self.phase_A(0, self.x_in)
        self.phase_E(0, self.x_in, self.out)
        self.finish()
        return self.nc

    def load_w(self, dst, src2d, tok, q="pool"):
        self.c.dma(q, dst, src2d.rearrange("(kt p) c -> p kt c", p=128), writes=[tok])

    def proj_fm(self, w, t_w, ncol_tiles, pbanks, t_pbanks, evac, nk=8, rhs=None, t_rhs=None):
        c = self.c
        rhs = self.hT if rhs is None else rhs
        cnt = 0
        for m in range(ncol_tiles):
            for nb in range(4):
                pi = cnt % len(pbanks)
                cnt += 1
                rt = self.t_hT[nb * 4:nb * 4 + 4] if t_rhs is None else t_rhs
                for k in range(nk):
                    c.op("pe", lambda e, k=k, m=m, nb=nb, pi=pi: e.matmul(
                        pbanks[pi][:, :], w[:, k, m * 128:(m + 1) * 128], rhs[:, k, nb * 512:(nb + 1) * 512],
                        start=(k == 0), stop=(k == nk - 1)),
                         reads=[t_w] + rt, writes=[t_pbanks[pi]], signal=(k == nk - 1))
                evac(m, nb, pbanks[pi], t_pbanks[pi])

    def proj_tm(self, w, t_w, col0, ncols, pbanks, t_pbanks, evac, nk=8, lhs=None, t_lhs=None):
        c = self.c
        lhs = self.hT if lhs is None else lhs
        for tt in range(NT):
            pi = tt % len(pbanks)
            lt = [self.t_hT[tt]] if t_lhs is None else t_lhs
            for k in range(nk):
                c.op("pe", lambda e, k=k, tt=tt, pi=pi: e.matmul(
                    pbanks[pi][:, 0:ncols], lhs[:, k, tt * 128:(tt + 1) * 128], w[:, k, col0:col0 + ncols],
                    start=(k == 0), stop=(k == nk - 1)),
                     reads=[t_w] + lt, writes=[t_pbanks[pi]], signal=(k == nk - 1))
            evac(tt, pbanks[pi], t_pbanks[pi])

    def setup_moba_consts(self):
        c = self.c
        nc = self.nc
        self.ohr_in = self.inp("ohr", [32, 384])
        self.causal_in = self.inp("causal_neg", [128, 128])
        self.rbT_in = self.inp("rel_biasT", [32, 8])
        self.T0m_d = nc.dram_tensor("T0m_d", [128, 8, 128], F32).ap()
        self.T1_d = nc.dram_tensor("T1_d", [128, 8, 128], F32).ap()
        self.c31 = self.sb("c31", [128, 8], F32).__enter__()
        t = self.t_const
        with ExitStack() as es:
            ohr = es.enter_context(self.sb("ohr_sb", [128, 384], F32))
            rbT = es.enter_context(self.sb("rbT_sb", [32, 8], F32))
            T0m = es.enter_context(self.sb("T0m_s", [128, 8, 128], F32))
            T1 = es.enter_context(self.sb("T1_s", [128, 8, 128], F32))
            caus = es.enter_context(self.sb("caus_s", [128, 128], F32))
            t1, t2 = Tok(), Tok()
            rbrep = es.enter_context(self.sb("rbrep_sb", [128, 8, 128], F32))
            vb = es.enter_context(self.sb("vb_sb", [128, 2, 384], F32))
            pp2 = es.enter_context(self.ps("bv_ps2", [128, 384], F32))
            ZS = 128 * 385 + 512
            self.zscr = nc.dram_tensor("zscr", [8, ZS], F32)
            c.op("dve", lambda e: e.memset(ohr[:], 0.0), writes=[t1])
            c.op("dve", lambda e: e.memset(rbrep[:], 0.0), writes=[t1])
            c.dma("sp", ohr[0:32, :], self.ohr_in[:, :], writes=[t1])
            c.dma("sp", rbT[:], self.rbT_in[:, :], writes=[t1])
            c.dma("sp", caus[:], self.causal_in[:, :], writes=[t])
            c.dma("sp", self.c31[:], self.rbT_in[31, :].partition_broadcast(128), writes=[t])
            c.op("dve", lambda e: e.tensor_copy(out=rbrep[0:32, :, :], in_=rbT[:, :].unsqueeze(2).broadcast_to([32, 8, 128])),
                 reads=[t1], writes=[t1])
            t_vb = [Tok(), Tok()]
            t3 = Tok()
            for h in range(8):
                c.op("pe", lambda e, h=h: e.matmul(pp2[:, :], rbrep[:, h, :], ohr[:, :], start=True, stop=True),
                     reads=[t1], writes=[t2])
                c.op("dve", lambda e, h=h: e.tensor_copy(out=vb[:, h % 2, :], in_=pp2[:, :]), reads=[t2],
                     writes=[t_vb[h % 2]])
                c.dma("sp", bass.AP(self.zscr, h * ZS, [[385, 128], [1, 384]]), vb[:, h % 2, :], reads=[t_vb[h % 2]],
                      writes=[t3])
            for h in range(8):
                src0 = bass.AP(self.zscr, h * ZS + 255, [[384, 128], [1, 128]])
                src1 = bass.AP(self.zscr, h * ZS + 127, [[384, 128], [1, 128]])
                c.dma("sp", T0m[:, h, :], src0, reads=[t3], writes=[t])
                c.dma("sp", T1[:, h, :], src1, reads=[t3], writes=[t])
            for h in range(8):
                c.op("dve", lambda e, h=h: e.tensor_tensor(out=T0m[:, h, :], in0=T0m[:, h, :],
                                                           in1=caus[:], op=ALU.add), reads=[t], writes=[t])
            c.dma("sp", self.T0m_d, T0m[:], reads=[t])
            c.dma("sp", self.T1_d, T1[:], reads=[t])
            c.barrier()

    def phase_moba(self, l, yT, t_yT):
        c = self.c
        NEG = -1e30
        with ExitStack() as es:
            S = lambda n, sh, dt: es.enter_context(self.sb(n, sh, dt))
            P = lambda n, sh, dt=F32: es.enter_context(self.ps(n, sh, dt))
            wq = S("M_wq", [128, 8, 512], BF16)
            wk = S("M_wk", [128, 8, 512], BF16)
            wv = S("M_wv", [128, 8, 512], BF16)
            aqT = S("M_aqT", [128, 4, SEQ], BF16)
            akT = S("M_akT", [128, 4, SEQ], BF16)
            av = S("M_av", [128, NT, 512], BF16)
            T0m = S("M_T0m", [128, 8, 128], F32)
            T1 = S("M_T1", [128, 8, 128], F32)
            kms = S("M_kms", [128, 4, 8], F32)
            kmBD = S("M_kmBD", [128, 4, 16], BF16)
            gpad = S("M_gpad", [128, 8, 8], F32)
            m8 = S("M_m8", [128, 8, 8], F32)
            selm = S("M_selm", [128, 8, 8, 8], F32)
            selb = S("M_selb", [128, 8, 8, 8], F32)
            lg = S("M_lg", [128, 2, SEQ], F32)
            pr = S("M_pr", [128, 3, SEQ], BF16)
            pT = S("M_pT", [128, 3, NT, 128], BF16)
            mx = S("M_mx", [128, 2, 2], F32)
            pmx = S("M_pmx", [128, 2, 16], F32)
            rs = S("M_rs", [128, 2, 8], F32)
            ytok = S("M_ytok", [128, 2, 512], BF16)
            sc = [P("M_sc%d" % i, [128, 512]) for i in range(4)]
            ptp = [P("M_ptp%d" % i, [128, 1024], BF16) for i in range(2)]
            pv = [P("M_pv%d" % i, [128, 512]) for i in range(2)]
            t_sc = [Tok() for _ in range(4)]
            t_ptp = [Tok() for _ in range(2)]
            t_pv = [Tok() for _ in range(2)]
            t_w = [Tok(), Tok(), Tok()]
            t_bias = Tok()
            self.load_w(wq[:], self.w_in[l][:, C_AQ:C_AQ + 512], t_w[0])
            self.load_w(wk[:], self.w_in[l][:, C_AK:C_AK + 512], t_w[1])
            self.load_w(wv[:], self.w_in[l][:, C_AV:C_AV + 512], t_w[2])
            c.dma("sp", T0m[:], self.T0m_d, writes=[t_bias])
            c.dma("sp", T1[:], self.T1_d, writes=[t_bias])
            t_aq = [Tok() for _ in range(4)]
            t_ak = [Tok() for _ in range(4)]
            t_av = [Tok() for _ in range(NT)]
            t_kms = Tok()

            def ev_q(m, nb, ps, tps):
                c.op("act", lambda e: e.mul(out=aqT[:, m, nb * 512:(nb + 1) * 512], in_=ps[:, :], mul=0.125),
                     reads=[tps], writes=[t_aq[nb]])

            def ev_k(m, nb, ps, tps):
                c.op("act", lambda e: e.copy(out=akT[:, m, nb * 512:(nb + 1) * 512], in_=ps[:, :]),
                     reads=[], writes=[t_ak[nb], tps])
                if self.debug.get("noreduce"):
                    return
                c.op("dve", lambda e: e.tensor_reduce(out=kms[:, m, nb * 2:nb * 2 + 2],
                                                      in_=ps[:, :].rearrange("p (b s) -> p b s", b=2),
                                                      op=ALU.add, axis=AX.X), reads=[], writes=[t_kms, tps])

            def ev_v(tt, ps, tps):
                c.op("dve", lambda e: e.tensor_copy(out=av[:, tt, :], in_=ps[:, 0:512]), reads=[tps], writes=[t_av[tt]])

            if not self.debug.get("noq"):
                self.proj_fm(wq, t_w[0], 4, sc, t_sc, ev_q)
            if not self.debug.get("nok"):
                self.proj_fm(wk, t_w[1], 4, sc, t_sc, ev_k)
            if not self.debug.get("nov"):
                self.proj_tm(wv, t_w[2], 0, 512, sc, t_sc, ev_v)
            if self.debug.get("moba_stop") == "proj":
                c.barrier()
                return
            t_km = Tok()
            c.op("pool", lambda e: e.memset(kmBD[:], 0.0), writes=[t_km])
            c.op("dve", lambda e: e.tensor_scalar(out=kmBD[0:64, :, 0:8], in0=kms[0:64, :, :], scalar1=1.0 / 256,
                                                  scalar2=None, op0=ALU.mult), reads=[t_kms], writes=[t_km])
            c.op("dve", lambda e: e.tensor_scalar(out=kmBD[64:128, :, 8:16], in0=kms[64:128, :, :], scalar1=1.0 / 256,
                                                  scalar2=None, op0=ALU.mult), reads=[t_kms], writes=[t_km])
            t_sel = Tok()
            t_gp = Tok()
            t_m8 = Tok()
            for i in range(8, NT):
                cur = i // 2
                gps = sc[i % 4]
                tg = t_sc[i % 4]
                for ct in range(4):
                    c.op("pe", lambda e, ct=ct, i=i, gps=gps: e.matmul(
                        gps[:, ct * 16:(ct + 1) * 16], aqT[:, ct, i * 128:(i + 1) * 128], kmBD[:, ct, :],
                        start=True, stop=True), reads=[t_aq[i // 4], t_km], writes=[tg], signal=(ct == 3))
                c.op("pool", lambda e: e.memset(gpad[:], NEG), writes=[t_gp])
                c.op("dve", lambda e, gps=gps, cur=cur: e.tensor_copy(
                    out=gpad[:, :, 0:cur], in_=gps[:, 0:64].rearrange("p (h n) -> p h n", h=8)[:, :, 0:cur]),
                     reads=[tg], writes=[t_gp])
                for h in range(8):
                    c.op("dve", lambda e, h=h: e.max(out=m8[:, h, :], in_=gpad[:, h, :]), reads=[t_gp], writes=[t_m8])
                c.op("dve", lambda e, i=i: e.tensor_tensor(out=selm[:, i - 8, :, :], in0=gpad[:],
                                                           in1=m8[:, :, 2:3].broadcast_to([128, 8, 8]), op=ALU.is_ge),
                     reads=[t_gp, t_m8], writes=[t_sel])
                c.op("dve", lambda e, i=i: e.tensor_scalar(out=selm[:, i - 8, :, :], in0=selm[:, i - 8, :, :],
                                                           scalar1=-NEG, scalar2=NEG, op0=ALU.mult, op1=ALU.add),
                     reads=[t_sel], writes=[t_sel])
                c.op("dve", lambda e, i=i: e.tensor_tensor(out=selb[:, i - 8, :, :], in0=selm[:, i - 8, :, :],
                                                           in1=self.c31[:, :].unsqueeze(2).broadcast_to([128, 8, 8]),
                                                           op=ALU.add), reads=[t_sel, self.t_const], writes=[t_sel])
            if self.debug.get("moba_stop") == "sel":
                c.barrier()
                return
            NB = 3
            t_lg = [Tok(), Tok()]
            t_pr = [Tok() for _ in range(NB)]
            t_pT = [Tok() for _ in range(NB)]
            t_mx = [Tok(), Tok()]
            t_rs = [Tok(), Tok()]
            t_yt = [Tok(), Tok()]
            cnts = {"sc": 0, "pt": 0}

            def stage_A(n, i, h):
                cur = i // 2
                nk = i + 1
                b = n % 2
                b3 = n % NB
                ib = i % 2
                ct, h2 = h // 2, h % 2
                rows = slice(h2 * 64, h2 * 64 + 64)
                nchunk = (nk * 128 + 511) // 512
                nslot = [0]
                for ch in range(nchunk):
                    k0 = ch * 512
                    kw = min(512, nk * 128 - k0)
                    pi = cnts["sc"] % 4
                    cnts["sc"] += 1
                    c.op("pe", lambda e, pi=pi, k0=k0, kw=kw: e.matmul(
                        sc[pi][:, 0:kw], aqT[rows, ct, i * 128:(i + 1) * 128], akT[rows, ct, k0:k0 + kw],
                        start=True, stop=True),
                         reads=[t_aq[i // 4], t_ak[ch]], writes=[t_sc[pi]])
                    kt0 = k0 // 128
                    j = kt0
                    while j < kt0 + kw // 128:
                        off = (j - kt0) * 128
                        if j == i:
                            c.op("dve", lambda e, pi=pi, off=off, j=j: e.tensor_tensor(
                                out=lg[:, b, j * 128:(j + 1) * 128], in0=sc[pi][:, off:off + 128], in1=T0m[:, h, :],
                                op=ALU.add), reads=[t_bias], writes=[t_lg[b], t_sc[pi]])
                            j += 1
                        elif j == i - 1:
                            if i % 2 == 1 or i < 8:
                                c.op("dve", lambda e, pi=pi, off=off, j=j: e.tensor_tensor(
                                    out=lg[:, b, j * 128:(j + 1) * 128], in0=sc[pi][:, off:off + 128],
                                    in1=T1[:, h, :], op=ALU.add), reads=[t_bias], writes=[t_lg[b], t_sc[pi]])
                            else:
                                c.op("dve", lambda e, pi=pi, off=off, j=j: e.scalar_tensor_tensor(
                                    out=lg[:, b, j * 128:(j + 1) * 128], in0=sc[pi][:, off:off + 128],
                                    scalar=selm[:, i - 8, h, cur - 1:cur], in1=T1[:, h, :], op0=ALU.add,
                                    op1=ALU.add), reads=[t_bias, t_sel], writes=[t_lg[b], t_sc[pi]])
                            j += 1
                        else:
                            n_ = j // 2
                            w = 128
                            if j % 2 == 0 and (j + 1) < kt0 + kw // 128 and (j + 1) < i - 1:
                                w = 256
                            scal = selb[:, i - 8, h, n_:n_ + 1] if i >= 8 else self.c31[:, h:h + 1]
                            slot = nslot[0]
                            nslot[0] += 1
                            c.op("dve", lambda e, pi=pi, off=off, j=j, w=w, scal=scal, slot=slot: e.tensor_scalar(
                                out=lg[:, b, j * 128:j * 128 + w], in0=sc[pi][:, off:off + w], scalar1=scal,
                                scalar2=None, op0=ALU.add, op1=ALU.max, accum_out=pmx[:, b, slot:slot + 1]),
                                 reads=[t_sel, self.t_const], writes=[t_lg[b], t_sc[pi], t_mx[b]])
                            j += w // 128
                L = nk * 128
                s0 = max(0, i - 1) * 128
                slot = nslot[0]
                c.op("dve", lambda e: e.tensor_reduce(out=pmx[:, b, slot:slot + 1], in_=lg[:, b, s0:L], axis=AX.X, op=ALU.max),
                     reads=[t_lg[b]], writes=[t_mx[b]])
                c.op("dve", lambda e: e.tensor_reduce(out=mx[:, b, 1:2], in_=pmx[:, b, 0:slot + 1], axis=AX.X, op=ALU.max,
                                                      negate=True),
                     reads=[], writes=[t_mx[b]])
                c.op("act", lambda e: e.activation(
                    out=pr[:, b3, 0:L], in_=lg[:, b, 0:L], func=AF.Exp, bias=mx[:, b, 1:2], scale=1.0,
                    accum_out=rs[:, ib, h:h + 1]), reads=[t_lg[b], t_mx[b]], writes=[t_pr[b3], t_rs[ib]])

            def stage_B(n, i, h):
                nk = i + 1
                b3 = n % NB
                for g0 in range(0, nk, 8):
                    gn = min(8, nk - g0)
                    pb = cnts["pt"] % 2
                    cnts["pt"] += 1
                    for jj in range(gn):
                        j = g0 + jj
                        c.op("pe", lambda e, pb=pb, jj=jj, j=j: e.transpose(
                            ptp[pb][:, jj * 128:(jj + 1) * 128], pr[:, b3, j * 128:(j + 1) * 128], self.ident_b[:]),
                             reads=[t_pr[b3], self.t_const], writes=[t_ptp[pb]], signal=(jj == gn - 1))
                    c.op("act", lambda e, pb=pb, gn=gn, g0=g0: e.copy(
                        out=pT[:, b3, g0:g0 + gn, :], in_=ptp[pb][:, 0:gn * 128].rearrange("p (j t) -> p j t", j=gn)),
                         reads=[], writes=[t_pT[b3], t_ptp[pb]])

            def stage_C(n, i, h):
                nk = i + 1
                b3 = n % NB
                pvb = pv[i % 2]
                t_pvb = t_pv[i % 2]
                ib = i % 2
                for j in range(nk):
                    c.op("pe", lambda e, j=j: e.matmul(
                        pvb[:, h * 64:(h + 1) * 64], pT[:, b3, j, :], av[:, j, h * 64:(h + 1) * 64],
                        start=(j == 0), stop=(j == nk - 1)),
                         reads=[t_pT[b3], t_av[j]], writes=[t_pvb], signal=(j == nk - 1))
                if h == 7:
                    c.op("dve", lambda e: e.reciprocal(out=rs[:, ib, :], in_=rs[:, ib, :]), reads=[t_rs[ib]],
                         writes=[t_rs[ib]])
                    c.op("dve", lambda e: e.tensor_tensor(
                        out=ytok[:, ib, :].rearrange("p (h d) -> p h d", h=8),
                        in0=pvb[:, :].rearrange("p (h d) -> p h d", h=8),
                        in1=rs[:, ib, :].unsqueeze(2).broadcast_to([128, 8, 64]), op=ALU.mult),
                         reads=[t_rs[ib]], writes=[t_yt[ib], t_pvb])
                    pb = cnts["pt"] % 2
                    cnts["pt"] += 1
                    for ct in range(4):
                        c.op("pe", lambda e, ct=ct, pb=pb: e.transpose(
                            ptp[pb][:, ct * 128:(ct + 1) * 128], ytok[:, ib, ct * 128:(ct + 1) * 128], self.ident_b[:]),
                             reads=[t_yt[ib], self.t_const], writes=[t_ptp[pb]], signal=(ct == 3))
                    c.op("act", lambda e, pb=pb: e.copy(out=yT[:, :, i * 128:(i + 1) * 128],
                                                        in_=ptp[pb][:, 0:512].rearrange("p (c t) -> p c t", c=4)),
                         reads=[], writes=[t_yT[i], t_ptp[pb]])

            its = [(i, h) for i in range(self.debug.get("moba_nq", NT)) for h in range(8)]
            N = len(its)
            for n in range(N + 2):
                if n < N:
                    stage_A(n, *its[n])
                if 1 <= n <= N:
                    stage_B(n - 1, *its[n - 1])
                if n >= 2:
                    stage_C(n - 2, *its[n - 2])
        c.barrier()

    def build_test_moba(self):
        self.declare_inputs()
        self.setup_consts()
        self.setup_moba_consts()
        self.phase_A(0, self.x_in)
        self.yT = [self.sb("yT%d" % i, [128, 4, SEQ], BF16).__enter__() for i in range(3)]
        self.t_yT = [[Tok() for _ in range(NT)] for _ in range(3)]
        self.phase_moba(0, self.yT[2], self.t_yT[2])
        self.dbg_dump("y_moba", self.yT[2][:], [128, 4, SEQ], self.t_yT[2], BF16)
        self.finish()
        return self.nc


def _t5_bucket_np(dist):
    dist = np.asarray(dist)
    max_exact = 16
    large = max_exact + (np.log(np.maximum(dist, 1).astype(np.float32) / max_exact)
                         / math.log(128 / max_exact) * (32 - max_exact)).astype(np.int32)
    large = np.minimum(large, 31)
    return np.where(dist < max_exact, dist, large)


def host_consts():
    cst = {}
    cst["ident"] = np.eye(128, dtype=np.float32)
    u = np.arange(384)
    dist = 255 - u
    ohr = np.zeros((32, 384), np.float32)
    valid = dist >= 0
    bk = _t5_bucket_np(np.maximum(dist, 0))
    ohr[bk[valid], u[valid]] = 1.0
    cst["ohr"] = ohr
    t = np.arange(128)[:, None]
    s = np.arange(128)[None, :]
    cst["causal_neg"] = np.where(s <= t, 0.0, -1e30).astype(np.float32)
    cst["triu"] = (s >= t).astype(np.float32)
    cst["bdmask"] = (np.arange(128)[:, None] // 32 == np.arange(4)[None, :]).astype(np.float32)
    return cst


def make_in_map(b, inputs, core, ssm=None):
    cst = host_consts()
    if any(n.startswith("ssm_") and n not in inputs for n in b.din):
        cst.update(ssm if ssm is not None else host_ssm_layouts(inputs))
    im = {}
    for name in b.din:
        if name == "x":
            im[name] = np.ascontiguousarray(inputs["x"][core])
        elif name == "rel_biasT":
            im[name] = np.ascontiguousarray(np.asarray(inputs["rel_bias"]).T)
        elif name == "conv_wT":
            im[name] = np.ascontiguousarray(np.asarray(inputs["conv_w"]).transpose(0, 2, 1))
        elif name in cst:
            im[name] = cst[name]
        else:
            im[name] = np.ascontiguousarray(inputs[name])
    return im


def _build_test_mconst(self):
    self.declare_inputs()
    self.setup_consts()
    self.setup_moba_consts()
    self.dbg_dump("T0m", self.T0m_d, [128, 8, 128], [])
    self.dbg_dump("T1", self.T1_d, [128, 8, 128], [])
    self.finish()
    return self.nc


Builder.build_test_mconst = _build_test_mconst


def _phase_D1(self, l):
    c = self.c
    yT, t_yT = self.yT, self.t_yT
    mT = self.mT
    with ExitStack() as es:
        S = lambda n, sh, dt: es.enter_context(self.sb(n, sh, dt))
        P = lambda n, sh, dt=F32: es.enter_context(self.ps(n, sh, dt))
        gw = S("D_gw", [128, 3, 8, 3, 128], BF16)
        pw = S("D_pw", [128, 3, 4, 3, 128], BF16)
        sg = S("D_sg", [128, 3, 512], BF16)
        acc = S("D_acc", [128, 3, 512], F32)
        pbk = [P("D_p%d" % i, [128, 512]) for i in range(6)]
        t_pbk = [Tok() for _ in range(6)]
        t_gw = [Tok(), Tok(), Tok()]
        t_pw = [Tok(), Tok(), Tok()]
        t_sg = [Tok() for _ in range(3)]
        t_acc = [Tok() for _ in range(3)]
        t_mT = [Tok() for _ in range(4)]
        self.t_mT = t_mT
        pws = [self.w_sp, self.w_mp, self.w_ap]
        pcnt = 0
        gen = None
        if getattr(self, "ssm_next", None) is not None:
            es_z = ExitStack()
            zb = [es_z.enter_context(self.ps("Z_p%d" % i, [128, 512])) for i in range(2)]
            gen = self.ssm_setup_gen(self.ssm_next, es_z, zb, [Tok(), Tok()])
        for dt in range(8):
            wb = dt % 3
            for b in range(3):
                c0 = C_G + b * 1024 + dt * 128
                self.load_w(gw[:, wb, :, b, :], self.w_in[l][:, c0:c0 + 128], t_gw[wb])
                self.load_w(pw[:, wb, :, b, :], pws[b][l][:, dt * 128:(dt + 1) * 128], t_pw[wb])
            for nb in range(4):
                tsl = slice(nb * 512, (nb + 1) * 512)
                for b in range(3):
                    pg = pcnt % 6
                    pp = (pcnt + 1) % 6
                    pcnt += 2
                    for k in range(8):
                        c.op("pe", lambda e, k=k, b=b, pg=pg, wb=wb: e.matmul(pbk[pg][:, :], gw[:, wb, k, b, :],
                                                                             self.hT[:, k, tsl], start=(k == 0),
                                                                             stop=(k == 7)),
                             reads=[t_gw[wb]] + self.t_hT[nb * 4:nb * 4 + 4], writes=[t_pbk[pg]], signal=(k == 7))
                    for k in range(4):
                        c.op("pe", lambda e, k=k, b=b, pp=pp, wb=wb: e.matmul(pbk[pp][:, :], pw[:, wb, k, b, :],
                                                                             yT[b][:, k, tsl], start=(k == 0),
                                                                             stop=(k == 3)),
                             reads=[t_pw[wb]] + t_yT[b][nb * 4:nb * 4 + 4], writes=[t_pbk[pp]], signal=(k == 3))
                    c.op("act", lambda e, b=b, pg=pg: e.activation(out=sg[:, b, :], in_=pbk[pg][:, :], func=AF.Sigmoid),
                         reads=[], writes=[t_sg[b], t_pbk[pg]])
                    c.op("dve", lambda e, b=b, pp=pp: e.tensor_tensor(out=acc[:, b, :], in0=pbk[pp][:, :],
                                                                      in1=sg[:, b, :], op=ALU.mult),
                         reads=[t_sg[b]], writes=[t_acc[b], t_pbk[pp]])
                c.op("dve", lambda e: e.tensor_tensor(out=acc[:, 0, :], in0=acc[:, 0, :], in1=acc[:, 1, :], op=ALU.add),
                     reads=[t_acc[1]], writes=[t_acc[0]])
                c.op("dve", lambda e, dt=dt: e.tensor_tensor(out=mT[:, dt, tsl], in0=acc[:, 0, :], in1=acc[:, 2, :],
                                                             op=ALU.add),
                     reads=[t_acc[0], t_acc[2]], writes=[t_mT[nb]])
                if gen is not None:
                    next(gen, None)
        if gen is not None:
            for _ in gen:
                pass
            c.barrier()
            es_z.close()
    c.barrier()


def _phase_D2(self, l, src, dst):
    c = self.c
    mT, t_mT = self.mT, self.t_mT
    with ExitStack() as es:
        S = lambda n, sh, dt: es.enter_context(self.sb(n, sh, dt))
        P = lambda n, sh, dt=F32: es.enter_context(self.ps(n, sh, dt))
        wout = S("D_wout", [128, 8, D_MODEL], BF16)
        xb = S("D_x", [128, 2, 1024], F32)
        yb = S("D_y", [128, 1024], F32)
        g1 = S("D_g1", [128, 1024], F32)
        g2 = S("D_g2", [128, 1024], F32)
        junk = S("D_junk", [128, 1024], BF16)
        ss = S("D_ss", [128, 2, 2], F32)
        rstd = S("D_rstd", [128, 2, 1], F32)
        ss2 = S("D_ss2", [128, 2, 2], F32)
        rstd2 = S("D_rstd2", [128, 2, 1], F32)
        hb = S("D_hb", [128, 2, 1024], BF16)
        pbk = [P("D2_p%d" % i, [128, 512]) for i in range(4)]
        t_pbk = [Tok() for _ in range(4)]
        pst = [P("D_pst%d" % i, [128, 1024], BF16) for i in range(2)]
        t_pst = [Tok(), Tok()]
        t_g1, t_g2, t_wout = Tok(), Tok(), Tok()
        self.load_gain(g1[:], self.n_mix_post[l], t_g1)
        self.load_gain(g2[:], self.n_ffn_pre[l], t_g2)
        self.load_w(wout[:], self.w_out[l], t_wout)
        self.ffn_prefetch(l, first=True)
        t_x = [Tok(), Tok()]
        t_y = Tok()
        t_junk = Tok()
        t_ss = [Tok(), Tok()]
        t_rstd = [Tok(), Tok()]
        t_ss2 = [Tok(), Tok()]
        t_rstd2 = [Tok(), Tok()]
        t_hb = [Tok(), Tok()]
        prevD = None
        for tt in range(NT):
            b = tt % 2
            c.dma("sp", xb[:, b, :], src[tt * 128:(tt + 1) * 128, :], writes=[t_x[b]])
            qq = [pbk[b * 2], pbk[b * 2 + 1]]
            t_qq = [t_pbk[b * 2], t_pbk[b * 2 + 1]]
            for hf in range(2):
                for k in range(8):
                    c.op("pe", lambda e, k=k, hf=hf, tt=tt, qq=qq: e.matmul(
                        qq[hf][:, :], mT[:, k, tt * 128:(tt + 1) * 128], wout[:, k, hf * 512:(hf + 1) * 512],
                        start=(k == 0), stop=(k == 7)),
                         reads=[t_mT[tt // 4], t_wout], writes=[t_qq[hf]], signal=(k == 7))
                c.op("act", lambda e, hf=hf, b=b, qq=qq: e.activation(out=junk[:, 0:512], in_=qq[hf][:, :], func=AF.Square,
                                                                     accum_out=ss[:, b, hf:hf + 1]),
                     reads=[], writes=[t_junk, t_ss[b], t_qq[hf]])
            self.rstd_from_ss(ss[:, b, :], rstd[:, b, :], t_ss[b], t_rstd[b], ncols=2)
            for hf in range(2):
                c.op("dve", lambda e, hf=hf, b=b, qq=qq: e.scalar_tensor_tensor(
                    out=yb[:, hf * 512:(hf + 1) * 512], in0=qq[hf][:, :], scalar=rstd[:, b, :],
                    in1=g1[:, hf * 512:(hf + 1) * 512], op0=ALU.mult, op1=ALU.mult),
                     reads=[t_rstd[b], t_g1], writes=[t_y, t_qq[hf]])
            c.op("dve", lambda e, b=b: e.tensor_tensor(out=xb[:, b, :], in0=xb[:, b, :], in1=yb[:], op=ALU.add),
                 reads=[t_y], writes=[t_x[b]])
            c.dma("sp", dst[tt * 128:(tt + 1) * 128, :], xb[:, b, :], reads=[t_x[b]])
            work = (junk[:], t_junk, ss2[:, b, :], t_ss2[b], rstd2[:, b, :], t_rstd2[b], hb[:, b, :], t_hb[b],
                    pst[b][:], t_pst[b])
            self.norm_part1(xb[:, b, :], t_x[b], g2[:], t_g2, tt, work)
            if prevD is not None:
                self.norm_part2(*prevD)
            prevD = (tt, work)
        self.norm_part2(*prevD)
    c.barrier()


def _ffn_prefetch(self, l, first):
    c = self.c
    w2v = self.w_ff2[l].rearrange("(jt p) f -> p jt f", p=128)
    if first:
        self.w1_cm = self.nc.sbuf_tensor("E_w1_l%d" % l, [128, 8, D_FF], BF16, side="right")
        self.w1 = self.w1_cm.__enter__()
        self.w2a_cm = self.nc.sbuf_tensor("E_w2a_l%d" % l, [128, 16, D_MODEL], BF16, side="right")
        self.w2a = self.w2a_cm.__enter__()
        self.t_w1 = [Tok() for _ in range(8)]
        self.t_w2 = [Tok() for _ in range(8)]
        w1v = self.w_ff1[l].rearrange("(kt p) f -> p kt f", p=128)
        for cb in range(8):
            c.dma("pool", self.w1[:, :, cb * 512:(cb + 1) * 512], w1v[:, :, cb * 512:(cb + 1) * 512],
                  writes=[self.t_w1[cb]])
        for k in range(4):
            c.dma("pool", self.w2a[:, 4 * k:4 * k + 4, :], w2v[:, 4 * k:4 * k + 4, :], writes=[self.t_w2[k]])
    else:
        self.w2b_cm = self.nc.sbuf_tensor("E_w2b_l%d" % l, [128, 16, D_MODEL], BF16, side="right")
        self.w2b = self.w2b_cm.__enter__()
        for k in range(4, 8):
            c.dma("pool", self.w2b[:, 4 * (k - 4):4 * (k - 4) + 4, :], w2v[:, 4 * k:4 * k + 4, :], writes=[self.t_w2[k]])


Builder.ffn_prefetch = _ffn_prefetch
Builder.phase_D1 = _phase_D1
Builder.phase_D2 = _phase_D2


def _phase_D(self, l, src, dst):
    with self.sb("D_mT", [128, 8, SEQ], BF16) as mT:
        self.mT = mT
        self.phase_D1(l)
        self.phase_D2(l, src, dst)


Builder.phase_D = _phase_D


def _build_test_mixD(self):
    c = self.c
    self.declare_inputs()
    self.setup_consts()
    self.setup_moba_consts()
    self.phase_A(0, self.x_in)
    with ExitStack() as es:
        self.yT = [es.enter_context(self.sb("yT%d" % i, [128, 4, SEQ], BF16)) for i in range(3)]
        self.t_yT = [[Tok() for _ in range(NT)] for _ in range(3)]
        tz = Tok()
        for i in range(2):
            c.op("pool", lambda e, i=i: e.memset(self.yT[i][:], 0.0), writes=self.t_yT[i])
        self.phase_moba(0, self.yT[2], self.t_yT[2])
        self.phase_D(0, self.x_in, self.out)
    self.finish()
    return self.nc


Builder.build_test_mixD = _build_test_mixD


def _phase_mlstm(self, l, yT, t_yT):
    c = self.c
    nc = self.nc
    SC = 128.0 ** -0.5
    with ExitStack() as es:
        cur = [es]
        S = lambda n, sh, dt: cur[0].enter_context(self.sb(n, sh, dt))
        P = lambda n, sh, dt=F32: es.enter_context(self.ps(n, sh, dt))
        pk = [P("L_p%d" % i, [128, 512]) for i in range(6)]
        t_pk = [Tok() for _ in range(6)]
        pb16 = [P("L_pb%d" % i, [128, 1024], BF16) for i in range(2)]
        t_pb16 = [Tok(), Tok()]
        qT = S("L_qT", [128, 4, SEQ], BF16)
        kT = S("L_kT", [128, 4, SEQ], BF16)
        vaug = S("L_vaug", [128, NT, 4, 129], BF16)
        so = S("L_so", [128, NT, 512], BF16)
        es1 = ExitStack()
        cur[0] = es1
        wbuf = S("L_w", [128, 2, 8, 512], BF16)
        wif = S("L_wif", [128, 8, 128], BF16)
        preb = S("L_preb", [128, 2, SEQ + 4], BF16)
        diagw = S("L_diagw", [128, 8, 4, 128], BF16)
        cw = S("L_cw", [128, 8, 4], F32)
        ifT = S("L_ifT", [128, 2, 512], F32)
        ifb = S("L_ifb", [128, 1], F32)

        t_c = Tok()
        c.op("pool", lambda e: e.memset(wif[:], 0.0), writes=[t_c])
        c.op("pool", lambda e: e.memset(vaug[:, :, :, 128:129], 1.0), writes=[t_c])
        c.op("pool", lambda e: e.memset(preb[:, :, 0:3], 0.0), writes=[t_c])
        c.dma("sp", cw[:], self.conv_wT[l].rearrange("(ct p) j -> p ct j", p=128), writes=[t_c])
        c.dma("sp", ifb[0:4, :], self.i_bias[l].rearrange("(h o) -> h o", o=1), writes=[t_c])
        c.dma("sp", ifb[4:8, :], self.f_bias[l].rearrange("(h o) -> h o", o=1), writes=[t_c])
        t_wif = Tok()
        c.dma("pool", wif[:, :, 0:8], self.w_in[l][:, C_MI:C_MI + 8].rearrange("(kt p) c -> p kt c", p=128),
              reads=[t_c], writes=[t_wif])

        t_w = [Tok(), Tok()]
        t_pre = [Tok(), Tok()]
        t_cacc = Tok()
        t_q = [Tok() for _ in range(4)]
        t_k = [Tok() for _ in range(4)]
        t_v = [Tok() for _ in range(NT)]
        t_so = [Tok() for _ in range(NT)]
        cnt = [0]

        t_dw = Tok()
        for ct in range(8):
            for j in range(4):
                c.op("dve", lambda e, ct=ct, j=j: e.tensor_scalar(out=diagw[:, ct, j, :], in0=self.ident_b[:],
                                                                  scalar1=cw[:, ct, j:j + 1], scalar2=None, op0=ALU.mult),
                     reads=[t_c, self.t_const], writes=[t_dw])
        pend = []

        def conv_tile(ct, pbi, dstT, m, t_dst):
            for nb in range(4):
                pi = 4 + nb % 2
                for j in range(4):
                    c.op("pe", lambda e, j=j, nb=nb, pi=pi: e.matmul(
                        pk[pi][:, :], diagw[:, ct, j, :], preb[:, pbi, nb * 512 + j:nb * 512 + j + 512],
                        start=(j == 0), stop=(j == 3)),
                         reads=[t_dw, t_pre[pbi]], writes=[t_pk[pi]], signal=(j == 3))
                c.op("act", lambda e, nb=nb, pi=pi: e.activation(out=dstT[:, m, nb * 512:(nb + 1) * 512], in_=pk[pi][:, :],
                                                                 func=AF.Silu), reads=[], writes=[t_dst[m], t_pk[pi]])

        def qk_proj(col0, dstT, t_dst, wb, ct_base):
            self.load_w(wbuf[:, wb, :, :], self.w_in[l][:, col0:col0 + 512], t_w[wb])
            for m in range(4):
                pbi = cnt[0] % 2
                cnt[0] += 1
                for nb in range(4):
                    pi = nb % 4
                    for k in range(8):
                        c.op("pe", lambda e, k=k, m=m, nb=nb, pi=pi: e.matmul(
                            pk[pi][:, :], wbuf[:, wb, k, m * 128:(m + 1) * 128], self.hT[:, k, nb * 512:(nb + 1) * 512],
                            start=(k == 0), stop=(k == 7)),
                             reads=[t_w[wb]] + self.t_hT[nb * 4:nb * 4 + 4], writes=[t_pk[pi]], signal=(k == 7))
                    if nb % 2 == 0:
                        c.op("act", lambda e, nb=nb, pi=pi, pbi=pbi: e.copy(
                            out=preb[:, pbi, 3 + nb * 512:3 + (nb + 1) * 512], in_=pk[pi][:, :]),
                             reads=[], writes=[t_pre[pbi], t_pk[pi]])
                    else:
                        c.op("dve", lambda e, nb=nb, pi=pi, pbi=pbi: e.tensor_copy(
                            out=preb[:, pbi, 3 + nb * 512:3 + (nb + 1) * 512], in_=pk[pi][:, :]),
                             reads=[], writes=[t_pre[pbi], t_pk[pi]])
                if pend:
                    conv_tile(*pend.pop())
                pend.append((ct_base + m, pbi, dstT, m, t_dst))

        qk_proj(C_MQ, qT, t_q, 0, 0)
        qk_proj(C_MK, kT, t_k, 1, 4)
        conv_tile(*pend.pop())
        self.load_w(wbuf[:, 0, :, :], self.w_in[l][:, C_MV:C_MV + 512], t_w[0])

        def ev_v(tt, ps, tps):
            c.op("dve", lambda e: e.tensor_copy(out=vaug[:, tt, :, 0:128],
                                                in_=ps[:, 0:512].rearrange("p (h d) -> p h d", h=4)),
                 reads=[t_c], writes=[t_v[tt], tps])
        self.proj_tm(wbuf[:, 0], t_w[0], 0, 512, pk[0:4], t_pk[0:4], ev_v)
        self.load_w(wbuf[:, 1, :, :], self.w_in[l][:, C_MO:C_MO + 512], t_w[1])

        def ev_o(tt, ps, tps):
            c.op("act", lambda e: e.activation(out=so[:, tt, :], in_=ps[:, 0:512], func=AF.Sigmoid),
                 reads=[], writes=[t_so[tt], tps])
        self.proj_tm(wbuf[:, 1], t_w[1], 0, 512, pk[0:4], t_pk[0:4], ev_o)
        t_ifb = [Tok(), Tok()]
        tscr = Tok()
        for nb in range(4):
            pi = nb % 4
            for k in range(8):
                c.op("pe", lambda e, k=k, nb=nb, pi=pi: e.matmul(pk[pi][:, :], wif[:, k, :],
                                                                 self.hT[:, k, nb * 512:(nb + 1) * 512],
                                                                 start=(k == 0), stop=(k == 7)),
                     reads=[t_wif] + self.t_hT[nb * 4:nb * 4 + 4], writes=[t_pk[pi]], signal=(k == 7))
            ib = nb % 2
            c.op("dve", lambda e, nb=nb, pi=pi, ib=ib: e.tensor_scalar(out=ifT[0:8, ib, :], in0=pk[pi][0:8, :],
                                                                       scalar1=ifb[0:8, 0:1], scalar2=None, op0=ALU.add),
                 reads=[t_c], writes=[t_ifb[ib], t_pk[pi]])
            c.dma("sp", self.ifscr[:, nb * 512:(nb + 1) * 512], ifT[0:8, ib, :], reads=[t_ifb[ib]], writes=[tscr])
        c.barrier()
        es1.close()
        cur[0] = es
        hg = S("L_hg", [128, 512], F32)
        G = S("L_G", [128, 12, 128], F32)
        F = S("L_F", [128, 4, 128], F32)
        cs = S("L_cs", [128, 4], F32)
        row = S("L_row", [128, 8, 64], F32)
        bc = S("L_bc", [128, 2, 64], F32)
        onesrow = S("L_onesrow", [128, 128], F32)
        e0 = S("L_e0", [128, 1], F32)
        ones = S("L_ones", [128, 128], F32)
        triu = S("L_triu", [128, 128], F32)
        tokfac = S("L_tokfac", [128, 4, 64], F32)
        CT = S("L_CT", [128, 4, 129], F32)
        CTb = S("L_CTb", [128, 4, 129], BF16)
        ktok = S("L_ktok", [128, 4, 128], BF16)
        vw = S("L_vw", [128, 4, 129], BF16)
        spT = S("L_spT", [128, 4, 128], BF16)
        res = S("L_res", [128, 4, 129], F32)
        tmpc = S("L_tmpc", [128, 4, 129], F32)
        sm = S("L_sm", [128, 4, 4], F32)
        hv = S("L_hv", [128, 4, 128], F32)
        junk = S("L_junk", [128, 128], BF16)
        ytok = S("L_ytok", [128, 2, 512], BF16)
        c.op("pool", lambda e: e.memset(onesrow[:], 0.0), writes=[t_c])
        c.op("pool", lambda e: e.memset(onesrow[0:1, :], 1.0), writes=[t_c])
        c.op("pool", lambda e: e.memset(e0[:], 0.0), writes=[t_c])
        c.op("pool", lambda e: e.memset(e0[0:1, :], 1.0), writes=[t_c])
        c.op("pool", lambda e: e.memset(ones[:], 1.0), writes=[t_c])
        c.op("pool", lambda e: e.memset(G[:], 0.0), writes=[t_c])
        c.op("pool", lambda e: e.memset(F[:], 0.0), writes=[t_c])
        c.op("pool", lambda e: e.memset(cs[:], 0.0), writes=[t_c])
        c.op("pool", lambda e: e.memset(row[:], 0.0), writes=[t_c])
        c.op("pool", lambda e: e.memset(CT[:], 0.0), writes=[t_c])
        c.op("pool", lambda e: e.memset(CTb[:], 0.0), writes=[t_c])
        c.dma("sp", triu[:], self.triu_in[:, :], writes=[t_c])
        c.dma("sp", hg[:], self.head_gain[l].partition_broadcast(128), writes=[t_c])
        t_G = Tok()
        c.dma("sp", G[0:64, 0, :], self.ifscr[0:4, :].rearrange("h (c t) -> (h c) t", t=128), reads=[tscr, t_c],
              writes=[t_G])
        c.dma("sp", G[0:64, 1, :], self.ifscr[4:8, :].rearrange("h (c t) -> (h c) t", t=128), reads=[tscr, t_c],
              writes=[t_G])
        R = slice(0, 64)

        def g_op(eng, fn, extra_r=()):
            c.op(eng, fn, reads=[t_c] + list(extra_r), writes=[t_G])
        g_op("act", lambda e: e.activation(out=G[R, 2, :], in_=G[R, 1, :], func=AF.Exp, scale=-1.0))
        g_op("act", lambda e: e.activation(out=G[R, 2, :], in_=G[R, 2, :], func=AF.Ln, bias=1.0, scale=1.0))
        g_op("dve", lambda e: e.tensor_tensor_scan(out=G[R, 3, :], data0=ones[R, :], data1=G[R, 2, :], initial=0.0,
                                                   op0=ALU.mult, op1=ALU.add))
        g_op("dve", lambda e: e.tensor_tensor(out=G[R, 4, :], in0=G[R, 0, :], in1=G[R, 3, :], op=ALU.add))
        g_op("dve", lambda e: e.tensor_tensor_scan(out=G[R, 5, :], data0=ones[R, :], data1=G[R, 4, :], initial=-1e30,
                                                   op0=ALU.mult, op1=ALU.max))
        g_op("dve", lambda e: e.tensor_scalar(out=cs[R, 0:1], in0=G[R, 3, 127:128], scalar1=-1.0, scalar2=None,
                                              op0=ALU.mult))
        g_op("dve", lambda e: e.tensor_copy(out=cs[R, 2:3], in_=G[R, 5, 127:128]))
        g_op("dve", lambda e: e.tensor_tensor(out=cs[R, 1:2], in0=cs[R, 2:3], in1=cs[R, 0:1], op=ALU.add))
        pr_ = pk[4]
        t_pr_ = t_pk[4]
        c.op("pe", lambda e: e.transpose(pr_[:, 0:128], cs[:, 0:1].broadcast_to([128, 128]), self.ident_f[:]),
             reads=[t_G, self.t_const], writes=[t_pr_])
        c.op("pe", lambda e: e.transpose(pr_[:, 128:256], cs[:, 1:2].broadcast_to([128, 128]), self.ident_f[:]),
             reads=[t_G, self.t_const], writes=[t_pr_])
        t_row = Tok()
        c.op("dve", lambda e: e.tensor_copy(out=row[0:1, 0:2, :], in_=pr_[0:1, 0:256].rearrange("p (a b) -> p a b", a=2)[:, :, 0:64]),
             reads=[t_c], writes=[t_row, t_pr_])
        for h in range(4):
            hs = slice(h * 16, (h + 1) * 16)
            c.op("dve", lambda e, hs=hs: e.tensor_tensor_scan(out=row[0:1, 2, hs], data0=row[0:1, 0, hs],
                                                              data1=row[0:1, 1, hs], initial=0.0, op0=ALU.add,
                                                              op1=ALU.max), reads=[], writes=[t_row])
            c.op("dve", lambda e, h=h: e.tensor_copy(out=row[0:1, 3, h * 16 + 1:(h + 1) * 16],
                                                     in_=row[0:1, 2, h * 16:(h + 1) * 16 - 1]), reads=[], writes=[t_row])
        c.op("dve", lambda e: e.tensor_tensor(out=row[0:1, 4, :], in0=row[0:1, 0, :], in1=row[0:1, 3, :], op=ALU.add),
             reads=[], writes=[t_row])
        c.op("dve", lambda e: e.tensor_tensor(out=row[0:1, 4, :], in0=row[0:1, 4, :], in1=row[0:1, 2, :],
                                              op=ALU.subtract), reads=[], writes=[t_row])
        c.op("dve", lambda e: e.tensor_tensor(out=row[0:1, 5, :], in0=row[0:1, 1, :], in1=row[0:1, 2, :],
                                              op=ALU.subtract), reads=[], writes=[t_row])
        c.op("act", lambda e: e.activation(out=row[0:1, 4:6, :], in_=row[0:1, 4:6, :], func=AF.Exp), reads=[],
             writes=[t_row])
        t_bc = Tok()
        c.op("pe", lambda e: e.matmul(pr_[:, 0:128], onesrow[:, :], row[:, 4:6, :].rearrange("p a b -> p (a b)"),
                                      start=True, stop=True), reads=[t_row, t_c], writes=[t_pr_])
        c.op("dve", lambda e: e.tensor_copy(out=bc[:], in_=pr_[:, 0:128].rearrange("p (a b) -> p a b", a=2)), reads=[],
             writes=[t_bc, t_pr_])
        c.op("pe", lambda e: e.matmul(pr_[0:64, 256:257], row[:, 3, :], e0[:, 0:1], start=True, stop=True),
             reads=[t_row, t_c], writes=[t_pr_])
        c.op("dve", lambda e: e.tensor_copy(out=cs[R, 3:4], in_=pr_[0:64, 256:257]), reads=[t_c], writes=[t_G, t_pr_])
        g_op("dve", lambda e: e.tensor_tensor(out=G[R, 6, :], in0=G[R, 5, :], in1=G[R, 3, :], op=ALU.subtract))
        g_op("dve", lambda e: e.tensor_scalar(out=G[R, 7, :], in0=G[R, 3, :], scalar1=-1.0, scalar2=cs[R, 3:4],
                                              op0=ALU.mult, op1=ALU.add))
        g_op("dve", lambda e: e.tensor_tensor(out=G[R, 8, :], in0=G[R, 6, :], in1=G[R, 7, :], op=ALU.max))
        g_op("dve", lambda e: e.tensor_scalar(out=G[R, 9, :], in0=G[R, 4, :], scalar1=cs[R, 2:3], scalar2=None,
                                              op0=ALU.subtract))
        g_op("act", lambda e: e.activation(out=F[R, 0, :], in_=G[R, 9, :], func=AF.Exp))
        g_op("dve", lambda e: e.tensor_scalar(out=F[R, 0, :], in0=F[R, 0, :], scalar1=SC, scalar2=None, op0=ALU.mult))
        g_op("dve", lambda e: e.tensor_tensor(out=G[R, 9, :], in0=G[R, 3, :], in1=G[R, 8, :], op=ALU.add))
        g_op("dve", lambda e: e.tensor_scalar(out=G[R, 9, :], in0=G[R, 9, :], scalar1=-1.0, scalar2=cs[R, 2:3],
                                              op0=ALU.mult, op1=ALU.add))
        g_op("act", lambda e: e.activation(out=F[R, 1, :], in_=G[R, 9, :], func=AF.Exp))
        g_op("dve", lambda e: e.tensor_tensor(out=G[R, 10, :], in0=G[R, 7, :], in1=G[R, 8, :], op=ALU.subtract))
        g_op("act", lambda e: e.activation(out=F[R, 2, :], in_=G[R, 10, :], func=AF.Exp))
        g_op("act", lambda e: e.activation(out=F[R, 3, :], in_=G[R, 8, :], func=AF.Exp, scale=-1.0))
        for q in range(4):
            c.op("pe", lambda e, q=q: e.transpose(pk[5][:, q * 128:(q + 1) * 128], F[:, q, :], self.ident_f[:]),
                 reads=[t_G, self.t_const], writes=[t_pk[5]])
        t_tf = Tok()
        c.op("dve", lambda e: e.tensor_copy(out=tokfac[:], in_=pk[5][:, :].rearrange("p (q m) -> p q m", q=4)[:, :, 0:64]),
             reads=[], writes=[t_tf, t_pk[5]])
        if self.debug.get("mlstm_dump"):
            self.dbg_dump("tokfac", tokfac[:], [128, 4, 64], [t_tf])
            self.dbg_dump("bc", bc[:], [128, 2, 64], [t_bc])
            self.dbg_dump("cs", cs[:], [128, 4], [t_G])

        t_CT = [Tok() for _ in range(4)]
        t_CTb = [Tok() for _ in range(4)]
        t_kt = [Tok() for _ in range(4)]
        t_vw = [Tok() for _ in range(4)]
        t_sp = [Tok() for _ in range(4)]
        t_res = [Tok() for _ in range(4)]
        t_tmp = [Tok() for _ in range(4)]
        t_sm = [Tok() for _ in range(4)]
        t_hv = [Tok() for _ in range(4)]
        t_junk = Tok()
        t_yt = [Tok(), Tok()]
        for cc in range(NT):
            csl = slice(cc * 128, (cc + 1) * 128)
            yb = cc % 2
            fac = []
            for h in range(4):
                n = h * 16 + cc
                fac.append((tokfac[:, 0, n:n + 1], tokfac[:, 1, n:n + 1], tokfac[:, 2, n:n + 1], tokfac[:, 3, n:n + 1], n))
            for h in range(4):
                fr = fac[h][0]
                p2 = h % 2
                c.op("pe", lambda e, h=h: e.transpose(pb16[0][:, h * 128:(h + 1) * 128], kT[:, h, csl], self.ident_b[:]),
                     reads=[t_k[h], self.t_const], writes=[t_pb16[0]], signal=(h == 3))
            c.op("act", lambda e: e.copy(out=ktok[:, :, :], in_=pb16[0][:, 0:512].rearrange("p (h t) -> p h t", h=4)),
                 reads=[], writes=t_kt + [t_pb16[0]])
            for h in range(4):
                fr = fac[h][0]
                p2 = h % 2
                c.op("act", lambda e, h=h, fr=fr: e.activation(out=vw[:, h, :], in_=vaug[:, cc, h, :], func=AF.Copy, scale=fr),
                     reads=[t_v[cc], t_tf, t_c], writes=[t_vw[h]])
                c.op("pe", lambda e, h=h, p2=p2: e.matmul(pk[p2][:, 0:128], kT[:, h, csl], qT[:, h, csl], start=True,
                                                          stop=True),
                     reads=[t_k[h], t_q[h]], writes=[t_pk[p2]])
                c.op("dve", lambda e, h=h, fr=fr, p2=p2: e.scalar_tensor_tensor(out=spT[:, h, :], in0=pk[p2][:, 0:128],
                                                                                scalar=fr, in1=triu[:], op0=ALU.mult,
                                                                                op1=ALU.mult),
                     reads=[t_tf, t_c], writes=[t_sp[h], t_pk[p2]])
            for h in range(4):
                fr, fc, fi, fe, n = fac[h]
                p2 = 2 + h % 2
                c.op("pe", lambda e, h=h, p2=p2: e.matmul(pk[p2][:, 0:129], spT[:, h, :], vaug[:, cc, h, :], start=True,
                                                          stop=True), reads=[t_sp[h], t_v[cc], t_c], writes=[t_pk[p2]],
                     signal=False)
                c.op("pe", lambda e, h=h, p2=p2: e.matmul(pk[p2][:, 256:385], qT[:, h, csl], CTb[:, h, :], start=True,
                                                          stop=True), reads=[t_q[h], t_CTb[h]], writes=[t_pk[p2]])
                c.op("dve", lambda e, h=h, fc=fc, p2=p2: e.tensor_scalar(out=tmpc[:, h, :], in0=pk[p2][:, 0:129], scalar1=fc,
                                                                         scalar2=None, op0=ALU.mult),
                     reads=[t_tf], writes=[t_tmp[h], t_pk[p2]])
                c.op("dve", lambda e, h=h, fi=fi, p2=p2: e.scalar_tensor_tensor(out=res[:, h, :], in0=pk[p2][:, 256:385],
                                                                                scalar=fi, in1=tmpc[:, h, :], op0=ALU.mult,
                                                                                op1=ALU.add),
                     reads=[t_tf, t_tmp[h]], writes=[t_res[h], t_pk[p2]])
            for h in range(4):
                n = fac[h][4]
                p2 = 4 + h % 2
                c.op("pe", lambda e, h=h, p2=p2: e.matmul(pk[p2][:, 0:129], ktok[:, h, :], vw[:, h, :], start=True, stop=True),
                     reads=[t_kt[h], t_vw[h]], writes=[t_pk[p2]])
                c.op("act", lambda e, h=h, n=n: e.activation(out=CT[:, h, :], in_=CT[:, h, :], func=AF.Copy,
                                                             scale=bc[:, 0, n:n + 1]),
                     reads=[t_bc, t_c], writes=[t_CT[h]])
                c.op("dve", lambda e, h=h, n=n, p2=p2: e.scalar_tensor_tensor(out=CT[:, h, :], in0=pk[p2][:, 0:129],
                                                                              scalar=bc[:, 1, n:n + 1], in1=CT[:, h, :],
                                                                              op0=ALU.mult, op1=ALU.add),
                     reads=[t_bc], writes=[t_CT[h], t_pk[p2]])
            for h in range(4):
                fe = fac[h][3]
                c.op("act", lambda e, h=h: e.copy(out=CTb[:, h, :], in_=CT[:, h, :]), reads=[t_CT[h]], writes=[t_CTb[h]])
                c.op("dve", lambda e, h=h: e.tensor_scalar(out=sm[:, h, 3:4], in0=res[:, h, 128:129], scalar1=-1.0,
                                                           scalar2=None, op0=ALU.mult), reads=[t_res[h]], writes=[t_sm[h]])
                c.op("dve", lambda e, h=h: e.tensor_tensor(out=sm[:, h, 0:1], in0=res[:, h, 128:129], in1=sm[:, h, 3:4],
                                                           op=ALU.max), reads=[t_res[h]], writes=[t_sm[h]])
                c.op("dve", lambda e, h=h, fe=fe: e.tensor_tensor(out=sm[:, h, 0:1], in0=sm[:, h, 0:1], in1=fe,
                                                                  op=ALU.max), reads=[t_tf], writes=[t_sm[h]])
                c.op("dve", lambda e, h=h: e.reciprocal(out=sm[:, h, 0:1], in_=sm[:, h, 0:1]), reads=[], writes=[t_sm[h]])
            for h in range(4):
                c.op("act", lambda e, h=h: e.activation(out=hv[:, h, :], in_=res[:, h, 0:128], func=AF.Copy,
                                                        scale=sm[:, h, 0:1]),
                     reads=[t_res[h], t_sm[h]], writes=[t_hv[h]])
                c.op("act", lambda e, h=h: e.activation(out=junk[:], in_=hv[:, h, :], func=AF.Square,
                                                        accum_out=sm[:, h, 1:2]),
                     reads=[t_hv[h]], writes=[t_junk, t_sm[h]])
                c.op("act", lambda e, h=h: e.activation(out=sm[:, h, 2:3], in_=sm[:, h, 1:2], func=AF.Sqrt, scale=1.0 / 128,
                                                        bias=self.eps_ap), reads=[self.t_const], writes=[t_sm[h]])
            for h in range(4):
                c.op("dve", lambda e, h=h: e.reciprocal(out=sm[:, h, 2:3], in_=sm[:, h, 2:3]), reads=[], writes=[t_sm[h]])
                c.op("dve", lambda e, h=h: e.scalar_tensor_tensor(out=hv[:, h, :], in0=hv[:, h, :], scalar=sm[:, h, 2:3],
                                                                  in1=hg[:, h * 128:(h + 1) * 128], op0=ALU.mult,
                                                                  op1=ALU.mult),
                     reads=[t_sm[h], t_c], writes=[t_hv[h]])
                c.op("dve", lambda e, h=h, yb=yb: e.tensor_tensor(out=ytok[:, yb, h * 128:(h + 1) * 128], in0=hv[:, h, :],
                                                                  in1=so[:, cc, h * 128:(h + 1) * 128], op=ALU.mult),
                     reads=[t_hv[h], t_so[cc]], writes=[t_yt[yb]])
            for ct in range(4):
                c.op("pe", lambda e, ct=ct, yb=yb: e.transpose(pb16[1][:, ct * 128:(ct + 1) * 128],
                                                               ytok[:, yb, ct * 128:(ct + 1) * 128], self.ident_b[:]),
                     reads=[t_yt[yb], self.t_const], writes=[t_pb16[1]], signal=(ct == 3))
            c.op("act", lambda e, cc=cc: e.copy(out=yT[:, :, cc * 128:(cc + 1) * 128],
                                                in_=pb16[1][:, 0:512].rearrange("p (c t) -> p c t", c=4)),
                 reads=[], writes=[t_yT[cc], t_pb16[1]])
    c.barrier()


Builder.phase_mlstm = _phase_mlstm


def _declare_mlstm_inputs(self):
    self.triu_in = self.inp("triu", [128, 128])
    self.conv_wT = self.inp("conv_wT", [DEPTH, 1024, 4])
    self.head_gain = self.inp("mlstm_head_gain", [DEPTH, 512])
    self.i_bias = self.inp("mlstm_i_bias", [DEPTH, 4])
    self.f_bias = self.inp("mlstm_f_bias", [DEPTH, 4])
    self.ifscr = self.nc.dram_tensor("ifscr", [8, SEQ], F32).ap()


Builder.declare_mlstm_inputs = _declare_mlstm_inputs


def _build_test_mlstm(self):
    self.declare_inputs()
    self.declare_mlstm_inputs()
    self.setup_consts()
    self.phase_A(0, self.x_in)
    self.yT = [self.sb("yT%d" % i, [128, 4, SEQ], BF16).__enter__() for i in range(3)]
    self.t_yT = [[Tok() for _ in range(NT)] for _ in range(3)]
    self.phase_mlstm(0, self.yT[1], self.t_yT[1])
    self.dbg_dump("y_mlstm", self.yT[1][:], [128, 4, SEQ], self.t_yT[1], BF16)
    self.finish()
    return self.nc


Builder.build_test_mlstm = _build_test_mlstm


def _declare_ssm_inputs(self):
    self.ssm_sp = self.inp("ssm_sp", [DEPTH, 128, 3, 16])
    self.ssm_b = self.inp("ssm_b", [DEPTH, 128, 2, 16, 32])
    self.ssm_c = self.inp("ssm_c", [DEPTH, 128, 2, 16, 32])
    self.ssm_dT = self.inp("ssm_dT", [DEPTH, 128, 4])
    self.bdmask_in = self.inp("bdmask", [128, 4])
    nc = self.nc
    self.ssm_injT_scr = nc.dram_tensor("ssm_injT_scr", [128, 4, SSM_L, 2, 128], BF16).ap()
    self.ssm_read_scr = nc.dram_tensor("ssm_read_scr", [128, SSM_L + 1, 2, 16, 32], BF16).ap()
    self.ssm_bb_scr = nc.dram_tensor("ssm_bb_scr", [128, 2, 16, 32], BF16).ap()
    self.ssm_m12_scr = nc.dram_tensor("ssm_m12_scr", [128, 2, 2, 16], F32).ap()


Builder.declare_ssm_inputs = _declare_ssm_inputs


def _ssm_setup_gen(self, l, es, pbanks, t_pbanks, injT_sb=None, t_injT=None):
    c = self.c
    L = SSM_L
    TWO_PI = 2.0 * math.pi
    MAGIC = 12582912.0
    S = lambda n, sh, dt: es.enter_context(self.sb(n, sh, dt))
    sp = S("Z_sp", [128, 3, 16], F32)
    APW = S("Z_APW", [128, L + 1, 2, 16], F32)
    Bb = S("Z_Bb", [128, 2, 16, 32], F32)
    Cc = S("Z_Cc", [128, 2, 16, 32], F32)
    W1 = S("Z_W1", [128, 2, 2, 16, 32], F32)
    W2 = S("Z_W2", [128, 2, 2, 16, 32], F32)
    nCc = S("Z_nCc", [128, 2, 16, 32], F32)
    sm = S("Z_sm", [128, 12, 16], F32)
    Inj = S("Z_Inj", [128, 1, 2, 2, 16, 32], F32)
    stg = S("Z_stg", [128, 2, 4, 2, 2, 128], BF16) if injT_sb is None else None
    rstg = S("Z_rstg", [128, 1, 2, 2, 16, 32], BF16)
    bstg = S("Z_bstg", [128, 2, 16, 32], BF16)
    M12 = S("Z_M12", [128, 2, 2, 16], F32)
    t_c = Tok()
    c.dma("sp", sp[:], self.ssm_sp[l], writes=[t_c])
    c.dma("sp", Bb[:], self.ssm_b[l], writes=[t_c])
    c.dma("sp", Cc[:], self.ssm_c[l], writes=[t_c])
    yield
    t_s = Tok()

    def sop(eng, fn):
        c.op(eng, fn, reads=[t_c], writes=[t_s])
    ar, ai, ldt = sp[:, 0, :], sp[:, 1, :], sp[:, 2, :]
    dt_, mag, th, cs_, sn_ = sm[:, 0, :], sm[:, 1, :], sm[:, 2, :], sm[:, 3, :], sm[:, 4, :]
    t0, t1_, abr, abi = sm[:, 5, :], sm[:, 6, :], sm[:, 7, :], sm[:, 8, :]
    fr, fi, rden = sm[:, 9, :], sm[:, 10, :], sm[:, 11, :]
    sop("act", lambda e: e.activation(out=dt_, in_=ldt, func=AF.Exp))
    sop("dve", lambda e: e.tensor_tensor(out=t0, in0=dt_, in1=ar, op=ALU.mult))
    sop("act", lambda e: e.activation(out=mag, in_=t0, func=AF.Exp))
    sop("dve", lambda e: e.tensor_tensor(out=th, in0=dt_, in1=ai, op=ALU.mult))

    def sin_of(dst, shift):
        sop("dve", lambda e: e.tensor_scalar(out=t0, in0=th, scalar1=shift, scalar2=None, op0=ALU.add))
        sop("dve", lambda e: e.tensor_scalar(out=t1_, in0=t0, scalar1=1.0 / TWO_PI, scalar2=MAGIC, op0=ALU.mult,
                                             op1=ALU.add))
        sop("dve", lambda e: e.tensor_scalar(out=t1_, in0=t1_, scalar1=-MAGIC, scalar2=None, op0=ALU.add))
        sop("dve", lambda e: e.scalar_tensor_tensor(out=t0, in0=t1_, scalar=-TWO_PI, in1=t0, op0=ALU.mult,
                                                    op1=ALU.add))
        sop("dve", lambda e: e.tensor_scalar(out=t0, in0=t0, scalar1=math.pi, scalar2=-math.pi, op0=ALU.min,
                                             op1=ALU.max))
        sop("act", lambda e: e.activation(out=dst, in_=t0, func=AF.Sin))
    sin_of(sn_, 0.0)
    sin_of(cs_, math.pi / 2)
    sop("dve", lambda e: e.tensor_tensor(out=abr, in0=mag, in1=cs_, op=ALU.mult))
    sop("dve", lambda e: e.tensor_tensor(out=abi, in0=mag, in1=sn_, op=ALU.mult))
    sop("dve", lambda e: e.tensor_tensor(out=t0, in0=ar, in1=ar, op=ALU.mult))
    sop("dve", lambda e: e.tensor_tensor(out=t1_, in0=ai, in1=ai, op=ALU.mult))
    sop("dve", lambda e: e.tensor_tensor(out=rden, in0=t0, in1=t1_, op=ALU.add))
    sop("dve", lambda e: e.reciprocal(out=rden, in_=rden))
    sop("dve", lambda e: e.tensor_scalar(out=mag, in0=abr, scalar1=-1.0, scalar2=None, op0=ALU.add))
    sop("dve", lambda e: e.tensor_tensor(out=t0, in0=mag, in1=ar, op=ALU.mult))
    sop("dve", lambda e: e.tensor_tensor(out=t1_, in0=abi, in1=ai, op=ALU.mult))
    sop("dve", lambda e: e.tensor_tensor(out=t0, in0=t0, in1=t1_, op=ALU.add))
    sop("dve", lambda e: e.tensor_tensor(out=fr, in0=t0, in1=rden, op=ALU.mult))
    sop("dve", lambda e: e.tensor_tensor(out=t0, in0=abi, in1=ar, op=ALU.mult))
    sop("dve", lambda e: e.tensor_tensor(out=t1_, in0=mag, in1=ai, op=ALU.mult))
    sop("dve", lambda e: e.tensor_tensor(out=t0, in0=t0, in1=t1_, op=ALU.subtract))
    sop("dve", lambda e: e.tensor_tensor(out=fi, in0=t0, in1=rden, op=ALU.mult))
    sop("dve", lambda e: e.memset(APW[:, 0, 0, :], 1.0))
    sop("dve", lambda e: e.memset(APW[:, 0, 1, :], 0.0))
    for k in range(1, L + 1):
        pr_, pi_ = APW[:, k - 1, 0, :], APW[:, k - 1, 1, :]
        sop("dve", lambda e, pr_=pr_: e.tensor_tensor(out=t0, in0=pr_, in1=abr, op=ALU.mult))
        sop("dve", lambda e, pi_=pi_: e.tensor_tensor(out=t1_, in0=pi_, in1=abi, op=ALU.mult))
        sop("dve", lambda e, k=k: e.tensor_tensor(out=APW[:, k, 0, :], in0=t0, in1=t1_, op=ALU.subtract))
        sop("dve", lambda e, pr_=pr_: e.tensor_tensor(out=t0, in0=pr_, in1=abi, op=ALU.mult))
        sop("dve", lambda e, pi_=pi_: e.tensor_tensor(out=t1_, in0=pi_, in1=abr, op=ALU.mult))
        sop("dve", lambda e, k=k: e.tensor_tensor(out=APW[:, k, 1, :], in0=t0, in1=t1_, op=ALU.add))

    def cmulK(out_r, out_i, Xr, Xi, Xi_imag_r, Xi_imag_i, k0, K, t_x, t_out):
        def xb(v):
            return v.unsqueeze(1).broadcast_to([128, K, 16, 32])

        def ab(ri):
            return APW[:, k0:k0 + K, ri, :].unsqueeze(3).broadcast_to([128, K, 16, 32])
        c.op("dve", lambda e: e.tensor_tensor(out=W1[:, 0, 0:K], in0=xb(Xr), in1=ab(0), op=ALU.mult), reads=[t_x, t_s],
             writes=[t_w1])
        c.op("dve", lambda e: e.tensor_tensor(out=W1[:, 1, 0:K], in0=xb(Xi), in1=ab(1), op=ALU.mult), reads=[t_x, t_s],
             writes=[t_w1])
        c.op("dve", lambda e: e.tensor_tensor(out=out_r, in0=W1[:, 0, 0:K], in1=W1[:, 1, 0:K], op=ALU.subtract),
             reads=[t_w1], writes=[t_out])
        c.op("pool", lambda e: e.tensor_tensor(out=W2[:, 0, 0:K], in0=xb(Xi_imag_r), in1=ab(1), op=ALU.mult),
             reads=[t_x, t_s], writes=[t_w2])
        c.op("pool", lambda e: e.tensor_tensor(out=W2[:, 1, 0:K], in0=xb(Xi_imag_i), in1=ab(0), op=ALU.mult),
             reads=[t_x, t_s], writes=[t_w2])
        c.op("pool", lambda e: e.tensor_tensor(out=out_i, in0=W2[:, 0, 0:K], in1=W2[:, 1, 0:K], op=ALU.add),
             reads=[t_w2], writes=[t_out])

    def bc32(v):
        return v.unsqueeze(2).broadcast_to([128, 16, 32])

    def cmul(out_r, out_i, Xr, Xi, Yr, Yi, t_x, t_out):
        c.op("dve", lambda e: e.tensor_tensor(out=W1[:, 0, 0], in0=Xr, in1=bc32(Yr), op=ALU.mult), reads=[t_x, t_s],
             writes=[t_w1])
        c.op("dve", lambda e: e.tensor_tensor(out=W1[:, 1, 0], in0=Xi, in1=bc32(Yi), op=ALU.mult), reads=[t_x, t_s],
             writes=[t_w1])
        c.op("dve", lambda e: e.tensor_tensor(out=out_r, in0=W1[:, 0, 0], in1=W1[:, 1, 0], op=ALU.subtract),
             reads=[t_w1], writes=[t_out])
        c.op("pool", lambda e: e.tensor_tensor(out=W2[:, 0, 0], in0=Xr, in1=bc32(Yi), op=ALU.mult), reads=[t_x, t_s],
             writes=[t_w2])
        c.op("pool", lambda e: e.tensor_tensor(out=W2[:, 1, 0], in0=Xi, in1=bc32(Yr), op=ALU.mult), reads=[t_x, t_s],
             writes=[t_w2])
        c.op("pool", lambda e: e.tensor_tensor(out=out_i, in0=W2[:, 0, 0], in1=W2[:, 1, 0], op=ALU.add), reads=[t_w2],
             writes=[t_out])
    t_w1, t_w2 = Tok(), Tok()
    t_Bb = Tok()
    t_inj = [Tok(), Tok()]
    cmul(Inj[:, 0, 0, 0], Inj[:, 0, 0, 1], Bb[:, 0], Bb[:, 1], fr, fi, t_c, t_inj[0])
    c.op("dve", lambda e: e.tensor_copy(out=Bb[:, 0], in_=Inj[:, 0, 0, 0]), reads=[t_inj[0], t_c], writes=[t_Bb])
    c.op("pool", lambda e: e.tensor_copy(out=Bb[:, 1], in_=Inj[:, 0, 0, 1]), reads=[t_inj[0], t_c], writes=[t_Bb])
    c.op("pool", lambda e: e.tensor_scalar(out=nCc[:], in0=Cc[:], scalar1=-1.0, scalar2=None, op0=ALU.mult),
         reads=[t_c], writes=[t_c])
    yield
    t_bst = Tok()
    c.op("act", lambda e: e.copy(out=bstg[:], in_=Bb[:]), reads=[t_Bb], writes=[t_bst])
    c.dma("sp", self.ssm_bb_scr, bstg[:], reads=[t_bst])
    t_m = Tok()
    c.op("dve", lambda e: e.tensor_copy(out=M12[:, 0, 0, :], in_=APW[:, L, 0, :]), reads=[t_s], writes=[t_m])
    c.op("dve", lambda e: e.tensor_copy(out=M12[:, 0, 1, :], in_=APW[:, L, 0, :]), reads=[t_s], writes=[t_m])
    c.op("dve", lambda e: e.tensor_scalar(out=M12[:, 1, 0, :], in0=APW[:, L, 1, :], scalar1=-1.0, scalar2=None,
                                          op0=ALU.mult), reads=[t_s], writes=[t_m])
    c.op("dve", lambda e: e.tensor_copy(out=M12[:, 1, 1, :], in_=APW[:, L, 1, :]), reads=[t_s], writes=[t_m])
    c.dma("sp", self.ssm_m12_scr, M12[:], reads=[t_m])
    yield
    t_stg = [Tok(), Tok()]
    for kp in range(L // 2):
        ib = 0
        sb_ = kp % 2
        cmulK(Inj[:, ib, :, 0], Inj[:, ib, :, 1], Bb[:, 0], Bb[:, 1], Bb[:, 0], Bb[:, 1], 2 * kp, 2, t_Bb, t_inj[ib])
        for kk in range(2):
            for ri in range(2):
                pi = (2 * kk + ri) % len(pbanks)
                pb, tpb = pbanks[pi], t_pbanks[pi]
                for T in range(4):
                    c.op("pe", lambda e, T=T, ri=ri, ib=ib, pb=pb, kk=kk: e.transpose(
                        pb[:, T * 128:(T + 1) * 128],
                        Inj[:, ib, kk, ri, 4 * T:4 * T + 4, :].rearrange("p a b -> p (a b)"),
                        self.ident_f[:]), reads=[t_inj[ib], self.t_const], writes=[tpb], signal=(T == 3))
                if injT_sb is not None:
                    k = 2 * kp + kk
                    c.op("act", lambda e, k=k, ri=ri, pb=pb: e.copy(out=injT_sb[:, :, k, ri, :],
                                                                  in_=pb[:, :].rearrange("p (t m) -> p t m", t=4)),
                         reads=[], writes=[t_injT, tpb])
                else:
                    c.op("act", lambda e, kk=kk, ri=ri, pb=pb, sb_=sb_: e.copy(out=stg[:, sb_, :, kk, ri, :],
                                                                              in_=pb[:, :].rearrange("p (t m) -> p t m", t=4)),
                         reads=[], writes=[t_stg[sb_], tpb])
        if injT_sb is None:
            c.dma("sp", self.ssm_injT_scr[:, :, 2 * kp:2 * kp + 2, :, :], stg[:, sb_], reads=[t_stg[sb_]])
        yield
    yield "inj_done"
    t_rst = [Tok(), Tok()]
    k0 = 0
    n_ = 0
    while k0 < L + 1:
        K = min(2, L + 1 - k0)
        rb = 0
        n_ += 1
        cmulK(rstg[:, rb, 0:K, 0], rstg[:, rb, 0:K, 1], Cc[:, 0], Cc[:, 1], nCc[:, 0], nCc[:, 1], k0, K, t_c, t_rst[rb])
        c.dma("sp", self.ssm_read_scr[:, k0:k0 + K], rstg[:, rb, 0:K], reads=[t_rst[rb]])
        k0 += K
        yield


Builder.ssm_setup_gen = _ssm_setup_gen


def _ssm_setup_standalone(self, l):
    with ExitStack() as es:
        banks = [es.enter_context(self.ps("Z_p%d" % i, [128, 512])) for i in range(4)]
        toks = [Tok() for _ in range(4)]
        for _ in self.ssm_setup_gen(l, es, banks, toks):
            pass
    self.c.barrier()


Builder.ssm_setup_standalone = _ssm_setup_standalone


def _phase_ssm(self, l, yT, t_yT):
    c = self.c
    L = SSM_L
    with ExitStack() as es:
        cur = [es]
        S = lambda n, sh, dt: cur[0].enter_context(self.sb(n, sh, dt))
        P = lambda n, sh, dt=F32: es.enter_context(self.ps(n, sh, dt))
        pk = [P("S_p%d" % i, [128, 512]) for i in range(8)]
        t_pk = [Tok() for _ in range(8)]
        uT = S("S_uT", [128, 4, SEQ], BF16)
        SL = S("S_SL", [128, NCH, 2, 16], F32)
        Bb = S("S_Bb16", [128, 2, 16, 32], BF16)
        M12 = S("S_M12", [128, 2, 2, 16], F32)
        dcol = S("S_dcol", [128, 4], F32)
        bdm = S("S_bdm", [128, 4], F32)
        es1 = ExitStack()
        cur[0] = es1
        wu = S("S_wu", [128, 8, 512], BF16)
        InjT = S("S_InjT", [128, 4, L, 2, 128], BF16)

        t_c = Tok()
        t_wu = Tok()
        t_injT = Tok()
        t_Bb = Tok()
        t_m = Tok()
        t_wg = Tok()
        self.load_w(wu[:], self.w_in[l][:, C_U:C_U + 512], t_wu)
        c.dma("sp", dcol[:], self.ssm_dT[l], writes=[t_c])
        c.dma("sp", bdm[:], self.bdmask_in[:, :], writes=[t_c])
        gen = self.ssm_setup_gen(l, es1, pk[4:8], t_pk[4:8], injT_sb=InjT, t_injT=t_injT)
        t_u = [Tok() for _ in range(4)]

        def ev_u(m, nb, ps, tps):
            eng = "act" if (m + nb) % 2 == 0 else "dve"
            if eng == "act":
                c.op("act", lambda e: e.copy(out=uT[:, m, nb * 512:(nb + 1) * 512], in_=ps[:, :]), reads=[],
                     writes=[t_u[m], tps])
            else:
                c.op("dve", lambda e: e.tensor_copy(out=uT[:, m, nb * 512:(nb + 1) * 512], in_=ps[:, :]), reads=[],
                     writes=[t_u[m], tps])
            next(gen, None)
        self.proj_fm(wu, t_wu, 4, pk[0:4], t_pk[0:4], ev_u)
        for r in gen:
            if r == "inj_done":
                break
        t_SL = Tok()
        cnt = 0
        for gp in range(16):
            T, j = gp // 4, gp % 4
            rows = slice(32 * j, 32 * j + 32)
            for ri in range(2):
                pb = pk[cnt % 4]
                tpb = t_pk[cnt % 4]
                cnt += 1
                for tp in range(L):
                    c.op("pe", lambda e, T=T, rows=rows, tp=tp, ri=ri, pb=pb, j=j: e.matmul(
                        pb[:, 0:NCH], InjT[rows, T, L - 1 - tp, ri, :], uT[rows, T, tp::L], start=(tp == 0),
                        stop=(tp == L - 1), tile_position=(32 * j, 0)),
                         reads=[t_injT, t_u[T]], writes=[tpb], signal=(tp == L - 1))
                eng = "act" if ri == 0 else "dve"
                if eng == "act":
                    c.op("act", lambda e, gp=gp, ri=ri, pb=pb: e.copy(out=SL[:, :, ri, gp], in_=pb[:, 0:NCH]), reads=[],
                         writes=[t_SL, tpb])
                else:
                    c.op("dve", lambda e, gp=gp, ri=ri, pb=pb: e.tensor_copy(out=SL[:, :, ri, gp], in_=pb[:, 0:NCH]),
                         reads=[], writes=[t_SL, tpb])
                next(gen, None)
        for _ in gen:
            pass
        c.barrier()
        es1.close()
        cur[0] = es
        Readb = S("S_Readb", [128, L + 1, 2, 16, 32], BF16)
        BD = S("S_BD", [128, 4, L, 128], BF16)
        Sinb = S("S_Sinb", [128, 2, 16, NCH], BF16)
        I0p = S("S_I0p", [128, 2, 4, 128], BF16)
        st = S("S_st", [128, 2, 2, 16], F32)
        SLflat = SL[:, :, :, :].rearrange("p c r g -> p (c r g)")
        yraw = SLflat[:, 0:SEQ]
        gtmp = SLflat[:, SEQ:2 * SEQ]
        wg = S("S_wg", [128, 4, 512], BF16)
        sgb = S("S_sgb", [128, 2, 512], BF16)
        c.dma("pool", wg[:], self.w_glu[l].rearrange("(kt p) c -> p kt c", p=128), writes=[t_wg])
        c.dma("sp", Bb[:], self.ssm_bb_scr, writes=[t_Bb])
        c.dma("sp", M12[:], self.ssm_m12_scr, writes=[t_m])
        t_rd = Tok()
        for kq in range(0, L + 1, 6):
            k1 = min(L + 1, kq + 6)
            c.dma("sp", Readb[:, kq:k1], self.ssm_read_scr[:, kq:k1], writes=[t_rd])
        t_s = t_m
        t_st = Tok()
        for cc in range(1, NCH - 1):
            prev = SL[:, cc - 1, :, :]
            prev_sw = SL[:, cc - 1, ::-1, :]
            c.op("dve", lambda e, prev=prev: e.tensor_tensor(out=st[:, 0], in0=M12[:, 0], in1=prev, op=ALU.mult),
                 reads=[t_m, t_SL], writes=[t_st])
            c.op("dve", lambda e, prev_sw=prev_sw: e.tensor_tensor(out=st[:, 1], in0=M12[:, 1], in1=prev_sw, op=ALU.mult),
                 reads=[t_m, t_SL], writes=[t_st])
            c.op("dve", lambda e, cc=cc: e.tensor_tensor(out=st[:, 0], in0=st[:, 0], in1=SL[:, cc, :, :], op=ALU.add),
                 reads=[t_SL], writes=[t_st])
            c.op("dve", lambda e, cc=cc: e.tensor_tensor(out=SL[:, cc, :, :], in0=st[:, 0], in1=st[:, 1], op=ALU.add),
                 reads=[t_st], writes=[t_SL])
        t_sin = Tok()
        c.op("pool", lambda e: e.memset(Sinb[:, :, :, 0:1], 0.0), writes=[t_sin])
        c.op("dve", lambda e: e.tensor_copy(out=Sinb[:, :, :, 1:NCH],
                                            in_=SL[:, 0:NCH - 1, :, :].rearrange("p c r g -> p r g c")),
             reads=[t_SL], writes=[t_sin])
        c.barrier()
        t_I0 = Tok()
        t_BD = [Tok() for _ in range(4)]
        t_yraw = Tok()
        t_g = Tok()
        GC = 2.0 * math.sqrt(2.0 / math.pi)
        ev_cnt = 0
        for T in range(4):
            c.op("pool", lambda e: e.memset(I0p[:], 0.0), writes=[t_I0])
            for ri in range(2):
                for j in range(4):
                    c.op("pool", lambda e, ri=ri, j=j, T=T: e.tensor_copy(out=I0p[:, ri, j, 32 * j:32 * j + 32],
                                                                         in_=Bb[:, ri, 4 * T + j, :]),
                         reads=[t_Bb], writes=[t_I0])
            pb = pk[4 + T % 2]
            tpb = t_pk[4 + T % 2]
            n_mm = 0
            for ri in range(2):
                for j in range(4):
                    c.op("pe", lambda e, ri=ri, j=j, T=T, pb=pb, n_mm=n_mm: e.matmul(
                        pb[:, :], I0p[:, ri, j, :], Readb[:, 0:L, ri, 4 * T + j, :], start=(n_mm == 0), stop=(n_mm == 7)),
                         reads=[t_I0, t_rd], writes=[tpb], signal=(n_mm == 7))
                    n_mm += 1
            c.op("dve", lambda e, T=T, pb=pb: e.tensor_tensor(
                out=BD[:, T, :, :].rearrange("p k (j x) -> p k j x", j=4),
                in0=pb[:, :].rearrange("p (k x) -> p k x", k=L).unsqueeze(2).broadcast_to([128, L, 4, 32]),
                in1=bdm[:, :].unsqueeze(1).unsqueeze(3).broadcast_to([128, L, 4, 32]), op=ALU.mult),
                 reads=[t_c], writes=[t_BD[T], tpb])
            for s_ in range(L):
                pb2 = pk[ev_cnt % 4]
                tpb2 = t_pk[ev_cnt % 4]
                ev_cnt += 1
                n_tot = (s_ + 1) + 8
                n_i = 0
                for tp in range(s_ + 1):
                    c.op("pe", lambda e, T=T, tp=tp, s_=s_, pb2=pb2, n_i=n_i: e.matmul(
                        pb2[:, 0:NCH], BD[:, T, s_ - tp, :], uT[:, T, tp::L], start=(n_i == 0), stop=False),
                         reads=[t_BD[T], t_u[T]], writes=[tpb2], signal=False)
                    n_i += 1
                for j in range(4):
                    for ri in range(2):
                        last = (n_i == n_tot - 1)
                        c.op("pe", lambda e, T=T, j=j, ri=ri, s_=s_, pb2=pb2, last=last: e.matmul(
                            pb2[32 * j:32 * j + 32, 0:NCH], Readb[:, s_ + 1, ri, 4 * T + j, :], Sinb[:, ri, 4 * T + j, :],
                            start=False, stop=(ri == 1), tile_position=(0, 32 * j)),
                             reads=[t_rd, t_sin], writes=[tpb2], signal=last)
                        n_i += 1
                if s_ % 2 == 0:
                    c.op("act", lambda e, s_=s_, pb2=pb2: e.copy(out=yraw[:, s_:SEQ:L], in_=pb2[:, 0:NCH]), reads=[],
                         writes=[t_yraw, tpb2])
                else:
                    c.op("dve", lambda e, s_=s_, pb2=pb2: e.tensor_copy(out=yraw[:, s_:SEQ:L], in_=pb2[:, 0:NCH]), reads=[],
                         writes=[t_yraw, tpb2])
            c.op("dve", lambda e, T=T: e.scalar_tensor_tensor(out=yraw, in0=uT[:, T, :], scalar=dcol[:, T:T + 1],
                                                              in1=yraw, op0=ALU.mult, op1=ALU.add),
                 reads=[t_u[T], t_c], writes=[t_yraw])
            c.op("pool", lambda e: e.tensor_tensor(out=gtmp, in0=yraw, in1=yraw, op=ALU.mult), reads=[t_yraw],
                 writes=[t_g])
            c.op("pool", lambda e: e.tensor_scalar(out=gtmp, in0=gtmp, scalar1=0.044715, scalar2=1.0, op0=ALU.mult,
                                                   op1=ALU.add), reads=[], writes=[t_g])
            c.op("dve", lambda e: e.tensor_tensor(out=gtmp, in0=gtmp, in1=yraw, op=ALU.mult), reads=[t_yraw],
                 writes=[t_g])
            c.op("act", lambda e: e.activation(out=gtmp, in_=gtmp, func=AF.Sigmoid, scale=GC), reads=[], writes=[t_g])
            c.op("dve", lambda e, T=T: e.tensor_tensor(out=yT[:, T, :], in0=gtmp, in1=yraw, op=ALU.mult),
                 reads=[t_g, t_yraw], writes=t_yT)
        t_sg = [Tok() for _ in range(2)]
        for nb in range(4):
            toks = t_yT[nb * 4:nb * 4 + 4]
            for m in range(4):
                for k in range(4):
                    c.op("pe", lambda e, m=m, k=k, nb=nb: e.matmul(pk[m][:, :], wg[:, k, m * 128:(m + 1) * 128],
                                                                   yT[:, k, nb * 512:(nb + 1) * 512], start=(k == 0),
                                                                   stop=(k == 3)),
                         reads=[t_wg] + toks, writes=[t_pk[m]], signal=(k == 3))
            for m in range(4):
                c.op("act", lambda e, m=m: e.activation(out=sgb[:, m % 2, :], in_=pk[m][:, :], func=AF.Sigmoid), reads=[],
                     writes=[t_sg[m % 2], t_pk[m]])
                c.op("dve", lambda e, m=m, nb=nb: e.tensor_tensor(out=yT[:, m, nb * 512:(nb + 1) * 512],
                                                                  in0=yT[:, m, nb * 512:(nb + 1) * 512], in1=sgb[:, m % 2, :],
                                                                  op=ALU.mult), reads=[t_sg[m % 2]], writes=toks)
    c.barrier()


Builder.phase_ssm = _phase_ssm


def _build_test_ssm(self):
    self.declare_inputs()
    self.declare_ssm_inputs()
    self.setup_consts()
    self.phase_A(0, self.x_in)
    self.yT = [self.sb("yT%d" % i, [128, 4, SEQ], BF16).__enter__() for i in range(3)]
    self.t_yT = [[Tok() for _ in range(NT)] for _ in range(3)]
    self.phase_ssm(0, self.yT[0], self.t_yT[0])
    self.dbg_dump("y_ssm", self.yT[0][:], [128, 4, SEQ], self.t_yT[0], BF16)
    self.finish()
    return self.nc


Builder.build_test_ssm = _build_test_ssm


def host_ssm_layouts(inputs):
    Lr = np.asarray(inputs["ssm_a_re"]).shape[0]
    a_re, a_im, ldt = (np.asarray(inputs[k], np.float32) for k in ("ssm_a_re", "ssm_a_im", "ssm_log_dt"))
    b_re, b_im = np.asarray(inputs["ssm_b_re"], np.float32), np.asarray(inputs["ssm_b_im"], np.float32)
    c_re, c_im = np.asarray(inputs["ssm_c_re"], np.float32), np.asarray(inputs["ssm_c_im"], np.float32)
    sp = np.zeros((Lr, 128, 3, 16), np.float32)
    bb = np.zeros((Lr, 128, 2, 16, 32), np.float32)
    cc = np.zeros((Lr, 128, 2, 16, 32), np.float32)
    for g2 in range(2):
        rows = slice(g2 * 64, g2 * 64 + 64)
        cols = slice(g2 * 16, g2 * 16 + 16)
        sp[:, rows, 0, :] = a_re[:, g2::2, :].transpose(0, 2, 1)
        sp[:, rows, 1, :] = a_im[:, g2::2, :].transpose(0, 2, 1)
        sp[:, rows, 2, :] = ldt[:, None, g2::2]
        bb[:, rows, 0, :, cols] = b_re[:, g2::2].transpose(0, 2, 1, 3)
        bb[:, rows, 1, :, cols] = b_im[:, g2::2].transpose(0, 2, 1, 3)
        cc[:, rows, 0, :, cols] = c_re[:, g2::2].transpose(0, 3, 1, 2)
        cc[:, rows, 1, :, cols] = c_im[:, g2::2].transpose(0, 3, 1, 2)
    dT = np.ascontiguousarray(np.asarray(inputs["ssm_d"], np.float32).reshape(Lr, 4, 128).transpose(0, 2, 1))
    return {"ssm_sp": sp, "ssm_b": bb, "ssm_c": cc, "ssm_dT": dT}


def _build_full(self, depth=DEPTH):
    self.declare_inputs()
    self.declare_mlstm_inputs()
    self.declare_ssm_inputs()
    self.setup_consts()
    self.setup_moba_consts()
    src = self.x_in
    for l in range(depth):
        last = (l == depth - 1)
        self.ssm_next = None
        if l == 0:
            self.phase_A(l, src)
        y_cms = [self.nc.sbuf_tensor("yT%d_l%d" % (i, l), [128, 4, SEQ], BF16, side="right") for i in range(3)]
        self.yT = [cm.__enter__() for cm in y_cms]
        self.t_yT = [[Tok() for _ in range(NT)] for _ in range(3)]
        self.phase_ssm(l, self.yT[0], self.t_yT[0])
        self.phase_mlstm(l, self.yT[1], self.t_yT[1])
        self.phase_moba(l, self.yT[2], self.t_yT[2])
        with self.sb("D_mT", [128, 8, SEQ], BF16) as mT:
            self.mT = mT
            self.phase_D1(l)
            for cm in reversed(y_cms):
                cm.__exit__(None, None, None)
            self.phase_D2(l, src, self.xres)
        dst = self.out if last else self.xres2
        self.phase_E(l, self.xres, dst, next_gain=None if last else self.n_mix_pre[l + 1])
        src = self.xres2
    self.finish()
    return self.nc


Builder.build_full = _build_full

_CACHE = {}


def kernel(**inputs):
    if "b" not in _CACHE:
        b = Builder()
        b.build_full()
        _CACHE["b"] = b
    b = _CACHE["b"]
    ssm = host_ssm_layouts(inputs)
    n = 8
    in_maps = [make_in_map(b, inputs, core, ssm=ssm) for core in range(n)]
    res = run_bass_kernel_spmd(b.nc, in_maps, core_ids=list(range(n)))
    out = np.stack([np.asarray(res.results[i]["out"]) for i in range(n)], axis=0)
    return out.astype(np.float32, copy=False)


def _build_test_D(self):
    c = self.c
    self.declare_inputs()
    self.setup_consts()
    self.phase_A(0, self.x_in)
    with ExitStack() as es:
        self.yT = [es.enter_context(self.sb("yT%d" % i, [128, 4, SEQ], BF16)) for i in range(3)]
        self.t_yT = [[Tok() for _ in range(NT)] for _ in range(3)]
        for i in range(3):
            c.op("pool", lambda e, i=i: e.memset(self.yT[i][:], 0.0), writes=self.t_yT[i])
        self.phase_D(0, self.x_in, self.out)
    self.finish()
    return self.nc


Builder.build_test_D = _build_test_D


def _build_test_D_setup(self):
    c = self.c
    self.declare_inputs()
    self.declare_ssm_inputs()
    self.setup_consts()
    self.phase_A(0, self.x_in)
    self.ssm_next = 1
    with ExitStack() as es:
        self.yT = [es.enter_context(self.nc.sbuf_tensor("yT%d" % i, [128, 4, SEQ], BF16, side="right")) for i in range(3)]
        self.t_yT = [[Tok() for _ in range(NT)] for _ in range(3)]
        for i in range(3):
            c.op("pool", lambda e, i=i: e.memset(self.yT[i][:], 0.0), writes=self.t_yT[i])
        with self.sb("D_mT", [128, 8, SEQ], BF16) as mT:
            self.mT = mT
            self.phase_D1(0)
    self.finish()
    return self.nc


Builder.build_test_D_setup = _build_test_D_setup
```

```python
import math
from contextlib import ExitStack
import numpy as np
import concourse.bass as bass
import concourse.mybir as mybir
from concourse.bass_utils import run_bass_kernel_spmd

F32 = mybir.dt.float32
BF16 = mybir.dt.bfloat16
AF = mybir.ActivationFunctionType
ALU = mybir.AluOpType
AX = mybir.AxisListType

D_MODEL = 1024
SEQ = 2048
DEPTH = 4
NT = SEQ // 128
D_FF = 4096
IN_COLS = 7176
RMS_EPS = 1e-6
SSM_L = 16
NCH = SEQ // SSM_L

C_U = 0
C_MQ, C_MK, C_MV, C_MO = 512, 1024, 1536, 2048
C_MI, C_MF = 2560, 2564
C_AQ, C_AK, C_AV = 2568, 3080, 3592
C_G = 4104


class Ev:
    __slots__ = ("key", "val", "snap")

    def __init__(self, key, val, snap):
        self.key, self.val, self.snap = key, val, snap


class Tok:
    __slots__ = ("w", "r", "name")

    def __init__(self, name=""):
        self.w = None
        self.r = {}
        self.name = name


class Eng:
    def __init__(self, ctx, key, eng, is_pe=False):
        self.ctx, self.key, self.eng, self.is_pe = ctx, key, eng, is_pe
        self.sem = ctx.nc.semaphore("sem_" + key).__enter__()
        self.count = 0
        self.seen = {}
        self.pending = False

    def need(self, ev):
        if ev is None:
            return
        if ev.key == self.key:
            if self.is_pe:
                return
            if ev.val < self.count - 1:
                return
            if self.seen.get(self.key, 0) >= ev.val:
                return
            self.eng.wait_ge(self.sem, ev.val)
            self.seen[self.key] = ev.val
            return
        if self.seen.get(ev.key, 0) >= ev.val:
            return
        self.eng.wait_ge(self.ctx.sems[ev.key], ev.val)
        s = self.seen
        for k, v in ev.snap.items():
            if s.get(k, 0) < v:
                s[k] = v
        s[ev.key] = max(s.get(ev.key, 0), ev.val)


class Ctx:
    def __init__(self, nc, n_dma_sems=24):
        self.nc = nc
        self.sems = {}
        self.E = {}
        for key, eng, is_pe in (("pe", nc.tensor, True), ("dve", nc.vector, False), ("act", nc.scalar, False),
                                ("pool", nc.gpsimd, False), ("sp", nc.sync, False)):
            e = Eng(self, key, eng, is_pe)
            self.E[key] = e
            self.sems[key] = e.sem
        self.dpool = {}
        for q in ("sp", "pool", "act"):
            lst = []
            for i in range(n_dma_sems if q != "act" else 8):
                k = "d_%s_%d" % (q, i)
                s = nc.semaphore(k).__enter__()
                self.sems[k] = s
                lst.append([k, s, 0])
            self.dpool[q] = [lst, 0]

    def op(self, engname, fn, reads=(), writes=(), signal=True):
        E = self.E[engname]
        for t in reads:
            E.need(t.w)
        for t in writes:
            E.need(t.w)
            for ev in t.r.values():
                E.need(ev)
        ins = fn(E.eng)
        if signal:
            E.count += 1
            ins.then_inc(E.sem, 1)
            val = E.count
        else:
            val = E.count + 1
        snap = dict(E.seen)
        ev = Ev(E.key, val, snap)
        for t in reads:
            t.r[ev.key] = ev
        for t in writes:
            t.w = ev
            t.r = {}
        return ev

    def dma(self, q, out, in_, reads=(), writes=()):
        E = self.E[q]
        for t in reads:
            E.need(t.w)
        for t in writes:
            E.need(t.w)
            for ev in t.r.values():
                E.need(ev)
        lst, idx = self.dpool[q]
        ent = lst[idx % len(lst)]
        self.dpool[q][1] = idx + 1
        if ent[2] > 0:
            E.need(Ev(ent[0], ent[2], {}))
        ent[2] += 16
        E.eng.dma_start(out=out, in_=in_).then_inc(ent[1], 16)
        ev = Ev(ent[0], ent[2], dict(E.seen))
        for t in reads:
            t.r[ev.key] = ev
        for t in writes:
            t.w = ev
            t.r = {}
        return ev

    def barrier(self):
        evs = []
        for k, e in self.E.items():
            if e.count > 0:
                evs.append(Ev(k, e.count, {}))
        for q, (lst, _) in self.dpool.items():
            for ent in lst:
                if ent[2] > 0:
                    evs.append(Ev(ent[0], ent[2], {}))
        for k, e in self.E.items():
            for ev in evs:
                if ev.key != k:
                    e.need(ev)
                else:
                    if not e.is_pe and e.seen.get(k, 0) < ev.val:
                        e.eng.wait_ge(e.sem, ev.val)
                        e.seen[k] = ev.val


class Builder:
    def __init__(self, depth=DEPTH, debug=None, phases=None):
        self.depth = depth
        self.debug = debug or {}
        self.phases = phases
        nc = bass.Bass("TRN2", target_bir_lowering=False)
        self.nc = nc
        self.c = Ctx(nc)
        self.din = {}
        self.dbg_out = {}

    def inp(self, name, shape, dt=F32):
        t = self.nc.dram_tensor(name, list(shape), dt, kind="ExternalInput").ap()
        self.din[name] = t
        return t

    def sb(self, name, shape, dt):
        self.uid = getattr(self, "uid", 0) + 1
        return self.nc.sbuf_tensor("%s_u%d" % (name, self.uid), list(shape), dt)

    def ps(self, name, shape, dt=F32):
        self.uid = getattr(self, "uid", 0) + 1
        return self.nc.psum_tensor("%s_u%d" % (name, self.uid), list(shape), dt)

    def dbg_dump(self, name, src_ap, shape, reads, dt=F32):
        t = self.nc.dram_tensor("dbg_" + name, list(shape), dt, kind="ExternalOutput").ap()
        self.dbg_out[name] = t
        ev = self.c.dma("sp", t, src_ap, reads=reads)
        self.c.E["sp"].need(ev)
        return t

    def rstd_from_ss(self, ss2, rstd, tok_ss, tok_rstd, ncols=2):
        c = self.c
        if ncols == 2:
            c.op("dve", lambda e: e.tensor_tensor(out=rstd, in0=ss2[:, 0:1], in1=ss2[:, 1:2], op=ALU.add),
                 reads=[tok_ss], writes=[tok_rstd])
            src = rstd
        else:
            src = ss2[:, 0:1]
        c.op("act", lambda e: e.activation(out=rstd, in_=src, func=AF.Sqrt, scale=1.0 / D_MODEL, bias=self.eps_ap),
             reads=[tok_rstd, tok_ss], writes=[tok_rstd])
        c.op("dve", lambda e: e.reciprocal(out=rstd, in_=rstd), reads=[tok_rstd], writes=[tok_rstd])

    def declare_inputs(self):
        L = DEPTH
        inp = self.inp
        self.x_in = inp("x", [SEQ, D_MODEL])
        self.w_in = inp("w_in", [L, D_MODEL, IN_COLS])
        self.conv_w = inp("conv_w", [L, 4, 1024])
        self.w_glu = inp("ssm_w_glu", [L, 512, 512])
        self.w_sp = inp("w_ssm_proj", [L, 512, D_MODEL])
        self.w_mp = inp("w_mlstm_proj", [L, 512, D_MODEL])
        self.w_ap = inp("w_moba_proj", [L, 512, D_MODEL])
        self.w_out = inp("w_out", [L, D_MODEL, D_MODEL])
        self.w_ff1 = inp("w_ff1", [L, D_MODEL, D_FF])
        self.w_ff2 = inp("w_ff2", [L, D_FF, D_MODEL])
        self.n_mix_pre = inp("norm_mix_pre", [L, D_MODEL])
        self.n_mix_post = inp("norm_mix_post", [L, D_MODEL])
        self.n_ffn_pre = inp("norm_ffn_pre", [L, D_MODEL])
        self.n_ffn_post = inp("norm_ffn_post", [L, D_MODEL])
        self.ident_in = inp("ident", [128, 128])
        self.out = self.nc.dram_tensor("out", [SEQ, D_MODEL], F32, kind="ExternalOutput").ap()
        self.xres = self.nc.dram_tensor("xres", [SEQ, D_MODEL], F32).ap()
        self.xres2 = self.nc.dram_tensor("xres2", [SEQ, D_MODEL], F32).ap()

    def setup_consts(self):
        c = self.c
        nc = self.nc
        self.ident_f = self.sb("ident_f", [128, 128], F32).__enter__()
        self.ident_b = self.sb("ident_b", [128, 128], BF16).__enter__()
        self.eps_t = self.sb("eps_t", [128, 1], F32).__enter__()
        self.eps_ap = self.eps_t[:, 0:1]
        self.t_const = Tok("const")
        c.dma("sp", self.ident_f[:], self.ident_in[:, :], writes=[self.t_const])
        c.op("dve", lambda e: e.tensor_copy(out=self.ident_b[:], in_=self.ident_f[:]), reads=[self.t_const],
             writes=[self.t_const])
        c.op("dve", lambda e: e.memset(self.eps_t[:], RMS_EPS), writes=[self.t_const])
        self.hT = self.sb("hT", [128, 8, SEQ], BF16).__enter__()
        self.t_hT = [Tok("hT%d" % i) for i in range(NT)]

    def load_gain(self, dst, src_row, tok):
        self.c.dma("sp", dst, src_row.partition_broadcast(128), writes=[tok])

    def norm_to_hT(self, xt, t_x, gain, t_gain, tt, work):
        self.norm_part1(xt, t_x, gain, t_gain, tt, work)
        self.norm_part2(tt, work)

    def norm_part1(self, xt, t_x, gain, t_gain, tt, work):
        c = self.c
        junk, t_junk, ss, t_ss, rstd, t_rstd, hb, t_hb, pst, t_pst = work
        c.op("act", lambda e: e.activation(out=junk, in_=xt, func=AF.Square, accum_out=ss[:, 0:1]),
             reads=[t_x], writes=[t_junk, t_ss])
        self.rstd_from_ss(ss, rstd, t_ss, t_rstd, ncols=1)
        c.op("dve", lambda e: e.scalar_tensor_tensor(out=hb, in0=xt, scalar=rstd, in1=gain, op0=ALU.mult,
                                                     op1=ALU.mult),
             reads=[t_x, t_rstd, t_gain], writes=[t_hb])

    def norm_part2(self, tt, work):
        c = self.c
        junk, t_junk, ss, t_ss, rstd, t_rstd, hb, t_hb, pst, t_pst = work
        for k in range(8):
            c.op("pe", lambda e, k=k: e.transpose(pst[:, k * 128:(k + 1) * 128], hb[:, k * 128:(k + 1) * 128],
                                                  self.ident_b[:]),
                 reads=[t_hb, self.t_const], writes=[t_pst], signal=(k == 7))
        c.op("act", lambda e: e.copy(out=self.hT[:, :, tt * 128:(tt + 1) * 128],
                                     in_=pst.rearrange("p (k t) -> p k t", k=8)),
             reads=[], writes=[self.t_hT[tt], t_pst])

    def phase_A(self, l, src):
        c = self.c
        with ExitStack() as es:
            S = lambda n, sh, dt: es.enter_context(self.sb(n, sh, dt))
            P = lambda n, sh, dt=F32: es.enter_context(self.ps(n, sh, dt))
            xb = S("A_x", [128, 2, 1024], F32)
            g = S("A_g", [128, 1024], F32)
            junk = S("A_junk", [128, 1024], BF16)
            ss = S("A_ss", [128, 2, 2], F32)
            rstd = S("A_rstd", [128, 2, 1], F32)
            hb = S("A_hb", [128, 2, 1024], BF16)
            pst0 = P("A_pst0", [128, 1024], BF16)
            pst1 = P("A_pst1", [128, 1024], BF16)
            t_g = Tok()
            self.load_gain(g[:], self.n_mix_pre[l], t_g)
            t_x = [Tok(), Tok()]
            t_junk = Tok()
            t_ss = [Tok(), Tok()]
            t_rstd = [Tok(), Tok()]
            t_hb = [Tok(), Tok()]
            t_pst = [Tok(), Tok()]
            psts = [pst0, pst1]
            prev = None
            for tt in range(NT):
                b = tt % 2
                c.dma("sp", xb[:, b, :], src[tt * 128:(tt + 1) * 128, :], writes=[t_x[b]])
                work = (junk[:], t_junk, ss[:, b, :], t_ss[b], rstd[:, b, :], t_rstd[b], hb[:, b, :], t_hb[b],
                        psts[b][:], t_pst[b])
                self.norm_part1(xb[:, b, :], t_x[b], g[:], t_g, tt, work)
                if prev is not None:
                    self.norm_part2(*prev)
                prev = (tt, work)
            self.norm_part2(*prev)
        c.barrier()

    def phase_E(self, l, src, dst, next_gain=None):
        c = self.c
        TB = 256
        with ExitStack() as es:
            S = lambda n, sh, dt: es.enter_context(self.sb(n, sh, dt))
            P = lambda n, sh, dt=F32: es.enter_context(self.ps(n, sh, dt))
            f1 = S("E_f1", [128, 32, TB], BF16)
            rbuf = S("E_r", [128, 4, TB], F32)
            xb = S("E_x", [128, 2, 1024], F32)
            yb = S("E_y", [128, 1024], F32)
            g = S("E_g", [128, 1024], F32)
            junk = S("E_junk", [128, 1024], BF16)
            g2 = S("E_g2", [128, 1024], F32)
            hb = S("E_hb", [128, 1024], BF16)
            ss2 = S("E_ss2", [128, 2, 2], F32)
            rstd2 = S("E_rstd2", [128, 2, 1], F32)
            ss = S("E_ss", [128, 2, 2], F32)
            rstd = S("E_rstd", [128, 2, 1], F32)
            p0, p1, p2 = [P("E_p%d" % i, [128, 512]) for i in range(3)]
            pstE = P("E_pst", [128, 1024], BF16)
            t_pstE = Tok()
            t_g2, t_hbE = Tok(), Tok()
            t_ss2 = [Tok(), Tok()]
            t_rstd2 = [Tok(), Tok()]
            prevE = None
            if next_gain is not None:
                self.load_gain(g2[:], next_gain, t_g2)
            q0, q1, q2, q3 = [P("E_q%d" % i, [128, 512]) for i in range(4)]
            t_g = Tok()
            self.load_gain(g[:], self.n_ffn_post[l], t_g)
            if getattr(self, "w1", None) is None:
                self.ffn_prefetch(l, first=True)
            self.ffn_prefetch(l, first=False)
            w1, t_w1, t_w2 = self.w1, self.t_w1, self.t_w2
            w2a, w2b = self.w2a, self.w2b
            pb = [p0, p1, p2]
            t_pb = [Tok() for _ in range(3)]
            qb = [[q0, q1], [q2, q3]]
            t_qb = [[Tok(), Tok()], [Tok(), Tok()]]
            t_f1 = [Tok() for _ in range(32)]
            t_r = [Tok() for _ in range(3)]
            t_x = [Tok(), Tok()]
            t_y = Tok()
            t_junk = Tok()
            t_ss = [Tok(), Tok()]
            t_rstd = [Tok(), Tok()]
            for tb in range(SEQ // TB):
                tsl = slice(tb * TB, (tb + 1) * TB)
                for j in range(32):
                    pi = j % 3
                    for k in range(8):
                        c.op("pe", lambda e, k=k, j=j, pi=pi: e.matmul(pb[pi][:, 0:TB], w1[:, k, j * 128:(j + 1) * 128],
                                                                       self.hT[:, k, tsl], start=(k == 0), stop=(k == 7)),
                             reads=[t_w1[j // 4]] + self.t_hT[tb * 2:tb * 2 + 2], writes=[t_pb[pi]], signal=(k == 7))
                    c.op("act", lambda e, pi=pi: e.activation(out=rbuf[:, pi, :], in_=pb[pi][:, 0:TB], func=AF.Relu),
                         reads=[t_pb[pi]], writes=[t_r[pi]])
                    c.op("dve", lambda e, pi=pi, j=j: e.tensor_tensor(out=f1[:, j, :], in0=rbuf[:, pi, :],
                                                                      in1=rbuf[:, pi, :], op=ALU.mult),
                         reads=[t_r[pi]], writes=[t_f1[j]])
                for ti in range(TB // 128):
                    tt = tb * (TB // 128) + ti
                    b = tt % 2
                    c.dma("sp", xb[:, b, :], src[tt * 128:(tt + 1) * 128, :], writes=[t_x[b]])
                    for hf in range(2):
                        for j in range(32):
                            c.op("pe", lambda e, j=j, hf=hf, b=b, ti=ti: e.matmul(
                                qb[b][hf][:, :], f1[:, j, ti * 128:(ti + 1) * 128],
                                (w2a if j < 16 else w2b)[:, j % 16, hf * 512:(hf + 1) * 512],
                                start=(j == 0), stop=(j == 31)),
                                 reads=[t_f1[j], t_w2[j // 4]], writes=[t_qb[b][hf]], signal=(j == 31))
                        c.op("act", lambda e, hf=hf, b=b: e.activation(out=junk[:, 0:512], in_=qb[b][hf][:, :], func=AF.Square,
                                                                       accum_out=ss[:, b, hf:hf + 1]),
                             reads=[t_qb[b][hf]], writes=[t_junk, t_ss[b]])
                    if prevE is not None:
                        self.norm_part2(*prevE)
                        prevE = None
                    self.rstd_from_ss(ss[:, b, :], rstd[:, b, :], t_ss[b], t_rstd[b], ncols=2)
                    for hf in range(2):
                        c.op("dve", lambda e, hf=hf, b=b: e.scalar_tensor_tensor(
                            out=yb[:, hf * 512:(hf + 1) * 512], in0=qb[b][hf][:, :], scalar=rstd[:, b, :],
                            in1=g[:, hf * 512:(hf + 1) * 512], op0=ALU.mult, op1=ALU.mult),
                             reads=[t_qb[b][hf], t_rstd[b], t_g], writes=[t_y])
                    c.op("dve", lambda e, b=b: e.tensor_tensor(out=xb[:, b, :], in0=xb[:, b, :], in1=yb[:],
                                                               op=ALU.add),
                         reads=[t_y, t_x[b]], writes=[t_x[b]])
                    c.dma("sp", dst[tt * 128:(tt + 1) * 128, :], xb[:, b, :], reads=[t_x[b]])
                    if next_gain is not None:
                        work = (junk[:], t_junk, ss2[:, b, :], t_ss2[b], rstd2[:, b, :], t_rstd2[b], hb[:], t_hbE,
                                pstE[:], t_pstE)
                        self.norm_part1(xb[:, b, :], t_x[b], g2[:], t_g2, tt, work)
                        prevE = (tt, work)
            if prevE is not None:
                self.norm_part2(*prevE)
        c.barrier()
        self.w2b_cm.__exit__(None, None, None)
        self.w2a_cm.__exit__(None, None, None)
        self.w1_cm.__exit__(None, None, None)
        self.w1 = None

    def finish(self):
        c = self.c
        c.barrier()

    def build_test_trunk(self):
        self.declare_inputs()
        self.setup_consts()
        self.phase_A(0, self.x_in)
        self.phase_E(0, self.x_in, self.out)
        self.finish()
        return self.nc

    def load_w(self, dst, src2d, tok, q="pool"):
        self.c.dma(q, dst, src2d.rearrange("(kt p) c -> p kt c", p=128), writes=[tok])

    def proj_fm(self, w, t_w, ncol_tiles, pbanks, t_pbanks, evac, nk=8, rhs=None, t_rhs=None):
        c = self.c
        rhs = self.hT if rhs is None else rhs
        cnt = 0
        for m in range(ncol_tiles):
            for nb in range(4):
                pi = cnt % len(pbanks)
                cnt += 1
                rt = self.t_hT[nb * 4:nb * 4 + 4] if t_rhs is None else t_rhs
                for k in range(nk):
                    c.op("pe", lambda e, k=k, m=m, nb=nb, pi=pi: e.matmul(
                        pbanks[pi][:, :], w[:, k, m * 128:(m + 1) * 128], rhs[:, k, nb * 512:(nb + 1) * 512],
                        start=(k == 0), stop=(k == nk - 1)),
                         reads=[t_w] + rt, writes=[t_pbanks[pi]], signal=(k == nk - 1))
                evac(m, nb, pbanks[pi], t_pbanks[pi])

    def proj_tm(self, w, t_w, col0, ncols, pbanks, t_pbanks, evac, nk=8, lhs=None, t_lhs=None):
        c = self.c
        lhs = self.hT if lhs is None else lhs
        for tt in range(NT):
            pi = tt % len(pbanks)
            lt = [self.t_hT[tt]] if t_lhs is None else t_lhs
            for k in range(nk):
                c.op("pe", lambda e, k=k, tt=tt, pi=pi: e.matmul(
                    pbanks[pi][:, 0:ncols], lhs[:, k, tt * 128:(tt + 1) * 128], w[:, k, col0:col0 + ncols],
                    start=(k == 0), stop=(k == nk - 1)),
                     reads=[t_w] + lt, writes=[t_pbanks[pi]], signal=(k == nk - 1))
            evac(tt, pbanks[pi], t_pbanks[pi])

    def setup_moba_consts(self):
        c = self.c
        nc = self.nc
        self.ohr_in = self.inp("ohr", [32, 384])
        self.causal_in = self.inp("causal_neg", [128, 128])
        self.rbT_in = self.inp("rel_biasT", [32, 8])
        self.T0m_d = nc.dram_tensor("T0m_d", [128, 8, 128], F32).ap()
        self.T1_d = nc.dram_tensor("T1_d", [128, 8, 128], F32).ap()
        self.c31 = self.sb("c31", [128, 8], F32).__enter__()
        t = self.t_const
        with ExitStack() as es:
            ohr = es.enter_context(self.sb("ohr_sb", [128, 384], F32))
            rbT = es.enter_context(self.sb("rbT_sb", [32, 8], F32))
            T0m = es.enter_context(self.sb("T0m_s", [128, 8, 128], F32))
            T1 = es.enter_context(self.sb("T1_s", [128, 8, 128], F32))
            caus = es.enter_context(self.sb("caus_s", [128, 128], F32))
            t1, t2 = Tok(), Tok()
            rbrep = es.enter_context(self.sb("rbrep_sb", [128, 8, 128], F32))
            vb = es.enter_context(self.sb("vb_sb", [128, 2, 384], F32))
            pp2 = es.enter_context(self.ps("bv_ps2", [128, 384], F32))
            ZS = 128 * 385 + 512
            self.zscr = nc.dram_tensor("zscr", [8, ZS], F32)
            c.op("dve", lambda e: e.memset(ohr[:], 0.0), writes=[t1])
            c.op("dve", lambda e: e.memset(rbrep[:], 0.0), writes=[t1])
            c.dma("sp", ohr[0:32, :], self.ohr_in[:, :], writes=[t1])
            c.dma("sp", rbT[:], self.rbT_in[:, :], writes=[t1])
            c.dma("sp", caus[:], self.causal_in[:, :], writes=[t])
            c.dma("sp", self.c31[:], self.rbT_in[31, :].partition_broadcast(128), writes=[t])
            c.op("dve", lambda e: e.tensor_copy(out=rbrep[0:32, :, :], in_=rbT[:, :].unsqueeze(2).broadcast_to([32, 8, 128])),
                 reads=[t1], writes=[t1])
            t_vb = [Tok(), Tok()]
            t3 = Tok()
            for h in range(8):
                c.op("pe", lambda e, h=h: e.matmul(pp2[:, :], rbrep[:, h, :], ohr[:, :], start=True, stop=True),
                     reads=[t1], writes=[t2])
                c.op("dve", lambda e, h=h: e.tensor_copy(out=vb[:, h % 2, :], in_=pp2[:, :]), reads=[t2],
                     writes=[t_vb[h % 2]])
                c.dma("sp", bass.AP(self.zscr, h * ZS, [[385, 128], [1, 384]]), vb[:, h % 2, :], reads=[t_vb[h % 2]],
                      writes=[t3])
            for h in range(8):
                src0 = bass.AP(self.zscr, h * ZS + 255, [[384, 128], [1, 128]])
                src1 = bass.AP(self.zscr, h * ZS + 127, [[384, 128], [1, 128]])
                c.dma("sp", T0m[:, h, :], src0, reads=[t3], writes=[t])
                c.dma("sp", T1[:, h, :], src1, reads=[t3], writes=[t])
            for h in range(8):
                c.op("dve", lambda e, h=h: e.tensor_tensor(out=T0m[:, h, :], in0=T0m[:, h, :],
                                                           in1=caus[:], op=ALU.add), reads=[t], writes=[t])
            c.dma("sp", self.T0m_d, T0m[:], reads=[t])
            c.dma("sp", self.T1_d, T1[:], reads=[t])
            c.barrier()

    def phase_moba(self, l, yT, t_yT):
        c = self.c
        NEG = -1e30
        with ExitStack() as es:
            S = lambda n, sh, dt: es.enter_context(self.sb(n, sh, dt))
            P = lambda n, sh, dt=F32: es.enter_context(self.ps(n, sh, dt))
            wq = S("M_wq", [128, 8, 512], BF16)
            wk = S("M_wk", [128, 8, 512], BF16)
            wv = S("M_wv", [128, 8, 512], BF16)
            aqT = S("M_aqT", [128, 4, SEQ], BF16)
            akT = S("M_akT", [128, 4, SEQ], BF16)
            av = S("M_av", [128, NT, 512], BF16)
            T0m = S("M_T0m", [128, 8, 128], F32)
            T1 = S("M_T1", [128, 8, 128], F32)
            kms = S("M_kms", [128, 4, 8], F32)
            kmBD = S("M_kmBD", [128, 4, 16], BF16)
            gpad = S("M_gpad", [128, 8, 8], F32)
            m8 = S("M_m8", [128, 8, 8], F32)
            selm = S("M_selm", [128, 8, 8, 8], F32)
            selb = S("M_selb", [128, 8, 8, 8], F32)
            lg = S("M_lg", [128, 2, SEQ], F32)
            pr = S("M_pr", [128, 3, SEQ], BF16)
            pT = S("M_pT", [128, 3, NT, 128], BF16)
            mx = S("M_mx", [128, 2, 2], F32)
            pmx = S("M_pmx", [128, 2, 16], F32)
            rs = S("M_rs", [128, 2, 8], F32)
            ytok = S("M_ytok", [128, 2, 512], BF16)
            sc = [P("M_sc%d" % i, [128, 512]) for i in range(4)]
            ptp = [P("M_ptp%d" % i, [128, 1024], BF16) for i in range(2)]
            pv = [P("M_pv%d" % i, [128, 512]) for i in range(2)]
            t_sc = [Tok() for _ in range(4)]
            t_ptp = [Tok() for _ in range(2)]
            t_pv = [Tok() for _ in range(2)]
            t_w = [Tok(), Tok(), Tok()]
            t_bias = Tok()
            self.load_w(wq[:], self.w_in[l][:, C_AQ:C_AQ + 512], t_w[0])
            self.load_w(wk[:], self.w_in[l][:, C_AK:C_AK + 512], t_w[1])
            self.load_w(wv[:], self.w_in[l][:, C_AV:C_AV + 512], t_w[2])
            c.dma("sp", T0m[:], self.T0m_d, writes=[t_bias])
            c.dma("sp", T1[:], self.T1_d, writes=[t_bias])
            t_aq = [Tok() for _ in range(4)]
            t_ak = [Tok() for _ in range(4)]
            t_av = [Tok() for _ in range(NT)]
            t_kms = Tok()

            def ev_q(m, nb, ps, tps):
                c.op("act", lambda e: e.mul(out=aqT[:, m, nb * 512:(nb + 1) * 512], in_=ps[:, :], mul=0.125),
                     reads=[tps], writes=[t_aq[nb]])

            def ev_k(m, nb, ps, tps):
                c.op("act", lambda e: e.copy(out=akT[:, m, nb * 512:(nb + 1) * 512], in_=ps[:, :]),
                     reads=[], writes=[t_ak[nb], tps])
                if self.debug.get("noreduce"):
                    return
                c.op("dve", lambda e: e.tensor_reduce(out=kms[:, m, nb * 2:nb * 2 + 2],
                                                      in_=ps[:, :].rearrange("p (b s) -> p b s", b=2),
                                                      op=ALU.add, axis=AX.X), reads=[], writes=[t_kms, tps])

            def ev_v(tt, ps, tps):
                c.op("dve", lambda e: e.tensor_copy(out=av[:, tt, :], in_=ps[:, 0:512]), reads=[tps], writes=[t_av[tt]])

            if not self.debug.get("noq"):
                self.proj_fm(wq, t_w[0], 4, sc, t_sc, ev_q)
            if not self.debug.get("nok"):
                self.proj_fm(wk, t_w[1], 4, sc, t_sc, ev_k)
            if not self.debug.get("nov"):
                self.proj_tm(wv, t_w[2], 0, 512, sc, t_sc, ev_v)
            if self.debug.get("moba_stop") == "proj":
                c.barrier()
                return
            t_km = Tok()
            c.op("pool", lambda e: e.memset(kmBD[:], 0.0), writes=[t_km])
            c.op("dve", lambda e: e.tensor_scalar(out=kmBD[0:64, :, 0:8], in0=kms[0:64, :, :], scalar1=1.0 / 256,
                                                  scalar2=None, op0=ALU.mult), reads=[t_kms], writes=[t_km])
            c.op("dve", lambda e: e.tensor_scalar(out=kmBD[64:128, :, 8:16], in0=kms[64:128, :, :], scalar1=1.0 / 256,
                                                  scalar2=None, op0=ALU.mult), reads=[t_kms], writes=[t_km])
            t_sel = Tok()
            t_gp = Tok()
            t_m8 = Tok()
            for i in range(8, NT):
                cur = i // 2
                gps = sc[i % 4]
                tg = t_sc[i % 4]
                for ct in range(4):
                    c.op("pe", lambda e, ct=ct, i=i, gps=gps: e.matmul(
                        gps[:, ct * 16:(ct + 1) * 16], aqT[:, ct, i * 128:(i + 1) * 128], kmBD[:, ct, :],
                        start=True, stop=True), reads=[t_aq[i // 4], t_km], writes=[tg], signal=(ct == 3))
                c.op("pool", lambda e: e.memset(gpad[:], NEG), writes=[t_gp])
                c.op("dve", lambda e, gps=gps, cur=cur: e.tensor_copy(
                    out=gpad[:, :, 0:cur], in_=gps[:, 0:64].rearrange("p (h n) -> p h n", h=8)[:, :, 0:cur]),
                     reads=[tg], writes=[t_gp])
                for h in range(8):
                    c.op("dve", lambda e, h=h: e.max(out=m8[:, h, :], in_=gpad[:, h, :]), reads=[t_gp], writes=[t_m8])
                c.op("dve", lambda e, i=i: e.tensor_tensor(out=selm[:, i - 8, :, :], in0=gpad[:],
                                                           in1=m8[:, :, 2:3].broadcast_to([128, 8, 8]), op=ALU.is_ge),
                     reads=[t_gp, t_m8], writes=[t_sel])
                c.op("dve", lambda e, i=i: e.tensor_scalar(out=selm[:, i - 8, :, :], in0=selm[:, i - 8, :, :],
                                                           scalar1=-NEG, scalar2=NEG, op0=ALU.mult, op1=ALU.add),
                     reads=[t_sel], writes=[t_sel])
                c.op("dve", lambda e, i=i: e.tensor_tensor(out=selb[:, i - 8, :, :], in0=selm[:, i - 8, :, :],
                                                           in1=self.c31[:, :].unsqueeze(2).broadcast_to([128, 8, 8]),
                                                           op=ALU.add), reads=[t_sel, self.t_const], writes=[t_sel])
            if self.debug.get("moba_stop") == "sel":
                c.barrier()
                return
            NB = 3
            t_lg = [Tok(), Tok()]
            t_pr = [Tok() for _ in range(NB)]
            t_pT = [Tok() for _ in range(NB)]
            t_mx = [Tok(), Tok()]
            t_rs = [Tok(), Tok()]
            t_yt = [Tok(), Tok()]
            cnts = {"sc": 0, "pt": 0}

            def stage_A(n, i, h):
                cur = i // 2
                nk = i + 1
                b = n % 2
                b3 = n % NB
                ib = i % 2
                ct, h2 = h // 2, h % 2
                rows = slice(h2 * 64, h2 * 64 + 64)
                nchunk = (nk * 128 + 511) // 512
                nslot = [0]
                for ch in range(nchunk):
                    k0 = ch * 512
                    kw = min(512, nk * 128 - k0)
                    pi = cnts["sc"] % 4
                    cnts["sc"] += 1
                    c.op("pe", lambda e, pi=pi, k0=k0, kw=kw: e.matmul(
                        sc[pi][:, 0:kw], aqT[rows, ct, i * 128:(i + 1) * 128], akT[rows, ct, k0:k0 + kw],
                        start=True, stop=True),
                         reads=[t_aq[i // 4], t_ak[ch]], writes=[t_sc[pi]])
                    kt0 = k0 // 128
                    j = kt0
                    while j < kt0 + kw // 128:
                        off = (j - kt0) * 128
                        if j == i:
                            c.op("dve", lambda e, pi=pi, off=off, j=j: e.tensor_tensor(
                                out=lg[:, b, j * 128:(j + 1) * 128], in0=sc[pi][:, off:off + 128], in1=T0m[:, h, :],
                                op=ALU.add), reads=[t_bias], writes=[t_lg[b], t_sc[pi]])
                            j += 1
                        elif j == i - 1:
                            if i % 2 == 1 or i < 8:
                                c.op("dve", lambda e, pi=pi, off=off, j=j: e.tensor_tensor(
                                    out=lg[:, b, j * 128:(j + 1) * 128], in0=sc[pi][:, off:off + 128],
                                    in1=T1[:, h, :], op=ALU.add), reads=[t_bias], writes=[t_lg[b], t_sc[pi]])
                            else:
                                c.op("dve", lambda e, pi=pi, off=off, j=j: e.scalar_tensor_tensor(
                                    out=lg[:, b, j * 128:(j + 1) * 128], in0=sc[pi][:, off:off + 128],
                                    scalar=selm[:, i - 8, h, cur - 1:cur], in1=T1[:, h, :], op0=ALU.add,
                                    op1=ALU.add), reads=[t_bias, t_sel], writes=[t_lg[b], t_sc[pi]])
                            j += 1
                        else:
                            n_ = j // 2
                            w = 128
                            if j % 2 == 0 and (j + 1) < kt0 + kw // 128 and (j + 1) < i - 1:
                                w = 256
                            scal = selb[:, i - 8, h, n_:n_ + 1] if i >= 8 else self.c31[:, h:h + 1]
                            slot = nslot[0]
                            nslot[0] += 1
                            c.op("dve", lambda e, pi=pi, off=off, j=j, w=w, scal=scal, slot=slot: e.tensor_scalar(
                                out=lg[:, b, j * 128:j * 128 + w], in0=sc[pi][:, off:off + w], scalar1=scal,
                                scalar2=None, op0=ALU.add, op1=ALU.max, accum_out=pmx[:, b, slot:slot + 1]),
                                 reads=[t_sel, self.t_const], writes=[t_lg[b], t_sc[pi], t_mx[b]])
                            j += w // 128
                L = nk * 128
                s0 = max(0, i - 1) * 128
                slot = nslot[0]
                c.op("dve", lambda e: e.tensor_reduce(out=pmx[:, b, slot:slot + 1], in_=lg[:, b, s0:L], axis=AX.X, op=ALU.max),
                     reads=[t_lg[b]], writes=[t_mx[b]])
                c.op("dve", lambda e: e.tensor_reduce(out=mx[:, b, 1:2], in_=pmx[:, b, 0:slot + 1], axis=AX.X, op=ALU.max,
                                                      negate=True),
                     reads=[], writes=[t_mx[b]])
                c.op("act", lambda e: e.activation(
                    out=pr[:, b3, 0:L], in_=lg[:, b, 0:L], func=AF.Exp, bias=mx[:, b, 1:2], scale=1.0,
                    accum_out=rs[:, ib, h:h + 1]), reads=[t_lg[b], t_mx[b]], writes=[t_pr[b3], t_rs[ib]])

            def stage_B(n, i, h):
                nk = i + 1
                b3 = n % NB
                for g0 in range(0, nk, 8):
                    gn = min(8, nk - g0)
                    pb = cnts["pt"] % 2
                    cnts["pt"] += 1
                    for jj in range(gn):
                        j = g0 + jj
                        c.op("pe", lambda e, pb=pb, jj=jj, j=j: e.transpose(
                            ptp[pb][:, jj * 128:(jj + 1) * 128], pr[:, b3, j * 128:(j + 1) * 128], self.ident_b[:]),
                             reads=[t_pr[b3], self.t_const], writes=[t_ptp[pb]], signal=(jj == gn - 1))
                    c.op("act", lambda e, pb=pb, gn=gn, g0=g0: e.copy(
                        out=pT[:, b3, g0:g0 + gn, :], in_=ptp[pb][:, 0:gn * 128].rearrange("p (j t) -> p j t", j=gn)),
                         reads=[], writes=[t_pT[b3], t_ptp[pb]])

            def stage_C(n, i, h):
                nk = i + 1
                b3 = n % NB
                pvb = pv[i % 2]
                t_pvb = t_pv[i % 2]
                ib = i % 2
                for j in range(nk):
                    c.op("pe", lambda e, j=j: e.matmul(
                        pvb[:, h * 64:(h + 1) * 64], pT[:, b3, j, :], av[:, j, h * 64:(h + 1) * 64],
                        start=(j == 0), stop=(j == nk - 1)),
                         reads=[t_pT[b3], t_av[j]], writes=[t_pvb], signal=(j == nk - 1))
                if h == 7:
                    c.op("dve", lambda e: e.reciprocal(out=rs[:, ib, :], in_=rs[:, ib, :]), reads=[t_rs[ib]],
                         writes=[t_rs[ib]])
                    c.op("dve", lambda e: e.tensor_tensor(
                        out=ytok[:, ib, :].rearrange("p (h d) -> p h d", h=8),
                        in0=pvb[:, :].rearrange("p (h d) -> p h d", h=8),
                        in1=rs[:, ib, :].unsqueeze(2).broadcast_to([128, 8, 64]), op=ALU.mult),
                         reads=[t_rs[ib]], writes=[t_yt[ib], t_pvb])
                    pb = cnts["pt"] % 2
                    cnts["pt"] += 1
                    for ct in range(4):
                        c.op("pe", lambda e, ct=ct, pb=pb: e.transpose(
                            ptp[pb][:, ct * 128:(ct + 1) * 128], ytok[:, ib, ct * 128:(ct + 1) * 128], self.ident_b[:]),
                             reads=[t_yt[ib], self.t_const], writes=[t_ptp[pb]], signal=(ct == 3))
                    c.op("act", lambda e, pb=pb: e.copy(out=yT[:, :, i * 128:(i + 1) * 128],
                                                        in_=ptp[pb][:, 0:512].rearrange("p (c t) -> p c t", c=4)),
                         reads=[], writes=[t_yT[i], t_ptp[pb]])

            its = [(i, h) for i in range(self.debug.get("moba_nq", NT)) for h in range(8)]
            N = len(its)
            for n in range(N + 2):
                if n < N:
                    stage_A(n, *its[n])
                if 1 <= n <= N:
                    stage_B(n - 1, *its[n - 1])
                if n >= 2:
                    stage_C(n - 2, *its[n - 2])
        c.barrier()

    def build_test_moba(self):
        self.declare_inputs()
        self.setup_consts()
        self.setup_moba_consts()
        self.phase_A(0, self.x_in)
        self.yT = [self.sb("yT%d" % i, [128, 4, SEQ], BF16).__enter__() for i in range(3)]
        self.t_yT = [[Tok() for _ in range(NT)] for _ in range(3)]
        self.phase_moba(0, self.yT[2], self.t_yT[2])
        self.dbg_dump("y_moba", self.yT[2][:], [128, 4, SEQ], self.t_yT[2], BF16)
        self.finish()
        return self.nc


def _t5_bucket_np(dist):
    dist = np.asarray(dist)
    max_exact = 16
    large = max_exact + (np.log(np.maximum(dist, 1).astype(np.float32) / max_exact)
                         / math.log(128 / max_exact) * (32 - max_exact)).astype(np.int32)
    large = np.minimum(large, 31)
    return np.where(dist < max_exact, dist, large)


def host_consts():
    cst = {}
    cst["ident"] = np.eye(128, dtype=np.float32)
    u = np.arange(384)
    dist = 255 - u
    ohr = np.zeros((32, 384), np.float32)
    valid = dist >= 0
    bk = _t5_bucket_np(np.maximum(dist, 0))
    ohr[bk[valid], u[valid]] = 1.0
    cst["ohr"] = ohr
    t = np.arange(128)[:, None]
    s = np.arange(128)[None, :]
    cst["causal_neg"] = np.where(s <= t, 0.0, -1e30).astype(np.float32)
    cst["triu"] = (s >= t).astype(np.float32)
    cst["bdmask"] = (np.arange(128)[:, None] // 32 == np.arange(4)[None, :]).astype(np.float32)
    return cst


def make_in_map(b, inputs, core, ssm=None):
    cst = host_consts()
    if any(n.startswith("ssm_") and n not in inputs for n in b.din):
        cst.update(ssm if ssm is not None else host_ssm_layouts(inputs))
    im = {}
    for name in b.din:
        if name == "x":
            im[name] = np.ascontiguousarray(inputs["x"][core])
        elif name == "rel_biasT":
            im[name] = np.ascontiguousarray(np.asarray(inputs["rel_bias"]).T)
        elif name == "conv_wT":
            im[name] = np.ascontiguousarray(np.asarray(inputs["conv_w"]).transpose(0, 2, 1))
        elif name in cst:
            im[name] = cst[name]
        else:
            im[name] = np.ascontiguousarray(inputs[name])
    return im


def _build_test_mconst(self):
    self.declare_inputs()
    self.setup_consts()
    self.setup_moba_consts()
    self.dbg_dump("T0m", self.T0m_d, [128, 8, 128], [])
    self.dbg_dump("T1", self.T1_d, [128, 8, 128], [])
    self.finish()
    return self.nc


Builder.build_test_mconst = _build_test_mconst


def _phase_D1(self, l):
    c = self.c
    yT, t_yT = self.yT, self.t_yT
    mT = self.mT
    with ExitStack() as es:
        S = lambda n, sh, dt: es.enter_context(self.sb(n, sh, dt))
        P = lambda n, sh, dt=F32: es.enter_context(self.ps(n, sh, dt))
        gw = S("D_gw", [128, 3, 8, 3, 128], BF16)
        pw = S("D_pw", [128, 3, 4, 3, 128], BF16)
        sg = S("D_sg", [128, 3, 512], BF16)
        acc = S("D_acc", [128, 3, 512], F32)
        pbk = [P("D_p%d" % i, [128, 512]) for i in range(6)]
        t_pbk = [Tok() for _ in range(6)]
        t_gw = [Tok(), Tok(), Tok()]
        t_pw = [Tok(), Tok(), Tok()]
        t_sg = [Tok() for _ in range(3)]
        t_acc = [Tok() for _ in range(3)]
        t_mT = [Tok() for _ in range(4)]
        self.t_mT = t_mT
        pws = [self.w_sp, self.w_mp, self.w_ap]
        pcnt = 0
        gen = None
        if getattr(self, "ssm_next", None) is not None:
            es_z = ExitStack()
            zb = [es_z.enter_context(self.ps("Z_p%d" % i, [128, 512])) for i in range(2)]
            gen = self.ssm_setup_gen(self.ssm_next, es_z, zb, [Tok(), Tok()])
        for dt in range(8):
            wb = dt % 3
            for b in range(3):
                c0 = C_G + b * 1024 + dt * 128
                self.load_w(gw[:, wb, :, b, :], self.w_in[l][:, c0:c0 + 128], t_gw[wb])
                self.load_w(pw[:, wb, :, b, :], pws[b][l][:, dt * 128:(dt + 1) * 128], t_pw[wb])
            for nb in range(4):
                tsl = slice(nb * 512, (nb + 1) * 512)
                for b in range(3):
                    pg = pcnt % 6
                    pp = (pcnt + 1) % 6
                    pcnt += 2
                    for k in range(8):
                        c.op("pe", lambda e, k=k, b=b, pg=pg, wb=wb: e.matmul(pbk[pg][:, :], gw[:, wb, k, b, :],
                                                                             self.hT[:, k, tsl], start=(k == 0),
                                                                             stop=(k == 7)),
                             reads=[t_gw[wb]] + self.t_hT[nb * 4:nb * 4 + 4], writes=[t_pbk[pg]], signal=(k == 7))
                    for k in range(4):
                        c.op("pe", lambda e, k=k, b=b, pp=pp, wb=wb: e.matmul(pbk[pp][:, :], pw[:, wb, k, b, :],
                                                                             yT[b][:, k, tsl], start=(k == 0),
                                                                             stop=(k == 3)),
                             reads=[t_pw[wb]] + t_yT[b][nb * 4:nb * 4 + 4], writes=[t_pbk[pp]], signal=(k == 3))
                    c.op("act", lambda e, b=b, pg=pg: e.activation(out=sg[:, b, :], in_=pbk[pg][:, :], func=AF.Sigmoid),
                         reads=[], writes=[t_sg[b], t_pbk[pg]])
                    c.op("dve", lambda e, b=b, pp=pp: e.tensor_tensor(out=acc[:, b, :], in0=pbk[pp][:, :],
                                                                      in1=sg[:, b, :], op=ALU.mult),
                         reads=[t_sg[b]], writes=[t_acc[b], t_pbk[pp]])
                c.op("dve", lambda e: e.tensor_tensor(out=acc[:, 0, :], in0=acc[:, 0, :], in1=acc[:, 1, :], op=ALU.add),
                     reads=[t_acc[1]], writes=[t_acc[0]])
                c.op("dve", lambda e, dt=dt: e.tensor_tensor(out=mT[:, dt, tsl], in0=acc[:, 0, :], in1=acc[:, 2, :],
                                                             op=ALU.add),
                     reads=[t_acc[0], t_acc[2]], writes=[t_mT[nb]])
                if gen is not None:
                    next(gen, None)
        if gen is not None:
            for _ in gen:
                pass
            c.barrier()
            es_z.close()
    c.barrier()


def _phase_D2(self, l, src, dst):
    c = self.c
    mT, t_mT = self.mT, self.t_mT
    with ExitStack() as es:
        S = lambda n, sh, dt: es.enter_context(self.sb(n, sh, dt))
        P = lambda n, sh, dt=F32: es.enter_context(self.ps(n, sh, dt))
        wout = S("D_wout", [128, 8, D_MODEL], BF16)
        xb = S("D_x", [128, 2, 1024], F32)
        yb = S("D_y", [128, 1024], F32)
        g1 = S("D_g1", [128, 1024], F32)
        g2 = S("D_g2", [128, 1024], F32)
        junk = S("D_junk", [128, 1024], BF16)
        ss = S("D_ss", [128, 2, 2], F32)
        rstd = S("D_rstd", [128, 2, 1], F32)
        ss2 = S("D_ss2", [128, 2, 2], F32)
        rstd2 = S("D_rstd2", [128, 2, 1], F32)
        hb = S("D_hb", [128, 2, 1024], BF16)
        pbk = [P("D2_p%d" % i, [128, 512]) for i in range(4)]
        t_pbk = [Tok() for _ in range(4)]
        pst = [P("D_pst%d" % i, [128, 1024], BF16) for i in range(2)]
        t_pst = [Tok(), Tok()]
        t_g1, t_g2, t_wout = Tok(), Tok(), Tok()
        self.load_gain(g1[:], self.n_mix_post[l], t_g1)
        self.load_gain(g2[:], self.n_ffn_pre[l], t_g2)
        self.load_w(wout[:], self.w_out[l], t_wout)
        self.ffn_prefetch(l, first=True)
        t_x = [Tok(), Tok()]
        t_y = Tok()
        t_junk = Tok()
        t_ss = [Tok(), Tok()]
        t_rstd = [Tok(), Tok()]
        t_ss2 = [Tok(), Tok()]
        t_rstd2 = [Tok(), Tok()]
        t_hb = [Tok(), Tok()]
        prevD = None
        for tt in range(NT):
            b = tt % 2
            c.dma("sp", xb[:, b, :], src[tt * 128:(tt + 1) * 128, :], writes=[t_x[b]])
            qq = [pbk[b * 2], pbk[b * 2 + 1]]
            t_qq = [t_pbk[b * 2], t_pbk[b * 2 + 1]]
            for hf in range(2):
                for k in range(8):
                    c.op("pe", lambda e, k=k, hf=hf, tt=tt, qq=qq: e.matmul(
                        qq[hf][:, :], mT[:, k, tt * 128:(tt + 1) * 128], wout[:, k, hf * 512:(hf + 1) * 512],
                        start=(k == 0), stop=(k == 7)),
                         reads=[t_mT[tt // 4], t_wout], writes=[t_qq[hf]], signal=(k == 7))
                c.op("act", lambda e, hf=hf, b=b, qq=qq: e.activation(out=junk[:, 0:512], in_=qq[hf][:, :], func=AF.Square,
                                                                     accum_out=ss[:, b, hf:hf + 1]),
                     reads=[], writes=[t_junk, t_ss[b], t_qq[hf]])
            self.rstd_from_ss(ss[:, b, :], rstd[:, b, :], t_ss[b], t_rstd[b], ncols=2)
            for hf in range(2):
                c.op("dve", lambda e, hf=hf, b=b, qq=qq: e.scalar_tensor_tensor(
                    out=yb[:, hf * 512:(hf + 1) * 512], in0=qq[hf][:, :], scalar=rstd[:, b, :],
                    in1=g1[:, hf * 512:(hf + 1) * 512], op0=ALU.mult, op1=ALU.mult),
                     reads=[t_rstd[b], t_g1], writes=[t_y, t_qq[hf]])
            c.op("dve", lambda e, b=b: e.tensor_tensor(out=xb[:, b, :], in0=xb[:, b, :], in1=yb[:], op=ALU.add),
                 reads=[t_y], writes=[t_x[b]])
            c.dma("sp", dst[tt * 128:(tt + 1) * 128, :], xb[:, b, :], reads=[t_x[b]])
            work = (junk[:], t_junk, ss2[:, b, :], t_ss2[b], rstd2[:, b, :], t_rstd2[b], hb[:, b, :], t_hb[b],
                    pst[b][:], t_pst[b])
            self.norm_part1(xb[:, b, :], t_x[b], g2[:], t_g2, tt, work)
            if prevD is not None:
                self.norm_part2(*prevD)
            prevD = (tt, work)
        self.norm_part2(*prevD)
    c.barrier()


def _ffn_prefetch(self, l, first):
    c = self.c
    w2v = self.w_ff2[l].rearrange("(jt p) f -> p jt f", p=128)
    if first:
        self.w1_cm = self.nc.sbuf_tensor("E_w1_l%d" % l, [128, 8, D_FF], BF16, side="right")
        self.w1 = self.w1_cm.__enter__()
        self.w2a_cm = self.nc.sbuf_tensor("E_w2a_l%d" % l, [128, 16, D_MODEL], BF16, side="right")
        self.w2a = self.w2a_cm.__enter__()
        self.t_w1 = [Tok() for _ in range(8)]
        self.t_w2 = [Tok() for _ in range(8)]
        w1v = self.w_ff1[l].rearrange("(kt p) f -> p kt f", p=128)
        for cb in range(8):
            c.dma("pool", self.w1[:, :, cb * 512:(cb + 1) * 512], w1v[:, :, cb * 512:(cb + 1) * 512],
                  writes=[self.t_w1[cb]])
        for k in range(4):
            c.dma("pool", self.w2a[:, 4 * k:4 * k + 4, :], w2v[:, 4 * k:4 * k + 4, :], writes=[self.t_w2[k]])
    else:
        self.w2b_cm = self.nc.sbuf_tensor("E_w2b_l%d" % l, [128, 16, D_MODEL], BF16, side="right")
        self.w2b = self.w2b_cm.__enter__()
        for k in range(4, 8):
            c.dma("pool", self.w2b[:, 4 * (k - 4):4 * (k - 4) + 4, :], w2v[:, 4 * k:4 * k + 4, :], writes=[self.t_w2[k]])


Builder.ffn_prefetch = _ffn_prefetch
Builder.phase_D1 = _phase_D1
Builder.phase_D2 = _phase_D2


def _phase_D(self, l, src, dst):
    with self.sb("D_mT", [128, 8, SEQ], BF16) as mT:
        self.mT = mT
        self.phase_D1(l)
        self.phase_D2(l, src, dst)


Builder.phase_D = _phase_D


def _build_test_mixD(self):
    c = self.c
    self.declare_inputs()
    self.setup_consts()
    self.setup_moba_consts()
    self.phase_A(0, self.x_in)
    with ExitStack() as es:
        self.yT = [es.enter_context(self.sb("yT%d" % i, [128, 4, SEQ], BF16)) for i in range(3)]
        self.t_yT = [[Tok() for _ in range(NT)] for _ in range(3)]
        tz = Tok()
        for i in range(2):
            c.op("pool", lambda e, i=i: e.memset(self.yT[i][:], 0.0), writes=self.t_yT[i])
        self.phase_moba(0, self.yT[2], self.t_yT[2])
        self.phase_D(0, self.x_in, self.out)
    self.finish()
    return self.nc


Builder.build_test_mixD = _build_test_mixD


def _phase_mlstm(self, l, yT, t_yT):
    c = self.c
    nc = self.nc
    SC = 128.0 ** -0.5
    with ExitStack() as es:
        cur = [es]
        S = lambda n, sh, dt: cur[0].enter_context(self.sb(n, sh, dt))
        P = lambda n, sh, dt=F32: es.enter_context(self.ps(n, sh, dt))
        pk = [P("L_p%d" % i, [128, 512]) for i in range(6)]
        t_pk = [Tok() for _ in range(6)]
        pb16 = [P("L_pb%d" % i, [128, 1024], BF16) for i in range(2)]
        t_pb16 = [Tok(), Tok()]
        qT = S("L_qT", [128, 4, SEQ], BF16)
        kT = S("L_kT", [128, 4, SEQ], BF16)
        vaug = S("L_vaug", [128, NT, 4, 129], BF16)
        so = S("L_so", [128, NT, 512], BF16)
        es1 = ExitStack()
        cur[0] = es1
        wbuf = S("L_w", [128, 2, 8, 512], BF16)
        wif = S("L_wif", [128, 8, 128], BF16)
        preb = S("L_preb", [128, 2, SEQ + 4], BF16)
        diagw = S("L_diagw", [128, 8, 4, 128], BF16)
        cw = S("L_cw", [128, 8, 4], F32)
        ifT = S("L_ifT", [128, 2, 512], F32)
        ifb = S("L_ifb", [128, 1], F32)

        t_c = Tok()
        c.op("pool", lambda e: e.memset(wif[:], 0.0), writes=[t_c])
        c.op("pool", lambda e: e.memset(vaug[:, :, :, 128:129], 1.0), writes=[t_c])
        c.op("pool", lambda e: e.memset(preb[:, :, 0:3], 0.0), writes=[t_c])
        c.dma("sp", cw[:], self.conv_wT[l].rearrange("(ct p) j -> p ct j", p=128), writes=[t_c])
        c.dma("sp", ifb[0:4, :], self.i_bias[l].rearrange("(h o) -> h o", o=1), writes=[t_c])
        c.dma("sp", ifb[4:8, :], self.f_bias[l].rearrange("(h o) -> h o", o=1), writes=[t_c])
        t_wif = Tok()
        c.dma("pool", wif[:, :, 0:8], self.w_in[l][:, C_MI:C_MI + 8].rearrange("(kt p) c -> p kt c", p=128),
              reads=[t_c], writes=[t_wif])

        t_w = [Tok(), Tok()]
        t_pre = [Tok(), Tok()]
        t_cacc = Tok()
        t_q = [Tok() for _ in range(4)]
        t_k = [Tok() for _ in range(4)]
        t_v = [Tok() for _ in range(NT)]
        t_so = [Tok() for _ in range(NT)]
        cnt = [0]

        t_dw = Tok()
        for ct in range(8):
            for j in range(4):
                c.op("dve", lambda e, ct=ct, j=j: e.tensor_scalar(out=diagw[:, ct, j, :], in0=self.ident_b[:],
                                                                  scalar1=cw[:, ct, j:j + 1], scalar2=None, op0=ALU.mult),
                     reads=[t_c, self.t_const], writes=[t_dw])
        pend = []

        def conv_tile(ct, pbi, dstT, m, t_dst):
            for nb in range(4):
                pi = 4 + nb % 2
                for j in range(4):
                    c.op("pe", lambda e, j=j, nb=nb, pi=pi: e.matmul(
                        pk[pi][:, :], diagw[:, ct, j, :], preb[:, pbi, nb * 512 + j:nb * 512 + j + 512],
                        start=(j == 0), stop=(j == 3)),
                         reads=[t_dw, t_pre[pbi]], writes=[t_pk[pi]], signal=(j == 3))
                c.op("act", lambda e, nb=nb, pi=pi: e.activation(out=dstT[:, m, nb * 512:(nb + 1) * 512], in_=pk[pi][:, :],
                                                                 func=AF.Silu), reads=[], writes=[t_dst[m], t_pk[pi]])

        def qk_proj(col0, dstT, t_dst, wb, ct_base):
            self.load_w(wbuf[:, wb, :, :], self.w_in[l][:, col0:col0 + 512], t_w[wb])
            for m in range(4):
                pbi = cnt[0] % 2
                cnt[0] += 1
                for nb in range(4):
                    pi = nb % 4
                    for k in range(8):
                        c.op("pe", lambda e, k=k, m=m, nb=nb, pi=pi: e.matmul(
                            pk[pi][:, :], wbuf[:, wb, k, m * 128:(m + 1) * 128], self.hT[:, k, nb * 512:(nb + 1) * 512],
                            start=(k == 0), stop=(k == 7)),
                             reads=[t_w[wb]] + self.t_hT[nb * 4:nb * 4 + 4], writes=[t_pk[pi]], signal=(k == 7))
                    if nb % 2 == 0:
                        c.op("act", lambda e, nb=nb, pi=pi, pbi=pbi: e.copy(
                            out=preb[:, pbi, 3 + nb * 512:3 + (nb + 1) * 512], in_=pk[pi][:, :]),
                             reads=[], writes=[t_pre[pbi], t_pk[pi]])
                    else:
                        c.op("dve", lambda e, nb=nb, pi=pi, pbi=pbi: e.tensor_copy(
                            out=preb[:, pbi, 3 + nb * 512:3 + (nb + 1) * 512], in_=pk[pi][:, :]),
                             reads=[], writes=[t_pre[pbi], t_pk[pi]])
                if pend:
                    conv_tile(*pend.pop())
                pend.append((ct_base + m, pbi, dstT, m, t_dst))

        qk_proj(C_MQ, qT, t_q, 0, 0)
        qk_proj(C_MK, kT, t_k, 1, 4)
        conv_tile(*pend.pop())
        self.load_w(wbuf[:, 0, :, :], self.w_in[l][:, C_MV:C_MV + 512], t_w[0])

        def ev_v(tt, ps, tps):
            c.op("dve", lambda e: e.tensor_copy(out=vaug[:, tt, :, 0:128],
                                                in_=ps[:, 0:512].rearrange("p (h d) -> p h d", h=4)),
                 reads=[t_c], writes=[t_v[tt], tps])
        self.proj_tm(wbuf[:, 0], t_w[0], 0, 512, pk[0:4], t_pk[0:4], ev_v)
        self.load_w(wbuf[:, 1, :, :], self.w_in[l][:, C_MO:C_MO + 512], t_w[1])

        def ev_o(tt, ps, tps):
            c.op("act", lambda e: e.activation(out=so[:, tt, :], in_=ps[:, 0:512], func=AF.Sigmoid),
                 reads=[], writes=[t_so[tt], tps])
        self.proj_tm(wbuf[:, 1], t_w[1], 0, 512, pk[0:4], t_pk[0:4], ev_o)
        t_ifb = [Tok(), Tok()]
        tscr = Tok()
        for nb in range(4):
            pi = nb % 4
            for k in range(8):
                c.op("pe", lambda e, k=k, nb=nb, pi=pi: e.matmul(pk[pi][:, :], wif[:, k, :],
                                                                 self.hT[:, k, nb * 512:(nb + 1) * 512],
                                                                 start=(k == 0), stop=(k == 7)),
                     reads=[t_wif] + self.t_hT[nb * 4:nb * 4 + 4], writes=[t_pk[pi]], signal=(k == 7))
            ib = nb % 2
            c.op("dve", lambda e, nb=nb, pi=pi, ib=ib: e.tensor_scalar(out=ifT[0:8, ib, :], in0=pk[pi][0:8, :],
                                                                       scalar1=ifb[0:8, 0:1], scalar2=None, op0=ALU.add),
                 reads=[t_c], writes=[t_ifb[ib], t_pk[pi]])
            c.dma("sp", self.ifscr[:, nb * 512:(nb + 1) * 512], ifT[0:8, ib, :], reads=[t_ifb[ib]], writes=[tscr])
        c.barrier()
        es1.close()
        cur[0] = es
        hg = S("L_hg", [128, 512], F32)
        G = S("L_G", [128, 12, 128], F32)
        F = S("L_F", [128, 4, 128], F32)
        cs = S("L_cs", [128, 4], F32)
        row = S("L_row", [128, 8, 64], F32)
        bc = S("L_bc", [128, 2, 64], F32)
        onesrow = S("L_onesrow", [128, 128], F32)
        e0 = S("L_e0", [128, 1], F32)
        ones = S("L_ones", [128, 128], F32)
        triu = S("L_triu", [128, 128], F32)
        tokfac = S("L_tokfac", [128, 4, 64], F32)
        CT = S("L_CT", [128, 4, 129], F32)
        CTb = S("L_CTb", [128, 4, 129], BF16)
        ktok = S("L_ktok", [128, 4, 128], BF16)
        vw = S("L_vw", [128, 4, 129], BF16)
        spT = S("L_spT", [128, 4, 128], BF16)
        res = S("L_res", [128, 4, 129], F32)
        tmpc = S("L_tmpc", [128, 4, 129], F32)
        sm = S("L_sm", [128, 4, 4], F32)
        hv = S("L_hv", [128, 4, 128], F32)
        junk = S("L_junk", [128, 128], BF16)
        ytok = S("L_ytok", [128, 2, 512], BF16)
        c.op("pool", lambda e: e.memset(onesrow[:], 0.0), writes=[t_c])
        c.op("pool", lambda e: e.memset(onesrow[0:1, :], 1.0), writes=[t_c])
        c.op("pool", lambda e: e.memset(e0[:], 0.0), writes=[t_c])
        c.op("pool", lambda e: e.memset(e0[0:1, :], 1.0), writes=[t_c])
        c.op("pool", lambda e: e.memset(ones[:], 1.0), writes=[t_c])
        c.op("pool", lambda e: e.memset(G[:], 0.0), writes=[t_c])
        c.op("pool", lambda e: e.memset(F[:], 0.0), writes=[t_c])
        c.op("pool", lambda e: e.memset(cs[:], 0.0), writes=[t_c])
        c.op("pool", lambda e: e.memset(row[:], 0.0), writes=[t_c])
        c.op("pool", lambda e: e.memset(CT[:], 0.0), writes=[t_c])
        c.op("pool", lambda e: e.memset(CTb[:], 0.0), writes=[t_c])
        c.dma("sp", triu[:], self.triu_in[:, :], writes=[t_c])
        c.dma("sp", hg[:], self.head_gain[l].partition_broadcast(128), writes=[t_c])
        t_G = Tok()
        c.dma("sp", G[0:64, 0, :], self.ifscr[0:4, :].rearrange("h (c t) -> (h c) t", t=128), reads=[tscr, t_c],
              writes=[t_G])
        c.dma("sp", G[0:64, 1, :], self.ifscr[4:8, :].rearrange("h (c t) -> (h c) t", t=128), reads=[tscr, t_c],
              writes=[t_G])
        R = slice(0, 64)

        def g_op(eng, fn, extra_r=()):
            c.op(eng, fn, reads=[t_c] + list(extra_r), writes=[t_G])
        g_op("act", lambda e: e.activation(out=G[R, 2, :], in_=G[R, 1, :], func=AF.Exp, scale=-1.0))
        g_op("act", lambda e: e.activation(out=G[R, 2, :], in_=G[R, 2, :], func=AF.Ln, bias=1.0, scale=1.0))
        g_op("dve", lambda e: e.tensor_tensor_scan(out=G[R, 3, :], data0=ones[R, :], data1=G[R, 2, :], initial=0.0,
                                                   op0=ALU.mult, op1=ALU.add))
        g_op("dve", lambda e: e.tensor_tensor(out=G[R, 4, :], in0=G[R, 0, :], in1=G[R, 3, :], op=ALU.add))
        g_op("dve", lambda e: e.tensor_tensor_scan(out=G[R, 5, :], data0=ones[R, :], data1=G[R, 4, :], initial=-1e30,
                                                   op0=ALU.mult, op1=ALU.max))
        g_op("dve", lambda e: e.tensor_scalar(out=cs[R, 0:1], in0=G[R, 3, 127:128], scalar1=-1.0, scalar2=None,
                                              op0=ALU.mult))
        g_op("dve", lambda e: e.tensor_copy(out=cs[R, 2:3], in_=G[R, 5, 127:128]))
        g_op("dve", lambda e: e.tensor_tensor(out=cs[R, 1:2], in0=cs[R, 2:3], in1=cs[R, 0:1], op=ALU.add))
        pr_ = pk[4]
        t_pr_ = t_pk[4]
        c.op("pe", lambda e: e.transpose(pr_[:, 0:128], cs[:, 0:1].broadcast_to([128, 128]), self.ident_f[:]),
             reads=[t_G, self.t_const], writes=[t_pr_])
        c.op("pe", lambda e: e.transpose(pr_[:, 128:256], cs[:, 1:2].broadcast_to([128, 128]), self.ident_f[:]),
             reads=[t_G, self.t_const], writes=[t_pr_])
        t_row = Tok()
        c.op("dve", lambda e: e.tensor_copy(out=row[0:1, 0:2, :], in_=pr_[0:1, 0:256].rearrange("p (a b) -> p a b", a=2)[:, :, 0:64]),
             reads=[t_c], writes=[t_row, t_pr_])
        for h in range(4):
            hs = slice(h * 16, (h + 1) * 16)
            c.op("dve", lambda e, hs=hs: e.tensor_tensor_scan(out=row[0:1, 2, hs], data0=row[0:1, 0, hs],
                                                              data1=row[0:1, 1, hs], initial=0.0, op0=ALU.add,
                                                              op1=ALU.max), reads=[], writes=[t_row])
            c.op("dve", lambda e, h=h: e.tensor_copy(out=row[0:1, 3, h * 16 + 1:(h + 1) * 16],
                                                     in_=row[0:1, 2, h * 16:(h + 1) * 16 - 1]), reads=[], writes=[t_row])
        c.op("dve", lambda e: e.tensor_tensor(out=row[0:1, 4, :], in0=row[0:1, 0, :], in1=row[0:1, 3, :], op=ALU.add),
             reads=[], writes=[t_row])
        c.op("dve", lambda e: e.tensor_tensor(out=row[0:1, 4, :], in0=row[0:1, 4, :], in1=row[0:1, 2, :],
                                              op=ALU.subtract), reads=[], writes=[t_row])
        c.op("dve", lambda e: e.tensor_tensor(out=row[0:1, 5, :], in0=row[0:1, 1, :], in1=row[0:1, 2, :],
                                              op=ALU.subtract), reads=[], writes=[t_row])
        c.op("act", lambda e: e.activation(out=row[0:1, 4:6, :], in_=row[0:1, 4:6, :], func=AF.Exp), reads=[],
             writes=[t_row])
        t_bc = Tok()
        c.op("pe", lambda e: e.matmul(pr_[:, 0:128], onesrow[:, :], row[:, 4:6, :].rearrange("p a b -> p (a b)"),
                                      start=True, stop=True), reads=[t_row, t_c], writes=[t_pr_])
        c.op("dve", lambda e: e.tensor_copy(out=bc[:], in_=pr_[:, 0:128].rearrange("p (a b) -> p a b", a=2)), reads=[],
             writes=[t_bc, t_pr_])
        c.op("pe", lambda e: e.matmul(pr_[0:64, 256:257], row[:, 3, :], e0[:, 0:1], start=True, stop=True),
             reads=[t_row, t_c], writes=[t_pr_])
        c.op("dve", lambda e: e.tensor_copy(out=cs[R, 3:4], in_=pr_[0:64, 256:257]), reads=[t_c], writes=[t_G, t_pr_])
        g_op("dve", lambda e: e.tensor_tensor(out=G[R, 6, :], in0=G[R, 5, :], in1=G[R, 3, :], op=ALU.subtract))
        g_op("dve", lambda e: e.tensor_scalar(out=G[R, 7, :], in0=G[R, 3, :], scalar1=-1.0, scalar2=cs[R, 3:4],
                                              op0=ALU.mult, op1=ALU.add))
        g_op("dve", lambda e: e.tensor_tensor(out=G[R, 8, :], in0=G[R, 6, :], in1=G[R, 7, :], op=ALU.max))
        g_op("dve", lambda e: e.tensor_scalar(out=G[R, 9, :], in0=G[R, 4, :], scalar1=cs[R, 2:3], scalar2=None,
                                              op0=ALU.subtract))
        g_op("act", lambda e: e.activation(out=F[R, 0, :], in_=G[R, 9, :], func=AF.Exp))
        g_op("dve", lambda e: e.tensor_scalar(out=F[R, 0, :], in0=F[R, 0, :], scalar1=SC, scalar2=None, op0=ALU.mult))
        g_op("dve", lambda e: e.tensor_tensor(out=G[R, 9, :], in0=G[R, 3, :], in1=G[R, 8, :], op=ALU.add))
        g_op("dve", lambda e: e.tensor_scalar(out=G[R, 9, :], in0=G[R, 9, :], scalar1=-1.0, scalar2=cs[R, 2:3],
                                              op0=ALU.mult, op1=ALU.add))
        g_op("act", lambda e: e.activation(out=F[R, 1, :], in_=G[R, 9, :], func=AF.Exp))
        g_op("dve", lambda e: e.tensor_tensor(out=G[R, 10, :], in0=G[R, 7, :], in1=G[R, 8, :], op=ALU.subtract))
        g_op("act", lambda e: e.activation(out=F[R, 2, :], in_=G[R, 10, :], func=AF.Exp))
        g_op("act", lambda e: e.activation(out=F[R, 3, :], in_=G[R, 8, :], func=AF.Exp, scale=-1.0))
        for q in range(4):
            c.op("pe", lambda e, q=q: e.transpose(pk[5][:, q * 128:(q + 1) * 128], F[:, q, :], self.ident_f[:]),
                 reads=[t_G, self.t_const], writes=[t_pk[5]])
        t_tf = Tok()
        c.op("dve", lambda e: e.tensor_copy(out=tokfac[:], in_=pk[5][:, :].rearrange("p (q m) -> p q m", q=4)[:, :, 0:64]),
             reads=[], writes=[t_tf, t_pk[5]])
        if self.debug.get("mlstm_dump"):
            self.dbg_dump("tokfac", tokfac[:], [128, 4, 64], [t_tf])
            self.dbg_dump("bc", bc[:], [128, 2, 64], [t_bc])
            self.dbg_dump("cs", cs[:], [128, 4], [t_G])

        t_CT = [Tok() for _ in range(4)]
        t_CTb = [Tok() for _ in range(4)]
        t_kt = [Tok() for _ in range(4)]
        t_vw = [Tok() for _ in range(4)]
        t_sp = [Tok() for _ in range(4)]
        t_res = [Tok() for _ in range(4)]
        t_tmp = [Tok() for _ in range(4)]
        t_sm = [Tok() for _ in range(4)]
        t_hv = [Tok() for _ in range(4)]
        t_junk = Tok()
        t_yt = [Tok(), Tok()]
        for cc in range(NT):
            csl = slice(cc * 128, (cc + 1) * 128)
            yb = cc % 2
            fac = []
            for h in range(4):
                n = h * 16 + cc
                fac.append((tokfac[:, 0, n:n + 1], tokfac[:, 1, n:n + 1], tokfac[:, 2, n:n + 1], tokfac[:, 3, n:n + 1], n))
            for h in range(4):
                fr = fac[h][0]
                p2 = h % 2
                c.op("pe", lambda e, h=h: e.transpose(pb16[0][:, h * 128:(h + 1) * 128], kT[:, h, csl], self.ident_b[:]),
                     reads=[t_k[h], self.t_const], writes=[t_pb16[0]], signal=(h == 3))
            c.op("act", lambda e: e.copy(out=ktok[:, :, :], in_=pb16[0][:, 0:512].rearrange("p (h t) -> p h t", h=4)),
                 reads=[], writes=t_kt + [t_pb16[0]])
            for h in range(4):
                fr = fac[h][0]
                p2 = h % 2
                c.op("act", lambda e, h=h, fr=fr: e.activation(out=vw[:, h, :], in_=vaug[:, cc, h, :], func=AF.Copy, scale=fr),
                     reads=[t_v[cc], t_tf, t_c], writes=[t_vw[h]])
                c.op("pe", lambda e, h=h, p2=p2: e.matmul(pk[p2][:, 0:128], kT[:, h, csl], qT[:, h, csl], start=True,
                                                          stop=True),
                     reads=[t_k[h], t_q[h]], writes=[t_pk[p2]])
                c.op("dve", lambda e, h=h, fr=fr, p2=p2: e.scalar_tensor_tensor(out=spT[:, h, :], in0=pk[p2][:, 0:128],
                                                                                scalar=fr, in1=triu[:], op0=ALU.mult,
                                                                                op1=ALU.mult),
                     reads=[t_tf, t_c], writes=[t_sp[h], t_pk[p2]])
            for h in range(4):
                fr, fc, fi, fe, n = fac[h]
                p2 = 2 + h % 2
                c.op("pe", lambda e, h=h, p2=p2: e.matmul(pk[p2][:, 0:129], spT[:, h, :], vaug[:, cc, h, :], start=True,
                                                          stop=True), reads=[t_sp[h], t_v[cc], t_c], writes=[t_pk[p2]],
                     signal=False)
                c.op("pe", lambda e, h=h, p2=p2: e.matmul(pk[p2][:, 256:385], qT[:, h, csl], CTb[:, h, :], start=True,
                                                          stop=True), reads=[t_q[h], t_CTb[h]], writes=[t_pk[p2]])
                c.op("dve", lambda e, h=h, fc=fc, p2=p2: e.tensor_scalar(out=tmpc[:, h, :], in0=pk[p2][:, 0:129], scalar1=fc,
                                                                         scalar2=None, op0=ALU.mult),
                     reads=[t_tf], writes=[t_tmp[h], t_pk[p2]])
                c.op("dve", lambda e, h=h, fi=fi, p2=p2: e.scalar_tensor_tensor(out=res[:, h, :], in0=pk[p2][:, 256:385],
                                                                                scalar=fi, in1=tmpc[:, h, :], op0=ALU.mult,
                                                                                op1=ALU.add),
                     reads=[t_tf, t_tmp[h]], writes=[t_res[h], t_pk[p2]])
            for h in range(4):
                n = fac[h][4]
                p2 = 4 + h % 2
                c.op("pe", lambda e, h=h, p2=p2: e.matmul(pk[p2][:, 0:129], ktok[:, h, :], vw[:, h, :], start=True, stop=True),
                     reads=[t_kt[h], t_vw[h]], writes=[t_pk[p2]])
                c.op("act", lambda e, h=h, n=n: e.activation(out=CT[:, h, :], in_=CT[:, h, :], func=AF.Copy,
                                                             scale=bc[:, 0, n:n + 1]),
                     reads=[t_bc, t_c], writes=[t_CT[h]])
                c.op("dve", lambda e, h=h, n=n, p2=p2: e.scalar_tensor_tensor(out=CT[:, h, :], in0=pk[p2][:, 0:129],
                                                                              scalar=bc[:, 1, n:n + 1], in1=CT[:, h, :],
                                                                              op0=ALU.mult, op1=ALU.add),
                     reads=[t_bc], writes=[t_CT[h], t_pk[p2]])
            for h in range(4):
                fe = fac[h][3]
                c.op("act", lambda e, h=h: e.copy(out=CTb[:, h, :], in_=CT[:, h, :]), reads=[t_CT[h]], writes=[t_CTb[h]])
                c.op("dve", lambda e, h=h: e.tensor_scalar(out=sm[:, h, 3:4], in0=res[:, h, 128:129], scalar1=-1.0,
                                                           scalar2=None, op0=ALU.mult), reads=[t_res[h]], writes=[t_sm[h]])
                c.op("dve", lambda e, h=h: e.tensor_tensor(out=sm[:, h, 0:1], in0=res[:, h, 128:129], in1=sm[:, h, 3:4],
                                                           op=ALU.max), reads=[t_res[h]], writes=[t_sm[h]])
                c.op("dve", lambda e, h=h, fe=fe: e.tensor_tensor(out=sm[:, h, 0:1], in0=sm[:, h, 0:1], in1=fe,
                                                                  op=ALU.max), reads=[t_tf], writes=[t_sm[h]])
                c.op("dve", lambda e, h=h: e.reciprocal(out=sm[:, h, 0:1], in_=sm[:, h, 0:1]), reads=[], writes=[t_sm[h]])
            for h in range(4):
                c.op("act", lambda e, h=h: e.activation(out=hv[:, h, :], in_=res[:, h, 0:128], func=AF.Copy,
                                                        scale=sm[:, h, 0:1]),
                     reads=[t_res[h], t_sm[h]], writes=[t_hv[h]])
                c.op("act", lambda e, h=h: e.activation(out=junk[:], in_=hv[:, h, :], func=AF.Square,
                                                        accum_out=sm[:, h, 1:2]),
                     reads=[t_hv[h]], writes=[t_junk, t_sm[h]])
                c.op("act", lambda e, h=h: e.activation(out=sm[:, h, 2:3], in_=sm[:, h, 1:2], func=AF.Sqrt, scale=1.0 / 128,
                                                        bias=self.eps_ap), reads=[self.t_const], writes=[t_sm[h]])
            for h in range(4):
                c.op("dve", lambda e, h=h: e.reciprocal(out=sm[:, h, 2:3], in_=sm[:, h, 2:3]), reads=[], writes=[t_sm[h]])
                c.op("dve", lambda e, h=h: e.scalar_tensor_tensor(out=hv[:, h, :], in0=hv[:, h, :], scalar=sm[:, h, 2:3],
                                                                  in1=hg[:, h * 128:(h + 1) * 128], op0=ALU.mult,
                                                                  op1=ALU.mult),
                     reads=[t_sm[h], t_c], writes=[t_hv[h]])
                c.op("dve", lambda e, h=h, yb=yb: e.tensor_tensor(out=ytok[:, yb, h * 128:(h + 1) * 128], in0=hv[:, h, :],
                                                                  in1=so[:, cc, h * 128:(h + 1) * 128], op=ALU.mult),
                     reads=[t_hv[h], t_so[cc]], writes=[t_yt[yb]])
            for ct in range(4):
                c.op("pe", lambda e, ct=ct, yb=yb: e.transpose(pb16[1][:, ct * 128:(ct + 1) * 128],
                                                               ytok[:, yb, ct * 128:(ct + 1) * 128], self.ident_b[:]),
                     reads=[t_yt[yb], self.t_const], writes=[t_pb16[1]], signal=(ct == 3))
            c.op("act", lambda e, cc=cc: e.copy(out=yT[:, :, cc * 128:(cc + 1) * 128],
                                                in_=pb16[1][:, 0:512].rearrange("p (c t) -> p c t", c=4)),
                 reads=[], writes=[t_yT[cc], t_pb16[1]])
    c.barrier()


Builder.phase_mlstm = _phase_mlstm


def _declare_mlstm_inputs(self):
    self.triu_in = self.inp("triu", [128, 128])
    self.conv_wT = self.inp("conv_wT", [DEPTH, 1024, 4])
    self.head_gain = self.inp("mlstm_head_gain", [DEPTH, 512])
    self.i_bias = self.inp("mlstm_i_bias", [DEPTH, 4])
    self.f_bias = self.inp("mlstm_f_bias", [DEPTH, 4])
    self.ifscr = self.nc.dram_tensor("ifscr", [8, SEQ], F32).ap()


Builder.declare_mlstm_inputs = _declare_mlstm_inputs


def _build_test_mlstm(self):
    self.declare_inputs()
    self.declare_mlstm_inputs()
    self.setup_consts()
    self.phase_A(0, self.x_in)
    self.yT = [self.sb("yT%d" % i, [128, 4, SEQ], BF16).__enter__() for i in range(3)]
    self.t_yT = [[Tok() for _ in range(NT)] for _ in range(3)]
    self.phase_mlstm(0, self.yT[1], self.t_yT[1])
    self.dbg_dump("y_mlstm", self.yT[1][:], [128, 4, SEQ], self.t_yT[1], BF16)
    self.finish()
    return self.nc


Builder.build_test_mlstm = _build_test_mlstm


def _declare_ssm_inputs(self):
    self.ssm_sp = self.inp("ssm_sp", [DEPTH, 128, 3, 16])
    self.ssm_b = self.inp("ssm_b", [DEPTH, 128, 2, 16, 32])
    self.ssm_c = self.inp("ssm_c", [DEPTH, 128, 2, 16, 32])
    self.ssm_dT = self.inp("ssm_dT", [DEPTH, 128, 4])
    self.bdmask_in = self.inp("bdmask", [128, 4])
    nc = self.nc
    self.ssm_injT_scr = nc.dram_tensor("ssm_injT_scr", [128, 4, SSM_L, 2, 128], BF16).ap()
    self.ssm_read_scr = nc.dram_tensor("ssm_read_scr", [128, SSM_L + 1, 2, 16, 32], BF16).ap()
    self.ssm_bb_scr = nc.dram_tensor("ssm_bb_scr", [128, 2, 16, 32], BF16).ap()
    self.ssm_m12_scr = nc.dram_tensor("ssm_m12_scr", [128, 2, 2, 16], F32).ap()


Builder.declare_ssm_inputs = _declare_ssm_inputs


def _ssm_setup_gen(self, l, es, pbanks, t_pbanks, injT_sb=None, t_injT=None):
    c = self.c
    L = SSM_L
    TWO_PI = 2.0 * math.pi
    MAGIC = 12582912.0
    S = lambda n, sh, dt: es.enter_context(self.sb(n, sh, dt))
    sp = S("Z_sp", [128, 3, 16], F32)
    APW = S("Z_APW", [128, L + 1, 2, 16], F32)
    Bb = S("Z_Bb", [128, 2, 16, 32], F32)
    Cc = S("Z_Cc", [128, 2, 16, 32], F32)
    W1 = S("Z_W1", [128, 2, 2, 16, 32], F32)
    W2 = S("Z_W2", [128, 2, 2, 16, 32], F32)
    nCc = S("Z_nCc", [128, 2, 16, 32], F32)
    sm = S("Z_sm", [128, 12, 16], F32)
    Inj = S("Z_Inj", [128, 1, 2, 2, 16, 32], F32)
    stg = S("Z_stg", [128, 2, 4, 2, 2, 128], BF16) if injT_sb is None else None
    rstg = S("Z_rstg", [128, 1, 2, 2, 16, 32], BF16)
    bstg = S("Z_bstg", [128, 2, 16, 32], BF16)
    M12 = S("Z_M12", [128, 2, 2, 16], F32)
    t_c = Tok()
    c.dma("sp", sp[:], self.ssm_sp[l], writes=[t_c])
    c.dma("sp", Bb[:], self.ssm_b[l], writes=[t_c])
    c.dma("sp", Cc[:], self.ssm_c[l], writes=[t_c])
    yield
    t_s = Tok()

    def sop(eng, fn):
        c.op(eng, fn, reads=[t_c], writes=[t_s])
    ar, ai, ldt = sp[:, 0, :], sp[:, 1, :], sp[:, 2, :]
    dt_, mag, th, cs_, sn_ = sm[:, 0, :], sm[:, 1, :], sm[:, 2, :], sm[:, 3, :], sm[:, 4, :]
    t0, t1_, abr, abi = sm[:, 5, :], sm[:, 6, :], sm[:, 7, :], sm[:, 8, :]
    fr, fi, rden = sm[:, 9, :], sm[:, 10, :], sm[:, 11, :]
    sop("act", lambda e: e.activation(out=dt_, in_=ldt, func=AF.Exp))
    sop("dve", lambda e: e.tensor_tensor(out=t0, in0=dt_, in1=ar, op=ALU.mult))
    sop("act", lambda e: e.activation(out=mag, in_=t0, func=AF.Exp))
    sop("dve", lambda e: e.tensor_tensor(out=th, in0=dt_, in1=ai, op=ALU.mult))

    def sin_of(dst, shift):
        sop("dve", lambda e: e.tensor_scalar(out=t0, in0=th, scalar1=shift, scalar2=None, op0=ALU.add))
        sop("dve", lambda e: e.tensor_scalar(out=t1_, in0=t0, scalar1=1.0 / TWO_PI, scalar2=MAGIC, op0=ALU.mult,
                                             op1=ALU.add))
        sop("dve", lambda e: e.tensor_scalar(out=t1_, in0=t1_, scalar1=-MAGIC, scalar2=None, op0=ALU.add))
        sop("dve", lambda e: e.scalar_tensor_tensor(out=t0, in0=t1_, scalar=-TWO_PI, in1=t0, op0=ALU.mult,
                                                    op1=ALU.add))
        sop("dve", lambda e: e.tensor_scalar(out=t0, in0=t0, scalar1=math.pi, scalar2=-math.pi, op0=ALU.min,
                                             op1=ALU.max))
        sop("act", lambda e: e.activation(out=dst, in_=t0, func=AF.Sin))
    sin_of(sn_, 0.0)
    sin_of(cs_, math.pi / 2)
    sop("dve", lambda e: e.tensor_tensor(out=abr, in0=mag, in1=cs_, op=ALU.mult))
    sop("dve", lambda e: e.tensor_tensor(out=abi, in0=mag, in1=sn_, op=ALU.mult))
    sop("dve", lambda e: e.tensor_tensor(out=t0, in0=ar, in1=ar, op=ALU.mult))
    sop("dve", lambda e: e.tensor_tensor(out=t1_, in0=ai, in1=ai, op=ALU.mult))
    sop("dve", lambda e: e.tensor_tensor(out=rden, in0=t0, in1=t1_, op=ALU.add))
    sop("dve", lambda e: e.reciprocal(out=rden, in_=rden))
    sop("dve", lambda e: e.tensor_scalar(out=mag, in0=abr, scalar1=-1.0, scalar2=None, op0=ALU.add))
    sop("dve", lambda e: e.tensor_tensor(out=t0, in0=mag, in1=ar, op=ALU.mult))
    sop("dve", lambda e: e.tensor_tensor(out=t1_, in0=abi, in1=ai, op=ALU.mult))
    sop("dve", lambda e: e.tensor_tensor(out=t0, in0=t0, in1=t1_, op=ALU.add))
    sop("dve", lambda e: e.tensor_tensor(out=fr, in0=t0, in1=rden, op=ALU.mult))
    sop("dve", lambda e: e.tensor_tensor(out=t0, in0=abi, in1=ar, op=ALU.mult))
    sop("dve", lambda e: e.tensor_tensor(out=t1_, in0=mag, in1=ai, op=ALU.mult))
    sop("dve", lambda e: e.tensor_tensor(out=t0, in0=t0, in1=t1_, op=ALU.subtract))
    sop("dve", lambda e: e.tensor_tensor(out=fi, in0=t0, in1=rden, op=ALU.mult))
    sop("dve", lambda e: e.memset(APW[:, 0, 0, :], 1.0))
    sop("dve", lambda e: e.memset(APW[:, 0, 1, :], 0.0))
    for k in range(1, L + 1):
        pr_, pi_ = APW[:, k - 1, 0, :], APW[:, k - 1, 1, :]
        sop("dve", lambda e, pr_=pr_: e.tensor_tensor(out=t0, in0=pr_, in1=abr, op=ALU.mult))
        sop("dve", lambda e, pi_=pi_: e.tensor_tensor(out=t1_, in0=pi_, in1=abi, op=ALU.mult))
        sop("dve", lambda e, k=k: e.tensor_tensor(out=APW[:, k, 0, :], in0=t0, in1=t1_, op=ALU.subtract))
        sop("dve", lambda e, pr_=pr_: e.tensor_tensor(out=t0, in0=pr_, in1=abi, op=ALU.mult))
        sop("dve", lambda e, pi_=pi_: e.tensor_tensor(out=t1_, in0=pi_, in1=abr, op=ALU.mult))
        sop("dve", lambda e, k=k: e.tensor_tensor(out=APW[:, k, 1, :], in0=t0, in1=t1_, op=ALU.add))

    def cmulK(out_r, out_i, Xr, Xi, Xi_imag_r, Xi_imag_i, k0, K, t_x, t_out):
        def xb(v):
            return v.unsqueeze(1).broadcast_to([128, K, 16, 32])

        def ab(ri):
            return APW[:, k0:k0 + K, ri, :].unsqueeze(3).broadcast_to([128, K, 16, 32])
        c.op("dve", lambda e: e.tensor_tensor(out=W1[:, 0, 0:K], in0=xb(Xr), in1=ab(0), op=ALU.mult), reads=[t_x, t_s],
             writes=[t_w1])
        c.op("dve", lambda e: e.tensor_tensor(out=W1[:, 1, 0:K], in0=xb(Xi), in1=ab(1), op=ALU.mult), reads=[t_x, t_s],
             writes=[t_w1])
        c.op("dve", lambda e: e.tensor_tensor(out=out_r, in0=W1[:, 0, 0:K], in1=W1[:, 1, 0:K], op=ALU.subtract),
             reads=[t_w1], writes=[t_out])
        c.op("pool", lambda e: e.tensor_tensor(out=W2[:, 0, 0:K], in0=xb(Xi_imag_r), in1=ab(1), op=ALU.mult),
             reads=[t_x, t_s], writes=[t_w2])
        c.op("pool", lambda e: e.tensor_tensor(out=W2[:, 1, 0:K], in0=xb(Xi_imag_i), in1=ab(0), op=ALU.mult),
             reads=[t_x, t_s], writes=[t_w2])
        c.op("pool", lambda e: e.tensor_tensor(out=out_i, in0=W2[:, 0, 0:K], in1=W2[:, 1, 0:K], op=ALU.add),
             reads=[t_w2], writes=[t_out])

    def bc32(v):
        return v.unsqueeze(2).broadcast_to([128, 16, 32])

    def cmul(out_r, out_i, Xr, Xi, Yr, Yi, t_x, t_out):
        c.op("dve", lambda e: e.tensor_tensor(out=W1[:, 0, 0], in0=Xr, in1=bc32(Yr), op=ALU.mult), reads=[t_x, t_s],
             writes=[t_w1])
        c.op("dve", lambda e: e.tensor_tensor(out=W1[:, 1, 0], in0=Xi, in1=bc32(Yi), op=ALU.mult), reads=[t_x, t_s],
             writes=[t_w1])
        c.op("dve", lambda e: e.tensor_tensor(out=out_r, in0=W1[:, 0, 0], in1=W1[:, 1, 0], op=ALU.subtract),
             reads=[t_w1], writes=[t_out])
        c.op("pool", lambda e: e.tensor_tensor(out=W2[:, 0, 0], in0=Xr, in1=bc32(Yi), op=ALU.mult), reads=[t_x, t_s],
             writes=[t_w2])
        c.op("pool", lambda e: e.tensor_tensor(out=W2[:, 1, 0], in0=Xi, in1=bc32(Yr), op=ALU.mult), reads=[t_x, t_s],
             writes=[t_w2])
        c.op("pool", lambda e: e.tensor_tensor(out=out_i, in0=W2[:, 0, 0], in1=W2[:, 1, 0], op=ALU.add), reads=[t_w2],
             writes=[t_out])
    t_w1, t_w2 = Tok(), Tok()
    t_Bb = Tok()
    t_inj = [Tok(), Tok()]
    cmul(Inj[:, 0, 0, 0], Inj[:, 0, 0, 1], Bb[:, 0], Bb[:, 1], fr, fi, t_c, t_inj[0])
    c.op("dve", lambda e: e.tensor_copy(out=Bb[:, 0], in_=Inj[:, 0, 0, 0]), reads=[t_inj[0], t_c], writes=[t_Bb])
    c.op("pool", lambda e: e.tensor_copy(out=Bb[:, 1], in_=Inj[:, 0, 0, 1]), reads=[t_inj[0], t_c], writes=[t_Bb])
    c.op("pool", lambda e: e.tensor_scalar(out=nCc[:], in0=Cc[:], scalar1=-1.0, scalar2=None, op0=ALU.mult),
         reads=[t_c], writes=[t_c])
    yield
    t_bst = Tok()
    c.op("act", lambda e: e.copy(out=bstg[:], in_=Bb[:]), reads=[t_Bb], writes=[t_bst])
    c.dma("sp", self.ssm_bb_scr, bstg[:], reads=[t_bst])
    t_m = Tok()
    c.op("dve", lambda e: e.tensor_copy(out=M12[:, 0, 0, :], in_=APW[:, L, 0, :]), reads=[t_s], writes=[t_m])
    c.op("dve", lambda e: e.tensor_copy(out=M12[:, 0, 1, :], in_=APW[:, L, 0, :]), reads=[t_s], writes=[t_m])
    c.op("dve", lambda e: e.tensor_scalar(out=M12[:, 1, 0, :], in0=APW[:, L, 1, :], scalar1=-1.0, scalar2=None,
                                          op0=ALU.mult), reads=[t_s], writes=[t_m])
    c.op("dve", lambda e: e.tensor_copy(out=M12[:, 1, 1, :], in_=APW[:, L, 1, :]), reads=[t_s], writes=[t_m])
    c.dma("sp", self.ssm_m12_scr, M12[:], reads=[t_m])
    yield
    t_stg = [Tok(), Tok()]
    for kp in range(L // 2):
        ib = 0
        sb_ = kp % 2
        cmulK(Inj[:, ib, :, 0], Inj[:, ib, :, 1], Bb[:, 0], Bb[:, 1], Bb[:, 0], Bb[:, 1], 2 * kp, 2, t_Bb, t_inj[ib])
        for kk in range(2):
            for ri in range(2):
                pi = (2 * kk + ri) % len(pbanks)
                pb, tpb = pbanks[pi], t_pbanks[pi]
                for T in range(4):
                    c.op("pe", lambda e, T=T, ri=ri, ib=ib, pb=pb, kk=kk: e.transpose(
                        pb[:, T * 128:(T + 1) * 128],
                        Inj[:, ib, kk, ri, 4 * T:4 * T + 4, :].rearrange("p a b -> p (a b)"),
                        self.ident_f[:]), reads=[t_inj[ib], self.t_const], writes=[tpb], signal=(T == 3))
                if injT_sb is not None:
                    k = 2 * kp + kk
                    c.op("act", lambda e, k=k, ri=ri, pb=pb: e.copy(out=injT_sb[:, :, k, ri, :],
                                                                  in_=pb[:, :].rearrange("p (t m) -> p t m", t=4)),
                         reads=[], writes=[t_injT, tpb])
                else:
                    c.op("act", lambda e, kk=kk, ri=ri, pb=pb, sb_=sb_: e.copy(out=stg[:, sb_, :, kk, ri, :],
                                                                              in_=pb[:, :].rearrange("p (t m) -> p t m", t=4)),
                         reads=[], writes=[t_stg[sb_], tpb])
        if injT_sb is None:
            c.dma("sp", self.ssm_injT_scr[:, :, 2 * kp:2 * kp + 2, :, :], stg[:, sb_], reads=[t_stg[sb_]])
        yield
    yield "inj_done"
    t_rst = [Tok(), Tok()]
    k0 = 0
    n_ = 0
    while k0 < L + 1:
        K = min(2, L + 1 - k0)
        rb = 0
        n_ += 1
        cmulK(rstg[:, rb, 0:K, 0], rstg[:, rb, 0:K, 1], Cc[:, 0], Cc[:, 1], nCc[:, 0], nCc[:, 1], k0, K, t_c, t_rst[rb])
        c.dma("sp", self.ssm_read_scr[:, k0:k0 + K], rstg[:, rb, 0:K], reads=[t_rst[rb]])
        k0 += K
        yield


Builder.ssm_setup_gen = _ssm_setup_gen


def _ssm_setup_standalone(self, l):
    with ExitStack() as es:
        banks = [es.enter_context(self.ps("Z_p%d" % i, [128, 512])) for i in range(4)]
        toks = [Tok() for _ in range(4)]
        for _ in self.ssm_setup_gen(l, es, banks, toks):
            pass
    self.c.barrier()


Builder.ssm_setup_standalone = _ssm_setup_standalone


def _phase_ssm(self, l, yT, t_yT):
    c = self.c
    L = SSM_L
    with ExitStack() as es:
        cur = [es]
        S = lambda n, sh, dt: cur[0].enter_context(self.sb(n, sh, dt))
        P = lambda n, sh, dt=F32: es.enter_context(self.ps(n, sh, dt))
        pk = [P("S_p%d" % i, [128, 512]) for i in range(8)]
        t_pk = [Tok() for _ in range(8)]
        uT = S("S_uT", [128, 4, SEQ], BF16)
        SL = S("S_SL", [128, NCH, 2, 16], F32)
        Bb = S("S_Bb16", [128, 2, 16, 32], BF16)
        M12 = S("S_M12", [128, 2, 2, 16], F32)
        dcol = S("S_dcol", [128, 4], F32)
        bdm = S("S_bdm", [128, 4], F32)
        es1 = ExitStack()
        cur[0] = es1
        wu = S("S_wu", [128, 8, 512], BF16)
        InjT = S("S_InjT", [128, 4, L, 2, 128], BF16)

        t_c = Tok()
        t_wu = Tok()
        t_injT = Tok()
        t_Bb = Tok()
        t_m = Tok()
        t_wg = Tok()
        self.load_w(wu[:], self.w_in[l][:, C_U:C_U + 512], t_wu)
        c.dma("sp", dcol[:], self.ssm_dT[l], writes=[t_c])
        c.dma("sp", bdm[:], self.bdmask_in[:, :], writes=[t_c])
        gen = self.ssm_setup_gen(l, es1, pk[4:8], t_pk[4:8], injT_sb=InjT, t_injT=t_injT)
        t_u = [Tok() for _ in range(4)]

        def ev_u(m, nb, ps, tps):
            eng = "act" if (m + nb) % 2 == 0 else "dve"
            if eng == "act":
                c.op("act", lambda e: e.copy(out=uT[:, m, nb * 512:(nb + 1) * 512], in_=ps[:, :]), reads=[],
                     writes=[t_u[m], tps])
            else:
                c.op("dve", lambda e: e.tensor_copy(out=uT[:, m, nb * 512:(nb + 1) * 512], in_=ps[:, :]), reads=[],
                     writes=[t_u[m], tps])
            next(gen, None)
        self.proj_fm(wu, t_wu, 4, pk[0:4], t_pk[0:4], ev_u)
        for r in gen:
            if r == "inj_done":
                break
        t_SL = Tok()
        cnt = 0
        for gp in range(16):
            T, j = gp // 4, gp % 4
            rows = slice(32 * j, 32 * j + 32)
            for ri in range(2):
                pb = pk[cnt % 4]
                tpb = t_pk[cnt % 4]
                cnt += 1
                for tp in range(L):
                    c.op("pe", lambda e, T=T, rows=rows, tp=tp, ri=ri, pb=pb, j=j: e.matmul(
                        pb[:, 0:NCH], InjT[rows, T, L - 1 - tp, ri, :], uT[rows, T, tp::L], start=(tp == 0),
                        stop=(tp == L - 1), tile_position=(32 * j, 0)),
                         reads=[t_injT, t_u[T]], writes=[tpb], signal=(tp == L - 1))
                eng = "act" if ri == 0 else "dve"
                if eng == "act":
                    c.op("act", lambda e, gp=gp, ri=ri, pb=pb: e.copy(out=SL[:, :, ri, gp], in_=pb[:, 0:NCH]), reads=[],
                         writes=[t_SL, tpb])
                else:
                    c.op("dve", lambda e, gp=gp, ri=ri, pb=pb: e.tensor_copy(out=SL[:, :, ri, gp], in_=pb[:, 0:NCH]),
                         reads=[], writes=[t_SL, tpb])
                next(gen, None)
        for _ in gen:
            pass
        c.barrier()
        es1.close()
        cur[0] = es
        Readb = S("S_Readb", [128, L + 1, 2, 16, 32], BF16)
        BD = S("S_BD", [128, 4, L, 128], BF16)
        Sinb = S("S_Sinb", [128, 2, 16, NCH], BF16)
        I0p = S("S_I0p", [128, 2, 4, 128], BF16)
        st = S("S_st", [128, 2, 2, 16], F32)
        SLflat = SL[:, :, :, :].rearrange("p c r g -> p (c r g)")
        yraws = [SLflat[:, 0:SEQ], SLflat[:, SEQ:2 * SEQ]]
        gtmp_t = S("S_gtmp", [128, SEQ], F32)
        gtmp = gtmp_t[:, :]
        wg = S("S_wg", [128, 4, 512], BF16)
        sgb = S("S_sgb", [128, 2, 512], BF16)
        c.dma("pool", wg[:], self.w_glu[l].rearrange("(kt p) c -> p kt c", p=128), writes=[t_wg])
        c.dma("sp", Bb[:], self.ssm_bb_scr, writes=[t_Bb])
        c.dma("sp", M12[:], self.ssm_m12_scr, writes=[t_m])
        t_rd = Tok()
        for kq in range(0, L + 1, 6):
            k1 = min(L + 1, kq + 6)
            c.dma("sp", Readb[:, kq:k1], self.ssm_read_scr[:, kq:k1], writes=[t_rd])
        t_s = t_m
        t_st = Tok()
        for cc in range(1, NCH - 1):
            prev = SL[:, cc - 1, :, :]
            prev_sw = SL[:, cc - 1, ::-1, :]
            c.op("dve", lambda e, prev=prev: e.tensor_tensor(out=st[:, 0], in0=M12[:, 0], in1=prev, op=ALU.mult),
                 reads=[t_m, t_SL], writes=[t_st])
            c.op("dve", lambda e, prev_sw=prev_sw: e.tensor_tensor(out=st[:, 1], in0=M12[:, 1], in1=prev_sw, op=ALU.mult),
                 reads=[t_m, t_SL], writes=[t_st])
            c.op("dve", lambda e, cc=cc: e.tensor_tensor(out=st[:, 0], in0=st[:, 0], in1=SL[:, cc, :, :], op=ALU.add),
                 reads=[t_SL], writes=[t_st])
            c.op("dve", lambda e, cc=cc: e.tensor_tensor(out=SL[:, cc, :, :], in0=st[:, 0], in1=st[:, 1], op=ALU.add),
                 reads=[t_st], writes=[t_SL])
        t_sin = Tok()
        c.op("pool", lambda e: e.memset(Sinb[:, :, :, 0:1], 0.0), writes=[t_sin])
        c.op("dve", lambda e: e.tensor_copy(out=Sinb[:, :, :, 1:NCH],
                                            in_=SL[:, 0:NCH - 1, :, :].rearrange("p c r g -> p r g c")),
             reads=[t_SL], writes=[t_sin])
        c.barrier()
        t_I0 = Tok()
        t_BD = [Tok() for _ in range(4)]
        t_yraws = [Tok(), Tok()]
        t_g = Tok()
        GC = 2.0 * math.sqrt(2.0 / math.pi)
        ev_cnt = 0
        for T in range(4):
            yraw = yraws[T % 2]
            t_yraw = t_yraws[T % 2]
            c.op("pool", lambda e: e.memset(I0p[:], 0.0), writes=[t_I0])
            for ri in range(2):
                for j in range(4):
                    c.op("pool", lambda e, ri=ri, j=j, T=T: e.tensor_copy(out=I0p[:, ri, j, 32 * j:32 * j + 32],
                                                                         in_=Bb[:, ri, 4 * T + j, :]),
                         reads=[t_Bb], writes=[t_I0])
            pb = pk[4 + T % 2]
            tpb = t_pk[4 + T % 2]
            n_mm = 0
            for ri in range(2):
                for j in range(4):
                    c.op("pe", lambda e, ri=ri, j=j, T=T, pb=pb, n_mm=n_mm: e.matmul(
                        pb[:, :], I0p[:, ri, j, :], Readb[:, 0:L, ri, 4 * T + j, :], start=(n_mm == 0), stop=(n_mm == 7)),
                         reads=[t_I0, t_rd], writes=[tpb], signal=(n_mm == 7))
                    n_mm += 1
            c.op("dve", lambda e, T=T, pb=pb: e.tensor_tensor(
                out=BD[:, T, :, :].rearrange("p k (j x) -> p k j x", j=4),
                in0=pb[:, :].rearrange("p (k x) -> p k x", k=L).unsqueeze(2).broadcast_to([128, L, 4, 32]),
                in1=bdm[:, :].unsqueeze(1).unsqueeze(3).broadcast_to([128, L, 4, 32]), op=ALU.mult),
                 reads=[t_c], writes=[t_BD[T], tpb])
            for s_ in range(L):
                pb2 = pk[ev_cnt % 4]
                tpb2 = t_pk[ev_cnt % 4]
                ev_cnt += 1
                n_tot = (s_ + 1) + 8
                n_i = 0
                for tp in range(s_ + 1):
                    c.op("pe", lambda e, T=T, tp=tp, s_=s_, pb2=pb2, n_i=n_i: e.matmul(
                        pb2[:, 0:NCH], BD[:, T, s_ - tp, :], uT[:, T, tp::L], start=(n_i == 0), stop=False),
                         reads=[t_BD[T], t_u[T]], writes=[tpb2], signal=False)
                    n_i += 1
                for j in range(4):
                    for ri in range(2):
                        last = (n_i == n_tot - 1)
                        c.op("pe", lambda e, T=T, j=j, ri=ri, s_=s_, pb2=pb2, last=last: e.matmul(
                            pb2[32 * j:32 * j + 32, 0:NCH], Readb[:, s_ + 1, ri, 4 * T + j, :], Sinb[:, ri, 4 * T + j, :],
                            start=False, stop=(ri == 1), tile_position=(0, 32 * j)),
                             reads=[t_rd, t_sin], writes=[tpb2], signal=last)
                        n_i += 1
                if s_ % 2 == 0:
                    c.op("act", lambda e, s_=s_, pb2=pb2: e.copy(out=yraw[:, s_:SEQ:L], in_=pb2[:, 0:NCH]), reads=[],
                         writes=[t_yraw, tpb2])
                else:
                    c.op("dve", lambda e, s_=s_, pb2=pb2: e.tensor_copy(out=yraw[:, s_:SEQ:L], in_=pb2[:, 0:NCH]), reads=[],
                         writes=[t_yraw, tpb2])
            c.op("dve", lambda e, T=T: e.scalar_tensor_tensor(out=yraw, in0=uT[:, T, :], scalar=dcol[:, T:T + 1],
                                                              in1=yraw, op0=ALU.mult, op1=ALU.add),
                 reads=[t_u[T], t_c], writes=[t_yraw])
            c.op("act", lambda e: e.activation(out=gtmp, in_=yraw, func=AF.Square), reads=[t_yraw],
                 writes=[t_g])
            c.op("pool", lambda e: e.tensor_scalar(out=gtmp, in0=gtmp, scalar1=0.044715, scalar2=1.0, op0=ALU.mult,
                                                   op1=ALU.add), reads=[], writes=[t_g])
            c.op("dve", lambda e: e.tensor_tensor(out=gtmp, in0=gtmp, in1=yraw, op=ALU.mult), reads=[t_yraw],
                 writes=[t_g])
            c.op("act", lambda e: e.activation(out=gtmp, in_=gtmp, func=AF.Sigmoid, scale=GC), reads=[], writes=[t_g])
            c.op("dve", lambda e, T=T: e.tensor_tensor(out=yT[:, T, :], in0=gtmp, in1=yraw, op=ALU.mult),
                 reads=[t_g, t_yraw], writes=t_yT)
        t_sg = [Tok() for _ in range(2)]
        for nb in range(4):
            toks = t_yT[nb * 4:nb * 4 + 4]
            for m in range(4):
                for k in range(4):
                    c.op("pe", lambda e, m=m, k=k, nb=nb: e.matmul(pk[m][:, :], wg[:, k, m * 128:(m + 1) * 128],
                                                                   yT[:, k, nb * 512:(nb + 1) * 512], start=(k == 0),
                                                                   stop=(k == 3)),
                         reads=[t_wg] + toks, writes=[t_pk[m]], signal=(k == 3))
            for m in range(4):
                c.op("act", lambda e, m=m: e.activation(out=sgb[:, m % 2, :], in_=pk[m][:, :], func=AF.Sigmoid), reads=[],
                     writes=[t_sg[m % 2], t_pk[m]])
                c.op("dve", lambda e, m=m, nb=nb: e.tensor_tensor(out=yT[:, m, nb * 512:(nb + 1) * 512],
                                                                  in0=yT[:, m, nb * 512:(nb + 1) * 512], in1=sgb[:, m % 2, :],
                                                                  op=ALU.mult), reads=[t_sg[m % 2]], writes=toks)
    c.barrier()


Builder.phase_ssm = _phase_ssm


def _build_test_ssm(self):
    self.declare_inputs()
    self.declare_ssm_inputs()
    self.setup_consts()
    self.phase_A(0, self.x_in)
    self.yT = [self.sb("yT%d" % i, [128, 4, SEQ], BF16).__enter__() for i in range(3)]
    self.t_yT = [[Tok() for _ in range(NT)] for _ in range(3)]
    self.phase_ssm(0, self.yT[0], self.t_yT[0])
    self.dbg_dump("y_ssm", self.yT[0][:], [128, 4, SEQ], self.t_yT[0], BF16)
    self.finish()
    return self.nc


Builder.build_test_ssm = _build_test_ssm


def host_ssm_layouts(inputs):
    Lr = np.asarray(inputs["ssm_a_re"]).shape[0]
    a_re, a_im, ldt = (np.asarray(inputs[k], np.float32) for k in ("ssm_a_re", "ssm_a_im", "ssm_log_dt"))
    b_re, b_im = np.asarray(inputs["ssm_b_re"], np.float32), np.asarray(inputs["ssm_b_im"], np.float32)
    c_re, c_im = np.asarray(inputs["ssm_c_re"], np.float32), np.asarray(inputs["ssm_c_im"], np.float32)
    sp = np.zeros((Lr, 128, 3, 16), np.float32)
    bb = np.zeros((Lr, 128, 2, 16, 32), np.float32)
    cc = np.zeros((Lr, 128, 2, 16, 32), np.float32)
    for g2 in range(2):
        rows = slice(g2 * 64, g2 * 64 + 64)
        cols = slice(g2 * 16, g2 * 16 + 16)
        sp[:, rows, 0, :] = a_re[:, g2::2, :].transpose(0, 2, 1)
        sp[:, rows, 1, :] = a_im[:, g2::2, :].transpose(0, 2, 1)
        sp[:, rows, 2, :] = ldt[:, None, g2::2]
        bb[:, rows, 0, :, cols] = b_re[:, g2::2].transpose(0, 2, 1, 3)
        bb[:, rows, 1, :, cols] = b_im[:, g2::2].transpose(0, 2, 1, 3)
        cc[:, rows, 0, :, cols] = c_re[:, g2::2].transpose(0, 3, 1, 2)
        cc[:, rows, 1, :, cols] = c_im[:, g2::2].transpose(0, 3, 1, 2)
    dT = np.ascontiguousarray(np.asarray(inputs["ssm_d"], np.float32).reshape(Lr, 4, 128).transpose(0, 2, 1))
    return {"ssm_sp": sp, "ssm_b": bb, "ssm_c": cc, "ssm_dT": dT}


def _build_full(self, depth=DEPTH):
    self.declare_inputs()
    self.declare_mlstm_inputs()
    self.declare_ssm_inputs()
    self.setup_consts()
    self.setup_moba_consts()
    src = self.x_in
    for l in range(depth):
        last = (l == depth - 1)
        self.ssm_next = None
        if l == 0:
            self.phase_A(l, src)
        y_cms = [self.nc.sbuf_tensor("yT%d_l%d" % (i, l), [128, 4, SEQ], BF16, side="right") for i in range(3)]
        self.yT = [cm.__enter__() for cm in y_cms]
        self.t_yT = [[Tok() for _ in range(NT)] for _ in range(3)]
        self.phase_ssm(l, self.yT[0], self.t_yT[0])
        self.phase_mlstm(l, self.yT[1], self.t_yT[1])
        self.phase_moba(l, self.yT[2], self.t_yT[2])
        with self.sb("D_mT", [128, 8, SEQ], BF16) as mT:
            self.mT = mT
            self.phase_D1(l)
            for cm in reversed(y_cms):
                cm.__exit__(None, None, None)
            self.phase_D2(l, src, self.xres)
        dst = self.out if last else self.xres2
        self.phase_E(l, self.xres, dst, next_gain=None if last else self.n_mix_pre[l + 1])
        src = self.xres2
    self.finish()
    return self.nc


Builder.build_full = _build_full

_CACHE = {}


def kernel(**inputs):
    if "b" not in _CACHE:
        b = Builder()
        b.build_full()
        _CACHE["b"] = b
    b = _CACHE["b"]
    ssm = host_ssm_layouts(inputs)
    n = 8
    in_maps = [make_in_map(b, inputs, core, ssm=ssm) for core in range(n)]
    res = run_bass_kernel_spmd(b.nc, in_maps, core_ids=list(range(n)))
    out = np.stack([np.asarray(res.results[i]["out"]) for i in range(n)], axis=0)
    return out.astype(np.float32, copy=False)


def _build_test_D(self):
    c = self.c
    self.declare_inputs()
    self.setup_consts()
    self.phase_A(0, self.x_in)
    with ExitStack() as es:
        self.yT = [es.enter_context(self.sb("yT%d" % i, [128, 4, SEQ], BF16)) for i in range(3)]
        self.t_yT = [[Tok() for _ in range(NT)] for _ in range(3)]
        for i in range(3):
            c.op("pool", lambda e, i=i: e.memset(self.yT[i][:], 0.0), writes=self.t_yT[i])
        self.phase_D(0, self.x_in, self.out)
    self.finish()
    return self.nc


Builder.build_test_D = _build_test_D


def _build_test_D_setup(self):
    c = self.c
    self.declare_inputs()
    self.declare_ssm_inputs()
    self.setup_consts()
    self.phase_A(0, self.x_in)
    self.ssm_next = 1
    with ExitStack() as es:
        self.yT = [es.enter_context(self.nc.sbuf_tensor("yT%d" % i, [128, 4, SEQ], BF16, side="right")) for i in range(3)]
        self.t_yT = [[Tok() for _ in range(NT)] for _ in range(3)]
        for i in range(3):
            c.op("pool", lambda e, i=i: e.memset(self.yT[i][:], 0.0), writes=self.t_yT[i])
        with self.sb("D_mT", [128, 8, SEQ], BF16) as mT:
            self.mT = mT
            self.phase_D1(0)
    self.finish()
    return self.nc


Builder.build_test_D_setup = _build_test_D_setup
```
